# Optimizing a Trainium2 kernel written in Bass

```python
import math
import jax, jax.numpy as jnp
from jax import lax
import numpy as np

D_MODEL = 2048
BATCH = 4
SEQ = 2048
DEPTH = 2
DEC_BATCH = 128
DEC_SEQ = 8
PAST_LEN = 16384
PAGE_SIZE = 128

P_DIM = 256
CONV_W = 4
CHUNK = 64
N_BRANCH = 3

ML_HEADS = 4
ML_DK = 128
ML_DV = 256
ML_GATE_CAP = 15.0

LRU_WIDTH = 1024
LRU_BLOCKS = 4
LRU_BLOCK = LRU_WIDTH // LRU_BLOCKS
LRU_C = 8.0

GDN_HEADS = 4
GDN_DK = 128
GDN_DV = 256
GDN_QK = GDN_HEADS * GDN_DK
GDN_CONV_DIM = 2 * GDN_QK + GDN_HEADS * GDN_DV

D_FF = 5632
N_EXPERTS = 8
TOP_K = 2
D_FF_EXPERT = 2816
N_DENSE = (DEPTH + 1) // 2
N_MOE = DEPTH // 2

DEEPNORM_ALPHA = (2 * DEPTH) ** 0.25
DEEPNORM_BETA = (8 * DEPTH) ** -0.25
LN_EPS = 1e-5
RMS_EPS = 1e-6
L2_EPS = 1e-6

IN_SPLITS = (
    ML_HEADS * ML_DK, ML_HEADS * ML_DK, ML_HEADS * ML_DV, ML_HEADS, ML_HEADS, ML_HEADS * ML_DV,
    LRU_WIDTH, LRU_WIDTH,
    GDN_CONV_DIM, GDN_HEADS, GDN_HEADS, GDN_HEADS * GDN_DV,
    N_BRANCH * D_MODEL,
)
D_IN = sum(IN_SPLITS)

kernel_name = 'hybrid_mlstm_rglru_gdn_decoder_step'

F32 = jnp.float32


def _layer_norm(x, g, b):
    xf = x.astype(F32)
    mu = jnp.mean(xf, -1, keepdims=True)
    var = jnp.mean(jnp.square(xf - mu), -1, keepdims=True)
    return ((xf - mu) * lax.rsqrt(var + LN_EPS) * g.astype(F32) + b.astype(F32)).astype(x.dtype)


def _rms_norm(x, w):
    xf = x.astype(F32)
    return xf * lax.rsqrt(jnp.mean(jnp.square(xf), -1, keepdims=True) + RMS_EPS) * w.astype(F32)


def _l2_normalize(x):
    return x * lax.rsqrt(jnp.sum(jnp.square(x), -1, keepdims=True) + L2_EPS)


def _causal_conv(x, buf, w):
    t = x.shape[1]
    xx = jnp.concatenate([buf.astype(x.dtype), x], axis=1)
    y = xx[:, 0:t] * w[0]
    for j in range(1, CONV_W):
        y = y + xx[:, j:j + t] * w[j]
    return y, xx[:, -(CONV_W - 1):]


def _chunk_len(t):
    return max(d for d in range(1, min(CHUNK, t) + 1) if t % d == 0)


def _to_chunks(a, nc, L):
    b = a.shape[0]
    a = a.reshape((b, nc, L) + a.shape[2:])
    return jnp.moveaxis(jnp.moveaxis(a, 3, 2), 1, 0)


def _from_chunks(a):
    nc, b, h, L = a.shape[:4]
    a = jnp.moveaxis(jnp.moveaxis(a, 0, 1), 3, 2)
    return a.reshape((b, nc * L, h) + a.shape[4:])


def _mlstm(q, k, v, i_pre, logf, c0, n0, m0):
    b, t = q.shape[:2]
    L = _chunk_len(t)
    nc = t // L
    q = q * ML_DK ** -0.5
    xs = tuple(_to_chunks(a, nc, L) for a in (q, k, v, i_pre, logf))
    causal = jnp.tril(jnp.ones((L, L), dtype=bool))

    def step(carry, xc):
        c, n, m = carry
        qc, kc, vc, ic, fc = xc
        bcum = jnp.cumsum(fc, axis=-1)
        m_t = bcum + jnp.maximum(m[..., None], lax.cummax(ic - bcum, axis=2))
        log_d = bcum[..., :, None] - bcum[..., None, :] + ic[..., None, :] - m_t[..., :, None]
        dmat = jnp.exp(jnp.where(causal, log_d, -jnp.inf))
        s = jnp.einsum('bhtd,bhsd->bhts', qc, kc) * dmat
        inter = jnp.exp(bcum + m[..., None] - m_t)
        num = jnp.einsum('bhts,bhse->bhte', s, vc) + inter[..., None] * jnp.einsum('bhed,bhtd->bhte', c, qc)
        den = jnp.sum(s, -1) + inter * jnp.einsum('bhd,bhtd->bht', n, qc)
        h = num / jnp.maximum(jnp.abs(den), jnp.exp(-m_t))[..., None]
        m_new = m_t[..., -1]
        w_end = jnp.exp(bcum[..., -1:] - bcum + ic - m_new[..., None])
        dec = jnp.exp(bcum[..., -1] + m - m_new)
        c_new = dec[..., None, None] * c + jnp.einsum('bhs,bhse,bhsd->bhed', w_end, vc, kc)
        n_new = dec[..., None] * n + jnp.einsum('bhs,bhsd->bhd', w_end, kc)
        return (c_new, n_new, m_new), h

    (c, n, m), h = lax.scan(step, (c0.astype(F32), n0.astype(F32), m0.astype(F32)), xs)
    return _from_chunks(h), c, n, m


def _rglru(xc, h0, w_a, b_a, w_x, b_x, lam):
    b, t, _ = xc.shape
    xb = xc.reshape(b, t, LRU_BLOCKS, LRU_BLOCK)
    r = jax.nn.sigmoid(jnp.einsum('btnd,nde->btne', xb, w_a.astype(F32)).reshape(b, t, LRU_WIDTH) + b_a.astype(F32))
    i = jax.nn.sigmoid(jnp.einsum('btnd,nde->btne', xb, w_x.astype(F32)).reshape(b, t, LRU_WIDTH) + b_x.astype(F32))
    log_a = -LRU_C * r * jax.nn.softplus(-lam.astype(F32))
    a = jnp.exp(log_a)
    u = jnp.sqrt(-jnp.expm1(2.0 * log_a)) * (i * xc)
    u = u.at[:, 0].add(a[:, 0] * h0.astype(F32))

    def comb(left, right):
        a1, b1 = left
        a2, b2 = right
        return a1 * a2, a2 * b1 + b2

    _, h = lax.associative_scan(comb, (a, u), axis=1)
    return h, h[:, -1]


def _gated_delta(q, k, v, beta, g, s0):
    b, t = q.shape[:2]
    L = _chunk_len(t)
    nc = t // L
    q = q * GDN_DK ** -0.5
    qc, kc, vc = (_to_chunks(a, nc, L) for a in (q, k, v))
    bc = _to_chunks(beta, nc, L)
    gcum = jnp.cumsum(_to_chunks(g, nc, L), axis=-1)
    causal = jnp.tril(jnp.ones((L, L), dtype=bool))
    strict = jnp.tril(jnp.ones((L, L), dtype=bool), -1)
    decay = jnp.exp(jnp.where(causal, gcum[..., :, None] - gcum[..., None, :], -jnp.inf))
    kk = jnp.einsum('cbhtd,cbhsd->cbhts', kc, kc)
    a_mat = jnp.where(strict, bc[..., None] * kk * decay, 0.0) + jnp.eye(L, dtype=F32)
    rhs = jnp.concatenate([bc[..., None] * vc, (bc * jnp.exp(gcum))[..., None] * kc], axis=-1)
    sol = lax.linalg.triangular_solve(a_mat, rhs, left_side=True, lower=True)
    u_pre, w = sol[..., :GDN_DV], sol[..., GDN_DV:]
    qk = jnp.einsum('cbhtd,cbhsd->cbhts', qc, kc) * decay
    q_dec = qc * jnp.exp(gcum)[..., None]
    g_end = gcum[..., -1]
    k_dec = kc * jnp.exp(g_end[..., None] - gcum)[..., None]

    def step(s, xc):
        u_c, w_c, qk_c, qd_c, kd_c, ge_c = xc
        u = u_c - jnp.einsum('bhtd,bhde->bhte', w_c, s)
        o = jnp.einsum('bhtd,bhde->bhte', qd_c, s) + jnp.einsum('bhts,bhse->bhte', qk_c, u)
        s_new = jnp.exp(ge_c)[..., None, None] * s + jnp.einsum('bhsd,bhse->bhde', kd_c, u)
        return s_new, o

    s_fin, o = lax.scan(step, s0.astype(F32), (u_pre, w, qk, q_dec, k_dec, g_end))
    return _from_chunks(o), s_fin


def _split_cols(h):
    idx = np.cumsum(np.array(IN_SPLITS))[:-1].tolist()
    return jnp.split(h, idx, axis=-1)


def _mixer(x, st, l, P):
    b, t, _ = x.shape
    (ml_q, ml_k, ml_v, ml_i, ml_f, ml_o, rg_x, rg_y,
     gd_qkv, gd_b, gd_a, gd_z, mg) = _split_cols(x @ P['w_in'][l])

    cap = lambda z: ML_GATE_CAP * jnp.tanh(z / ML_GATE_CAP)
    i_pre = cap(ml_i.astype(F32) + P['ml_b_i'][l].astype(F32))
    logf = jax.nn.log_sigmoid(cap(ml_f.astype(F32) + P['ml_b_f'][l].astype(F32)))
    h_a, ml_c, ml_n, ml_m = _mlstm(
        ml_q.astype(F32).reshape(b, t, ML_HEADS, ML_DK),
        ml_k.astype(F32).reshape(b, t, ML_HEADS, ML_DK),
        ml_v.astype(F32).reshape(b, t, ML_HEADS, ML_DV),
        i_pre, logf, st['ml_C'], st['ml_n'], st['ml_m'])
    h_a = _rms_norm(h_a, P['ml_norm_w'][l].reshape(ML_HEADS, ML_DV)).reshape(b, t, ML_HEADS * ML_DV)
    y_a = (jax.nn.sigmoid(ml_o.astype(F32)) * h_a).astype(x.dtype)

    xc, rg_conv = _causal_conv(rg_x, st['rg_conv'], P['rg_conv_w'][l])
    xc = xc.astype(F32) + P['rg_conv_b'][l].astype(F32)
    h_b, rg_h = _rglru(xc, st['rg_h'], P['rg_w_a'][l], P['rg_b_a'][l], P['rg_w_x'][l], P['rg_b_x'][l],
                       P['rg_lambda'][l])
    y_b = (jax.nn.gelu(rg_y.astype(F32)) * h_b).astype(x.dtype)

    qkv, gd_conv = _causal_conv(gd_qkv, st['gd_conv'], P['gd_conv_w'][l])
    qkv = jax.nn.silu(qkv.astype(F32))
    g_q = _l2_normalize(qkv[..., :GDN_QK].reshape(b, t, GDN_HEADS, GDN_DK))
    g_k = _l2_normalize(qkv[..., GDN_QK:2 * GDN_QK].reshape(b, t, GDN_HEADS, GDN_DK))
    g_v = qkv[..., 2 * GDN_QK:].reshape(b, t, GDN_HEADS, GDN_DV)
    beta = jax.nn.sigmoid(gd_b.astype(F32))
    g = -jnp.exp(P['gd_A_log'][l].astype(F32)) * jax.nn.softplus(gd_a.astype(F32) + P['gd_dt_bias'][l].astype(F32))
    h_c, gd_s = _gated_delta(g_q, g_k, g_v, beta, g, st['gd_S'])
    h_c = _rms_norm(h_c, P['gd_norm_w'][l]) * jax.nn.silu(gd_z.astype(F32).reshape(b, t, GDN_HEADS, GDN_DV))
    y_c = h_c.reshape(b, t, GDN_HEADS * GDN_DV).astype(x.dtype)

    g_a, g_b, g_c = jnp.split(jax.nn.sigmoid(mg), N_BRANCH, axis=-1)
    merged = (g_a * (y_a @ P['w_up_mlstm'][l]) + g_b * (y_b @ P['w_up_rglru'][l])
              + g_c * (y_c @ P['w_up_gdn'][l]))
    out = merged @ P['w_out'][l]
    new_st = dict(ml_C=ml_c, ml_n=ml_n, ml_m=ml_m, rg_h=rg_h, rg_conv=rg_conv, gd_S=gd_s, gd_conv=gd_conv)
    return out, new_st


def _swiglu(x, w1, w3, w2):
    return (jax.nn.silu(x @ w1) * (x @ w3)) @ w2


def _moe(x, router, w1, w3, w2):
    b, t, d = x.shape
    xt = x.reshape(b * t, d)
    logits = (xt @ router).astype(F32)
    top_v, top_i = lax.top_k(logits, TOP_K)
    top_w = jax.nn.softmax(top_v, axis=-1)
    gates = jnp.sum(top_w[..., None] * jax.nn.one_hot(top_i, N_EXPERTS, dtype=F32), axis=1)
    out = jnp.zeros((b * t, d), F32)
    for e in range(N_EXPERTS):
        out = out + gates[:, e:e + 1] * _swiglu(xt, w1[e], w3[e], w2[e]).astype(F32)
    return out.astype(x.dtype).reshape(b, t, d)


def _run_trunk(x, p, states, P):
    names = ('ml_C', 'ml_n', 'ml_m', 'rg_h', 'rg_conv', 'gd_S', 'gd_conv')
    new = {k: [] for k in names}
    for l in range(DEPTH):
        st = {k: states[k][l] for k in names}
        mix, st_new = _mixer(x, st, l, P)
        x = _layer_norm(DEEPNORM_ALPHA * x + mix, P['ln1_g'][l], P['ln1_b'][l])
        if l % 2 == 0:
            j = l // 2
            f = _swiglu(x, P['ffn_w1'][j], P['ffn_w3'][j], P['ffn_w2'][j])
        else:
            j = l // 2
            f = _moe(x, P['moe_router'][j], P['moe_w1'][j], P['moe_w3'][j], P['moe_w2'][j])
        ple = jax.nn.sigmoid(x @ P['ple_gate_w'][l]) * (p[l].astype(x.dtype) @ P['ple_w'][l])
        x = _layer_norm(DEEPNORM_ALPHA * x + f + ple, P['ln2_g'][l], P['ln2_b'][l])
        for k in names:
            new[k].append(st_new[k])
    return x, {k: jnp.stack(v) for k, v in new.items()}


def setup_inputs(seed: int = 0) -> dict:
    key = jax.random.key(seed)
    ks = iter(jax.random.split(key, 64))

    def nrm(shape, scale=1.0):
        return scale * jax.random.normal(next(ks), shape, F32)

    x_prompt = nrm((BATCH, SEQ, D_MODEL))
    x_sample = nrm((DEC_BATCH, DEC_SEQ, D_MODEL))
    state_mlstm_C = nrm((DEPTH, DEC_BATCH, ML_HEADS, ML_DV, ML_DK), 0.3)
    state_mlstm_n = nrm((DEPTH, DEC_BATCH, ML_HEADS, ML_DK), 0.3)
    state_mlstm_m = nrm((DEPTH, DEC_BATCH, ML_HEADS), 1.0)
    state_rglru_h = nrm((DEPTH, DEC_BATCH, LRU_WIDTH), 0.5)
    state_rglru_conv = nrm((DEPTH, DEC_BATCH, CONV_W - 1, LRU_WIDTH))
    state_gdn_S = nrm((DEPTH, DEC_BATCH, GDN_HEADS, GDN_DK, GDN_DV), GDN_DK ** -0.5)
    state_gdn_conv = nrm((DEPTH, DEC_BATCH, CONV_W - 1, GDN_CONV_DIM))
    p_prompt = nrm((DEPTH, BATCH, SEQ, P_DIM))
    p_sample = nrm((DEPTH, DEC_BATCH, DEC_SEQ, P_DIM))

    w_in = nrm((DEPTH, D_MODEL, D_IN), D_MODEL ** -0.5)
    ml_b_i = nrm((DEPTH, ML_HEADS), 0.1)
    ml_b_f = 3.0 + nrm((DEPTH, ML_HEADS), 0.5)
    ml_norm_w = 1.0 + nrm((DEPTH, ML_HEADS * ML_DV), 0.02)
    rg_conv_w = nrm((DEPTH, CONV_W, LRU_WIDTH), CONV_W ** -0.5)
    rg_conv_b = nrm((DEPTH, LRU_WIDTH), 0.02)
    rg_w_a = nrm((DEPTH, LRU_BLOCKS, LRU_BLOCK, LRU_BLOCK), LRU_BLOCK ** -0.5)
    rg_b_a = nrm((DEPTH, LRU_WIDTH), 0.02)
    rg_w_x = nrm((DEPTH, LRU_BLOCKS, LRU_BLOCK, LRU_BLOCK), LRU_BLOCK ** -0.5)
    rg_b_x = nrm((DEPTH, LRU_WIDTH), 0.02)
    a_target = jax.random.uniform(next(ks), (DEPTH, LRU_WIDTH), F32, 0.9, 0.999)
    s_lam = a_target ** (1.0 / LRU_C)
    rg_lambda = jnp.log(s_lam) - jnp.log1p(-s_lam)
    gd_conv_w = nrm((DEPTH, CONV_W, GDN_CONV_DIM), CONV_W ** -0.5)
    gd_A_log = jnp.log(jax.random.uniform(next(ks), (DEPTH, GDN_HEADS), F32, 1.0, 16.0))
    dt = jnp.exp(jax.random.uniform(next(ks), (DEPTH, GDN_HEADS), F32, math.log(1e-3), math.log(1e-1)))
    gd_dt_bias = dt + jnp.log(-jnp.expm1(-dt))
    gd_norm_w = 1.0 + nrm((DEPTH, GDN_DV), 0.02)
    w_up_mlstm = nrm((DEPTH, ML_HEADS * ML_DV, D_MODEL), (ML_HEADS * ML_DV) ** -0.5)
    w_up_rglru = nrm((DEPTH, LRU_WIDTH, D_MODEL), LRU_WIDTH ** -0.5)
    w_up_gdn = nrm((DEPTH, GDN_HEADS * GDN_DV, D_MODEL), (GDN_HEADS * GDN_DV) ** -0.5)
    w_out = nrm((DEPTH, D_MODEL, D_MODEL), DEEPNORM_BETA * D_MODEL ** -0.5)
    ln1_g = 1.0 + nrm((DEPTH, D_MODEL), 0.02)
    ln1_b = nrm((DEPTH, D_MODEL), 0.02)
    ffn_w1 = nrm((N_DENSE, D_MODEL, D_FF), D_MODEL ** -0.5)
    ffn_w3 = nrm((N_DENSE, D_MODEL, D_FF), D_MODEL ** -0.5)
    ffn_w2 = nrm((N_DENSE, D_FF, D_MODEL), DEEPNORM_BETA * D_FF ** -0.5)
    moe_router = nrm((N_MOE, D_MODEL, N_EXPERTS), D_MODEL ** -0.5)
    moe_w1 = nrm((N_MOE, N_EXPERTS, D_MODEL, D_FF_EXPERT), D_MODEL ** -0.5)
    moe_w3 = nrm((N_MOE, N_EXPERTS, D_MODEL, D_FF_EXPERT), D_MODEL ** -0.5)
    moe_w2 = nrm((N_MOE, N_EXPERTS, D_FF_EXPERT, D_MODEL), DEEPNORM_BETA * D_FF_EXPERT ** -0.5)
    ple_w = nrm((DEPTH, P_DIM, D_MODEL), P_DIM ** -0.5)
    ple_gate_w = nrm((DEPTH, D_MODEL, D_MODEL), D_MODEL ** -0.5)
    ln2_g = 1.0 + nrm((DEPTH, D_MODEL), 0.02)
    ln2_b = nrm((DEPTH, D_MODEL), 0.02)
    return {
        'x_prompt': x_prompt, 'x_sample': x_sample,
        'state_mlstm_C': state_mlstm_C, 'state_mlstm_n': state_mlstm_n, 'state_mlstm_m': state_mlstm_m,
        'state_rglru_h': state_rglru_h, 'state_rglru_conv': state_rglru_conv,
        'state_gdn_S': state_gdn_S, 'state_gdn_conv': state_gdn_conv,
        'p_prompt': p_prompt, 'p_sample': p_sample,
        'w_in': w_in, 'ml_b_i': ml_b_i, 'ml_b_f': ml_b_f, 'ml_norm_w': ml_norm_w,
        'rg_conv_w': rg_conv_w, 'rg_conv_b': rg_conv_b, 'rg_w_a': rg_w_a, 'rg_b_a': rg_b_a,
        'rg_w_x': rg_w_x, 'rg_b_x': rg_b_x, 'rg_lambda': rg_lambda,
        'gd_conv_w': gd_conv_w, 'gd_A_log': gd_A_log, 'gd_dt_bias': gd_dt_bias, 'gd_norm_w': gd_norm_w,
        'w_up_mlstm': w_up_mlstm, 'w_up_rglru': w_up_rglru, 'w_up_gdn': w_up_gdn, 'w_out': w_out,
        'ln1_g': ln1_g, 'ln1_b': ln1_b,
        'ffn_w1': ffn_w1, 'ffn_w3': ffn_w3, 'ffn_w2': ffn_w2,
        'moe_router': moe_router, 'moe_w1': moe_w1, 'moe_w3': moe_w3, 'moe_w2': moe_w2,
        'ple_w': ple_w, 'ple_gate_w': ple_gate_w, 'ln2_g': ln2_g, 'ln2_b': ln2_b,
    }


def reference(x_prompt, x_sample, state_mlstm_C, state_mlstm_n, state_mlstm_m, state_rglru_h,
              state_rglru_conv, state_gdn_S, state_gdn_conv, p_prompt, p_sample,
              w_in, ml_b_i, ml_b_f, ml_norm_w, rg_conv_w, rg_conv_b, rg_w_a, rg_b_a, rg_w_x, rg_b_x,
              rg_lambda, gd_conv_w, gd_A_log, gd_dt_bias, gd_norm_w, w_up_mlstm, w_up_rglru, w_up_gdn,
              w_out, ln1_g, ln1_b, ffn_w1, ffn_w3, ffn_w2, moe_router, moe_w1, moe_w3, moe_w2,
              ple_w, ple_gate_w, ln2_g, ln2_b):
    P = dict(w_in=w_in, ml_b_i=ml_b_i, ml_b_f=ml_b_f, ml_norm_w=ml_norm_w, rg_conv_w=rg_conv_w,
             rg_conv_b=rg_conv_b, rg_w_a=rg_w_a, rg_b_a=rg_b_a, rg_w_x=rg_w_x, rg_b_x=rg_b_x,
             rg_lambda=rg_lambda, gd_conv_w=gd_conv_w, gd_A_log=gd_A_log, gd_dt_bias=gd_dt_bias,
             gd_norm_w=gd_norm_w, w_up_mlstm=w_up_mlstm, w_up_rglru=w_up_rglru, w_up_gdn=w_up_gdn,
             w_out=w_out, ln1_g=ln1_g, ln1_b=ln1_b, ffn_w1=ffn_w1, ffn_w3=ffn_w3, ffn_w2=ffn_w2,
             moe_router=moe_router, moe_w1=moe_w1, moe_w3=moe_w3, moe_w2=moe_w2,
             ple_w=ple_w, ple_gate_w=ple_gate_w, ln2_g=ln2_g, ln2_b=ln2_b)

    b = x_prompt.shape[0]
    init = dict(
        ml_C=jnp.zeros((DEPTH, b, ML_HEADS, ML_DV, ML_DK), F32),
        ml_n=jnp.zeros((DEPTH, b, ML_HEADS, ML_DK), F32),
        ml_m=jnp.zeros((DEPTH, b, ML_HEADS), F32),
        rg_h=jnp.zeros((DEPTH, b, LRU_WIDTH), F32),
        rg_conv=jnp.zeros((DEPTH, b, CONV_W - 1, LRU_WIDTH), x_prompt.dtype),
        gd_S=jnp.zeros((DEPTH, b, GDN_HEADS, GDN_DK, GDN_DV), F32),
        gd_conv=jnp.zeros((DEPTH, b, CONV_W - 1, GDN_CONV_DIM), x_prompt.dtype))
    y_prompt, sp = _run_trunk(x_prompt, p_prompt, init, P)

    past = dict(ml_C=state_mlstm_C, ml_n=state_mlstm_n, ml_m=state_mlstm_m, rg_h=state_rglru_h,
                rg_conv=state_rglru_conv, gd_S=state_gdn_S, gd_conv=state_gdn_conv)
    y_sample, ss = _run_trunk(x_sample, p_sample, past, P)

    pd = x_prompt.dtype
    return (y_prompt, y_sample,
            sp['ml_C'].astype(pd), sp['ml_n'].astype(pd), sp['ml_m'].astype(pd), sp['rg_h'].astype(pd),
            sp['rg_conv'].astype(pd), sp['gd_S'].astype(pd), sp['gd_conv'].astype(pd),
            ss['ml_C'].astype(state_mlstm_C.dtype), ss['ml_n'].astype(state_mlstm_n.dtype),
            ss['ml_m'].astype(state_mlstm_m.dtype), ss['rg_h'].astype(state_rglru_h.dtype),
            ss['rg_conv'].astype(state_rglru_conv.dtype), ss['gd_S'].astype(state_gdn_S.dtype),
            ss['gd_conv'].astype(state_gdn_conv.dtype))
```

```python
import contextlib
import os
import numpy as np
import concourse.bass as bass
import concourse.mybir as mybir
from concourse.bass_utils import run_bass_kernel_spmd

F32 = mybir.dt.float32
BF16 = mybir.dt.bfloat16
AF = mybir.ActivationFunctionType
ALU = mybir.AluOpType
AX = mybir.AxisListType

ENGS = ('pe', 'act', 'dve', 'pool', 'sp')
SEM_CAP = 30000
N_DMA_SEMS = {'sp': 16, 'act': 4, 'pool': 8}
NEG = -1.0e30
MOE_DBG = set(x for x in os.environ.get('MOE_DBG', '').split(',') if x)


class _Op:
    __slots__ = ('eng', 'fn', 'deps', 'dma', 'needs_inc', 'sem', 'val', 'accum')

    def __init__(self, eng, fn, dma, accum):
        self.eng = eng
        self.fn = fn
        self.dma = dma
        self.accum = accum
        self.deps = ()
        self.needs_inc = False
        self.sem = None
        self.val = 0


class _St:
    __slots__ = ('w', 'wd', 'r', 'rd')

    def __init__(self):
        self.w = {}
        self.wd = []
        self.r = {}
        self.rd = []


class Prog:
    def __init__(self, nc):
        self.nc = nc
        self.ops = {e: [] for e in ENGS}
        self.res = {}
        self.last = {}
        self.dmas = []
        self.pending = {e: [] for e in ENGS}

    def barrier(self):
        deps = list(self.last.values()) + list(self.dmas)
        for e in ENGS:
            self.pending[e] = list(deps)
        self.dmas = []
        self.res = {}

    def op(self, eng, fn, reads=(), writes=(), dma=False, accum=False):
        o = _Op(eng, fn, dma, accum)
        deps = []
        res = self.res
        for k in reads:
            st = res.get(k)
            if st is not None:
                deps.extend(st.w.values())
                deps.extend(st.wd)
                if isinstance(k, str) and k.startswith('ps') and k[2:].isdigit():
                    for re_, ro in st.r.items():
                        if re_ != eng:
                            deps.append(ro)
        for k in writes:
            st = res.get(k)
            if st is not None:
                for we, wo in st.w.items():
                    if accum and wo.accum and we == eng:
                        continue
                    deps.append(wo)
                deps.extend(st.wd)
                deps.extend(st.r.values())
                deps.extend(st.rd)
        if self.pending[eng]:
            deps.extend(self.pending[eng])
            self.pending[eng] = []
        for k in reads:
            st = res.get(k)
            if st is None:
                st = res[k] = _St()
            if dma:
                st.rd.append(o)
            else:
                st.r[eng] = o
        for k in writes:
            st = res.get(k)
            if st is None:
                st = res[k] = _St()
            if dma:
                st.w = {}
                st.wd = [o]
            else:
                st.w = {eng: o}
                st.wd = []
            st.r = {}
            st.rd = []
        dd = []
        seen = set()
        for d in deps:
            if d is o or id(d) in seen:
                continue
            seen.add(id(d))
            dd.append(d)
            d.needs_inc = True
        o.deps = dd
        self.ops[eng].append(o)
        if dma:
            self.dmas.append(o)
        else:
            self.last[eng] = o
        return o

    def emit(self, stack):
        nc = self.nc
        eng_sems = {}
        for e in ENGS:
            n_inc = sum(1 for o in self.ops[e] if (o.needs_inc and not o.dma))
            n_s = max(1, (n_inc + SEM_CAP - 1) // SEM_CAP)
            eng_sems[e] = [stack.enter_context(nc.semaphore(f"c_{e}_{i}")) for i in range(n_s)]
        dma_sems = {}
        for e in ('sp', 'act', 'pool'):
            if any(o.dma for o in self.ops[e]):
                dma_sems[e] = [stack.enter_context(nc.semaphore(f"d_{e}_{i}")) for i in range(N_DMA_SEMS[e])]
        final_dma = {}
        for e in ENGS:
            cnt = 0
            dcnt = 0
            dvals = {}
            for o in self.ops[e]:
                if o.dma:
                    ss = dma_sems[e]
                    s = ss[dcnt % len(ss)]
                    dcnt += 1
                    o.sem = s
                    o.val = dvals.get(id(s), 0) + 16
                    dvals[id(s)] = o.val
                    final_dma[id(s)] = (s, o.val)
                elif o.needs_inc:
                    o.sem = eng_sems[e][cnt // SEM_CAP]
                    o.val = cnt % SEM_CAP + 1
                    cnt += 1
        block = stack.enter_context(nc.Block())
        handles = {'pe': 'tensor', 'act': 'scalar', 'dve': 'vector', 'pool': 'gpsimd', 'sp': 'sync'}

        def make(e):
            def body(h):
                waited = {}
                for o in self.ops[e]:
                    for d in o.deps:
                        key = id(d.sem)
                        if waited.get(key, 0) >= d.val:
                            continue
                        h.wait_ge(d.sem, d.val)
                        waited[key] = d.val
                    if o.dma:
                        key = id(o.sem)
                        if o.val > 16 and waited.get(key, 0) < o.val - 16:
                            h.wait_ge(o.sem, o.val - 16)
                            waited[key] = o.val - 16
                        o.fn(h).then_inc(o.sem, 16)
                    else:
                        ins = o.fn(h)
                        if o.needs_inc:
                            ins.then_inc(o.sem, 1)
                if e == 'sp':
                    for (s, v) in final_dma.values():
                        if waited.get(id(s), 0) < v:
                            h.wait_ge(s, v)
            return body

        for e in ENGS:
            getattr(block, handles[e])(make(e))


class V:
    __slots__ = ('ap', 'key')

    def __init__(self, ap, key):
        self.ap = ap
        self.key = key

    def __getitem__(self, idx):
        return V(self.ap[idx], self.key)

    def bc(self, shape):
        return V(self.ap.broadcast_to(list(shape)), self.key)

    def un(self, axis):
        return V(self.ap.unsqueeze(axis), self.key)

    def k(self, sub):
        return V(self.ap, (self.key, sub))

    def re(self, pat, **kw):
        return V(self.ap.rearrange(pat, **kw), self.key)


class Cfg:
    def __init__(self, D=2048, SEQ=2048, DFF=5632, DFFE=2816, NE=8, PD=256, DEPTH=2):
        self.D, self.SEQ, self.DFF, self.DFFE, self.NE, self.PD, self.DEPTH = D, SEQ, DFF, DFFE, NE, PD, DEPTH
        self.H, self.DK, self.DV, self.LW = 4, 128, 256, 1024
        self.NS, self.TS = 16, 8
        self.KC = D // 128
        self.NT = SEQ + 128
        self.NTT = self.NT // 128
        self.NCH = SEQ // 64
        self.GC = 2048
        o = 0
        self.off = {}
        for name, n in (('mlq', 512), ('mlk', 512), ('mlv', 1024), ('mli', 4), ('mlf', 4), ('mlo', 1024),
                        ('rgx', 1024), ('rgy', 1024), ('gdqkv', 2048), ('gdb', 4), ('gda', 4), ('gdz', 1024),
                        ('mg', 3 * D)):
            self.off[name] = o
            o += n
        self.DIN = o
        self.groups = []
        t = 0
        while t < self.NT:
            n = min(512, self.NT - t)
            self.groups.append((t, n))
            t += n
        self.alpha = (2 * DEPTH) ** 0.25


def make_consts():
    idx = np.arange(128)
    c = {}
    c['ident'] = np.eye(128, dtype=np.float32)
    for nm, B in (('P', 128), ('S', 8)):
        same = (idx[:, None] // B) == (idx[None, :] // B)
        le = idx[:, None] <= idx[None, :]
        c['tri' + nm] = (same & le).astype(np.float32)
        c['negT' + nm] = np.where(same & le, 0.0, NEG).astype(np.float32)
        c['neg' + nm] = np.where(same & le, 0.0, NEG).astype(np.float32).T.copy()
        c['strictT' + nm] = (same & (idx[:, None] < idx[None, :])).astype(np.float32)
    lastP = np.zeros((128, 128), np.float32)
    lastP[63, :] = 1.0
    c['lastP'] = lastP
    lastS = np.zeros((128, 128), np.float32)
    for m in range(128):
        lastS[8 * (m // 8) + 7, m] = 1.0
    c['lastS'] = lastS
    c['ones'] = np.ones((128, 128), np.float32)
    bT = np.zeros((128, 128), np.float32)
    for m in range(128):
        bT[m // 8, m] = 1.0
    c['blockindT'] = bT
    names = ['ident', 'triP', 'negTP', 'negP', 'strictTP', 'lastP', 'triS', 'negTS', 'negS', 'strictTS', 'lastS',
             'ones', 'blockindT']
    arr = np.stack([c[n] for n in names]).astype(np.float32)
    blockind = np.zeros((128, 16), np.float32)
    lastind = np.zeros((128, 16), np.float32)
    for t in range(128):
        blockind[t, t // 8] = 1.0
    for i in range(16):
        lastind[8 * i + 7, i] = 1.0
    bm3 = np.zeros((128, 16, 128), np.float32)
    for i in range(16):
        bm3[:, i, 8 * i:8 * i + 8] = 1.0
    c2 = np.concatenate([blockind, lastind, bm3.reshape(128, 2048)], axis=1).astype(np.float32)
    return names, arr, c2


class Builder:
    def __init__(self, cfg, debug=False, stop_after=None):
        self.cfg = cfg
        self.debug = debug
        self.stop_after = stop_after
        self.nc = bass.Bass("TRN2", target_bir_lowering=False)
        self.P = Prog(self.nc)
        self.AW = 50176
        self.off = 0
        self.mark = 0
        self.dr = {}
        self.rr = {}
        self.rec = None
        self.stopped = False


    def emit_op(self, *a, **kw):
        if self.rec is not None:
            self.rec.append((a, kw))
        else:
            self.P.op(*a, **kw)

    def record(self, fn, *args):
        assert self.rec is None
        self.rec = []
        fn(*args)
        r, self.rec = self.rec, None
        return r

    def play_merged(self, A, B):
        na, nb = len(A), len(B)
        ia = ib = 0
        while ia < na or ib < nb:
            if ib >= nb or (ia < na and ia * nb <= ib * na):
                a, kw = A[ia]
                ia += 1
            else:
                a, kw = B[ib]
                ib += 1
            self.P.op(*a, **kw)

    def dram(self, name, shape, dt=F32, kind="Internal"):
        if self.debug and kind == "Internal":
            kind = "ExternalOutput"
        t = self.nc.dram_tensor(name, list(shape), dt, kind=kind)
        v = V(t.ap(), name)
        self.dr[name] = v
        return v

    def alloc(self, name, shape, dt=F32):
        p = shape[0]
        n = int(np.prod(shape[1:]))
        words = n if dt == F32 else (n + 1) // 2
        off = self.off
        self.off += words
        assert self.off <= self.AW, f"SBUF arena overflow at {name}: {self.off}"
        ap = self.arena[0:p, off:off + words]
        if dt != F32:
            ap = ap.bitcast(dt)[:, 0:n]
        if len(shape) == 3:
            ap = ap.rearrange("p (a b) -> p a b", a=shape[1], b=shape[2])
        elif len(shape) == 4:
            ap = ap.rearrange("p (a b c) -> p a b c", a=shape[1], b=shape[2], c=shape[3])
        return V(ap, name)

    def phase_end(self):
        self.P.barrier()
        self.off = self.mark

    def mm(self, out, lhsT, rhs, start=True, stop=True):
        self.emit_op('pe', lambda h: h.matmul(out.ap, lhsT=lhsT.ap, rhs=rhs.ap, start=start, stop=stop),
                  reads=[lhsT.key, rhs.key], writes=[out.key], accum=True)

    def tr(self, out, in_):
        n = in_.ap.shape[0]
        idt = self.ident[0:n, 0:n]
        self.emit_op('pe', lambda h: h.transpose(out=out.ap, in_=in_.ap, identity=idt.ap),
                  reads=[in_.key, idt.key], writes=[out.key], accum=True)

    def act(self, out, in_, func, bias=None, scale=None):
        reads = [in_.key]
        kw = {}
        if bias is not None:
            if isinstance(bias, V):
                reads.append(bias.key)
                kw['bias'] = bias.ap
            else:
                kw['bias'] = float(bias)
        if scale is not None:
            if isinstance(scale, V):
                reads.append(scale.key)
                kw['scale'] = scale.ap
            else:
                kw['scale'] = float(scale)
        self.emit_op('act', lambda h: h.activation(out=out.ap, in_=in_.ap, func=func, **kw), reads=reads, writes=[out.key])

    def tt(self, out, a, b, op, eng='dve'):
        self.emit_op(eng, lambda h: h.tensor_tensor(out=out.ap, in0=a.ap, in1=b.ap, op=op), reads=[a.key, b.key],
                  writes=[out.key])

    def ts(self, out, a, s1, op0, s2=None, op1=None, eng='dve'):
        reads = [a.key]
        a1 = s1
        if isinstance(s1, V):
            reads.append(s1.key)
            a1 = s1.ap
        a2 = s2
        if isinstance(s2, V):
            reads.append(s2.key)
            a2 = s2.ap
        if op1 is None:
            self.emit_op(eng, lambda h: h.tensor_scalar(out=out.ap, in0=a.ap, scalar1=a1, scalar2=None, op0=op0),
                      reads=reads, writes=[out.key])
        else:
            self.emit_op(eng, lambda h: h.tensor_scalar(out=out.ap, in0=a.ap, scalar1=a1, scalar2=a2, op0=op0, op1=op1),
                      reads=reads, writes=[out.key])

    def stt(self, out, in0, scalar, in1, op0, op1):
        reads = [in0.key, in1.key]
        sc = scalar
        if isinstance(scalar, V):
            reads.append(scalar.key)
            sc = scalar.ap
        self.emit_op('dve', lambda h: h.scalar_tensor_tensor(out=out.ap, in0=in0.ap, scalar=sc, in1=in1.ap, op0=op0, op1=op1),
                  reads=reads, writes=[out.key])

    def red(self, out, in_, op):
        self.emit_op('dve', lambda h: h.tensor_reduce(out=out.ap, in_=in_.ap, axis=AX.X, op=op), reads=[in_.key],
                  writes=[out.key])

    def cp(self, out, in_, eng='dve'):
        if eng == 'act':
            self.emit_op('act', lambda h: h.activation(out=out.ap, in_=in_.ap, func=AF.Copy), reads=[in_.key], writes=[out.key])
        else:
            self.emit_op(eng, lambda h: h.tensor_copy(out=out.ap, in_=in_.ap), reads=[in_.key], writes=[out.key])

    def memset(self, out, val, eng='dve'):
        self.emit_op(eng, lambda h: h.memset(out.ap, val), writes=[out.key])

    def recip(self, out, in_):
        self.emit_op('dve', lambda h: h.reciprocal(out=out.ap, in_=in_.ap), reads=[in_.key], writes=[out.key])

    def scan(self, out, a, u):
        self.emit_op('dve', lambda h: h.tensor_tensor_scan(out=out.ap, data0=a.ap, data1=u.ap, initial=0.0, op0=ALU.mult,
                                                        op1=ALU.add), reads=[a.key, u.key], writes=[out.key])

    def dma(self, out, in_, eng='sp'):
        self.emit_op(eng, lambda h: h.dma_start(out=out.ap, in_=in_.ap), reads=[in_.key], writes=[out.key], dma=True)

    def rot(self, name, n):
        i = self.rr.get(name, 0)
        self.rr[name] = i + 1
        return i % n

    def evac(self, out, in_, i=None):
        if i is None:
            i = self.rot('evac', 2)
        self.cp(out, in_, eng=('dve' if i % 2 == 0 else 'act'))

    def build(self):
        cfg = self.cfg
        nc = self.nc
        D, SEQ, NT, KC, DEPTH = cfg.D, cfg.SEQ, cfg.NT, cfg.KC, cfg.DEPTH
        NCH = cfg.NCH
        dr = self.dram
        EI, EO = "ExternalInput", "ExternalOutput"
        self.xp = dr("xp", [SEQ, D], kind=EI)
        self.xs = dr("xs", [128, D], kind=EI)
        self.pp = dr("pp", [DEPTH, SEQ, cfg.PD], kind=EI)
        self.psm = dr("psm", [DEPTH, 128, cfg.PD], kind=EI)
        self.sCT = dr("sCT", [DEPTH, 16, 4, 128, 256], kind=EI)
        self.snT = dr("snT", [DEPTH, 4, 128, 16], kind=EI)
        self.sm = dr("sm", [DEPTH, 16, 4], kind=EI)
        self.rhT = dr("rhT", [DEPTH, 1024, 16], kind=EI)
        self.rcT = dr("rcT", [DEPTH, 1024, 16, 3], kind=EI)
        self.gS = dr("gS", [DEPTH, 16, 4, 128, 256], kind=EI)
        self.gcT = dr("gcT", [DEPTH, 2048, 16, 3], kind=EI)
        self.consts = dr("consts", [13, 128, 128], kind=EI)
        self.consts2 = dr("consts2", [128, 32 + 2048], kind=EI)
        W = {}
        W['w_in'] = dr("w_in", [DEPTH, D, cfg.DIN], kind=EI)
        W['gbias'] = dr("gbias", [DEPTH, 16], kind=EI)
        W['gd_A_log'] = dr("gd_A_log", [DEPTH, 4], kind=EI)
        W['ml_norm_w'] = dr("ml_norm_w", [DEPTH, 1024], kind=EI)
        W['rg_conv_wT'] = dr("rg_conv_wT", [DEPTH, 1024, 4], kind=EI)
        W['rg_vecs'] = dr("rg_vecs", [DEPTH, 1024, 4], kind=EI)
        W['rg_w_a'] = dr("rg_w_a", [DEPTH, 4, 256, 256], kind=EI)
        W['rg_w_x'] = dr("rg_w_x", [DEPTH, 4, 256, 256], kind=EI)
        W['gd_conv_wT'] = dr("gd_conv_wT", [DEPTH, 2048, 4], kind=EI)
        W['gd_norm_w'] = dr("gd_norm_w", [DEPTH, 256], kind=EI)
        W['w_up_mlstm'] = dr("w_up_mlstm", [DEPTH, 1024, D], kind=EI)
        W['w_up_rglru'] = dr("w_up_rglru", [DEPTH, 1024, D], kind=EI)
        W['w_up_gdn'] = dr("w_up_gdn", [DEPTH, 1024, D], kind=EI)
        W['w_out'] = dr("w_out", [DEPTH, D, D], kind=EI)
        W['ln'] = dr("ln", [DEPTH, 4, D], kind=EI)
        n_dense = (DEPTH + 1) // 2
        n_moe = DEPTH // 2
        W['ffn_w1'] = dr("ffn_w1", [n_dense, D, cfg.DFF], kind=EI)
        W['ffn_w3'] = dr("ffn_w3", [n_dense, D, cfg.DFF], kind=EI)
        W['ffn_w2'] = dr("ffn_w2", [n_dense, cfg.DFF, D], kind=EI)
        W['moe_router'] = dr("moe_router", [max(n_moe, 1), D, cfg.NE], kind=EI)
        W['moe_w1'] = dr("moe_w1", [max(n_moe, 1), cfg.NE, D, cfg.DFFE], kind=EI)
        W['moe_w3'] = dr("moe_w3", [max(n_moe, 1), cfg.NE, D, cfg.DFFE], kind=EI)
        W['moe_w2'] = dr("moe_w2", [max(n_moe, 1), cfg.NE, cfg.DFFE, D], kind=EI)
        W['ple_w'] = dr("ple_w", [DEPTH, cfg.PD, D], kind=EI)
        W['ple_gate_w'] = dr("ple_gate_w", [DEPTH, D, D], kind=EI)
        self.W = W
        self.yp = dr("yp", [SEQ, D], kind=EO)
        self.ys = dr("ys", [128, D], kind=EO)
        self.o_pCT = dr("o_pCT", [DEPTH, 4, 128, 257], kind=EO)
        self.o_pm = dr("o_pm", [DEPTH, 4], kind=EO)
        self.o_rh = dr("o_rh", [DEPTH, 1024, 17], kind=EO)
        self.o_conv = dr("o_conv", [DEPTH, 2, 128, 3072], kind=EO)
        self.o_pgS = dr("o_pgS", [DEPTH, 4, 128, 256], kind=EO)
        self.o_sCT = dr("o_sCT", [DEPTH, 16, 4, 128, 257], kind=EO)
        self.o_sm = dr("o_sm", [DEPTH, 16, 4], kind=EO)
        self.o_sgS = dr("o_sgS", [DEPTH, 16, 4, 128, 256], kind=EO)
        self.FM = dr("FM", [5120, NT])
        self.MG = dr("MG", [3 * D, NT], BF16)
        self.TM = dr("TM", [NT, 3600])
        self.KV = dr("KV", [NT, 1536])
        self.YA = dr("YA", [1024, NT], BF16)
        self.YB = dr("YB", [1024, NT], BF16)
        self.YC = dr("YC", [1024, NT], BF16)
        self.MT = dr("MT", [D, NT], BF16)
        self.X1 = dr("X1", [NT, D])
        self.XC = dr("XC", [NT, D])
        self.FF = dr("FF", [NT, D])
        self.HT = dr("HT", [max(cfg.DFF, cfg.NE * cfg.DFFE), NT], BF16)
        self.Ascr = dr("Ascr", [NCH * 4, 64, 64])
        self.Tscr = dr("Tscr", [NCH * 4, 64, 64])

        with contextlib.ExitStack() as stack:
            self.arena = stack.enter_context(nc.sbuf_tensor("arena", [128, self.AW], F32))
            self.ps = [V(stack.enter_context(nc.psum_tensor(f"ps{i}", [128, 512], F32))[:, :], f"ps{i}") for i in range(8)]
            cn, _, _ = make_consts()
            self.C = {}
            call = self.alloc("call", [128, 13, 128])
            self.dma(call, self.consts.re("c p n -> p c n"))
            for i, n in enumerate(cn):
                self.C[n] = call[:, i, :]
            self.ident = self.C['ident']
            c2 = self.alloc("c2", [128, 32 + 2048])
            self.dma(c2, self.consts2)
            self.blockind = c2[:, 0:16]
            self.lastind = c2[:, 16:32]
            self.bm3 = c2[:, 32:32 + 2048].re("p (i t) -> p i t", i=16, t=128)
            self.mark = self.off
            for l in range(DEPTH):
                self.layer(l)
            self.P.emit(stack)
        return nc

    def x_src(self, l, tt):
        cfg = self.cfg
        if l == 0:
            if tt < cfg.NTT - 1:
                return self.xp[tt * 128:(tt + 1) * 128, :].k(tt)
            return self.xs[:, :].k(tt)
        return self.XC[tt * 128:(tt + 1) * 128, :].k(tt)

    def x_dst(self, l, tt):
        cfg = self.cfg
        if l == cfg.DEPTH - 1:
            if tt < cfg.NTT - 1:
                return self.yp[tt * 128:(tt + 1) * 128, :].k(tt)
            return self.ys[:, :].k(tt)
        return self.XC[tt * 128:(tt + 1) * 128, :].k(tt)

    def layer(self, l):
        for ph in (self.phaseA, self.phaseRG, self.phaseML, self.phaseGD, self.phaseC1, self.phaseC2, self.phaseFFN,
                   self.phaseF):
            if self.stop_after is not None and self.stopped:
                return
            ph(l)
            self.phase_end()
            if self.stop_after == (l, ph.__name__):
                self.stopped = True

    def make_xT(self, xT, src_fn, want_f32=None):
        cfg = self.cfg
        KC = cfg.KC
        xt = [self.alloc(f"xt_stage{i}", [128, cfg.D]) for i in range(2)]
        for tt in range(cfg.NTT):
            st = xt[tt % 2]
            self.dma(st, src_fn(tt))
            for q in range(KC // 4 if KC >= 4 else 1):
                nk = min(4, KC)
                ps = self.ps[self.rot('xTps', 2)]
                for j in range(nk):
                    k = q * 4 + j
                    self.tr(ps[:, j * 128:(j + 1) * 128], st[:, k * 128:(k + 1) * 128])
                dst = xT[:, q * 4:q * 4 + nk, tt * 128:(tt + 1) * 128].k(tt)
                self.evac(dst, ps[:, 0:nk * 128].re("p (a b) -> p a b", a=nk, b=128))

    def phaseA(self, l):
        cfg = self.cfg
        KC, NT, NTT, D = cfg.KC, cfg.NT, cfg.NTT, cfg.D
        xT = self.alloc("xT", [128, KC, NT], BF16)
        self.make_xT(xT, lambda tt: self.x_src(l, tt))
        wbuf = [self.alloc(f"wA{i}", [128, KC, 512], BF16) for i in range(2)]
        stf = [self.alloc(f"stA{i}", [128, 512]) for i in range(4)]
        stb = [self.alloc(f"stAb{i}", [128, 512], BF16) for i in range(2)]
        wg = self.alloc("wAg", [128, KC, 16], BF16)
        win = self.W['w_in']
        o = cfg.off
        segs = [
            (o['mlq'], 512, [('fm', 0, 'q')]),
            (o['mlk'], 512, [('fm', 512, 'c'), ('tm', 0, 'c')]),
            (o['mlv'], 1024, [('tm', 512, 'c')]),
            (o['mlo'], 1024, [('tm', 1536, 'c')]),
            (o['rgx'], 1024, [('fm', 1024, 'c'), ('cv', 0, 'c')]),
            (o['rgy'], 1024, [('fm', 2048, 'c')]),
            (o['gdqkv'], 2048, [('fm', 3072, 'c'), ('cv', 1024, 'c')]),
            (o['gdz'], 1024, [('tm', 2560, 'c')]),
            (o['mg'], 3 * D, [('mg', 0, 's')]),
        ]
        xTr = lambda t0, n: [("xT", tt) for tt in range(t0 // 128, (t0 + n + 127) // 128)]

        def fm_block(wt, sub, row0, kind, dst):
            for (t0, n) in cfg.groups:
                ps = self.ps[2 + self.rot('Aps', 6)]
                for k in range(KC):
                    self.emit_op('pe', lambda h, ps=ps, k=k, t0=t0, n=n: h.matmul(
                        ps.ap[:, 0:n], lhsT=wt.ap[:, k, sub * 128:(sub + 1) * 128], rhs=xT.ap[:, k, t0:t0 + n],
                        start=(k == 0), stop=(k == KC - 1)), reads=[wt.key] + xTr(t0, n), writes=[ps.key], accum=True)
                if kind == 's':
                    st = stb[self.rot('stb', 2)]
                    self.act(st[:, 0:n], ps[:, 0:n], AF.Sigmoid)
                elif kind == 'q':
                    st = stf[self.rot('stf', 4)]
                    self.act(st[:, 0:n], ps[:, 0:n], AF.Copy, scale=cfg.DK ** -0.5)
                else:
                    st = stf[self.rot('stf', 4)]
                    self.evac(st[:, 0:n], ps[:, 0:n])
                self.dma(dst[row0:row0 + 128, t0:t0 + n].k((row0, t0)), st[:, 0:n])

        def tm_block(wt, nc_, col0, dst, tiles, dst_rows=None):
            for ti, tt in enumerate(tiles):
                ps = self.ps[2 + self.rot('Aps', 6)]
                for k in range(KC):
                    self.emit_op('pe', lambda h, ps=ps, k=k, tt=tt: h.matmul(
                        ps.ap[:, 0:nc_], lhsT=xT.ap[:, k, tt * 128:(tt + 1) * 128], rhs=wt.ap[:, k, 0:nc_],
                        start=(k == 0), stop=(k == KC - 1)), reads=[wt.key, ("xT", tt)], writes=[ps.key], accum=True)
                st = stf[self.rot('stf', 4)]
                self.evac(st[:, 0:nc_], ps[:, 0:nc_])
                if dst_rows is None:
                    self.dma(dst[tt * 128:(tt + 1) * 128, col0:col0 + nc_].k((tt, col0)), st[:, 0:nc_])
                else:
                    self.dma(dst_rows(ti)[:, col0:col0 + nc_].k((ti, col0)), st[:, 0:nc_])

        for (c0, ncols, outs) in segs:
            for b0 in range(0, ncols, 512):
                nb = min(512, ncols - b0)
                wt = wbuf[self.rot('wA', 2)]
                self.dma(wt[:, :, 0:nb], win[l, :, c0 + b0:c0 + b0 + nb].re("(k p) n -> p k n", p=128), eng='pool')
                for (mode, base, kind) in outs:
                    if mode == 'fm':
                        for sub in range(nb // 128):
                            fm_block(wt, sub, base + b0 + sub * 128, kind, self.FM)
                    elif mode == 'mg':
                        for sub in range(nb // 128):
                            fm_block(wt, sub, base + b0 + sub * 128, kind, self.MG)
                    elif mode == 'tm':
                        tm_block(wt, nb, base + b0, self.TM, list(range(NTT)))
                    elif mode == 'cv':
                        tm_block(wt, nb, base + b0, None, [NTT - 2, NTT - 1],
                                 dst_rows=lambda ti: self.o_conv[l, ti, :, :])
        self.dma(wg[:, :, 0:8], win[l, :, o['mli']:o['mli'] + 8].re("(k p) n -> p k n", p=128), eng='pool')
        self.dma(wg[:, :, 8:16], win[l, :, o['gdb']:o['gdb'] + 8].re("(k p) n -> p k n", p=128), eng='pool')
        tm_block(wg, 16, 3584, self.TM, list(range(NTT)))

    def conv_fm(self, raw_rows, bufT, cw, out, bias, xpad_p, xpad_s):
        cfg = self.cfg
        SEQ = cfg.SEQ
        self.memset(xpad_p[:, 0:3], 0.0)
        self.dma(xpad_p[:, 3:3 + SEQ], raw_rows[:, 0:SEQ])
        self.dma(xpad_s[:, :, 3:11], raw_rows[:, SEQ:SEQ + 128].re("p (i t) -> p i t", i=16, t=8))
        self.dma(xpad_s[:, :, 0:3], bufT)
        op = out[:, 0:SEQ]
        os_ = out[:, SEQ:SEQ + 128].re("p (i t) -> p i t", i=16, t=8)
        for (o_, xp_, sl) in ((op, xpad_p, lambda j: xpad_p[:, j:j + SEQ]), (os_, xpad_s, lambda j: xpad_s[:, :, j:j + 8])):
            if bias is None:
                self.ts(o_, sl(0), cw[:, 0:1], ALU.mult)
            else:
                self.ts(o_, sl(0), cw[:, 0:1], ALU.mult, bias, ALU.add)
            for j in range(1, 4):
                self.stt(o_, sl(j), cw[:, j:j + 1], o_, ALU.mult, ALU.add)

    def phaseRG(self, l):
        cfg = self.cfg
        NT, SEQ = cfg.NT, cfg.SEQ
        W = self.W
        cw = self.alloc("rg_cw", [128, 8, 4])
        self.dma(cw, W['rg_conv_wT'][l].re("(c p) j -> p c j", p=128))
        vec = self.alloc("rg_vec", [128, 8, 4])
        self.dma(vec, W['rg_vecs'][l].re("(c p) j -> p c j", p=128))
        h0 = self.alloc("rg_h0", [128, 8, 16])
        self.dma(h0, self.rhT[l].re("(c p) i -> p c i", p=128))
        hl = self.alloc("rg_hl", [128, 8, 17])
        nl = self.alloc("rg_nl", [128, 8])
        t1 = self.alloc("rg_t1", [128, 8])
        t2 = self.alloc("rg_t2", [128, 8])
        sp8 = self.alloc("rg_sp8", [128, 8])
        self.ts(nl, vec[:, :, 3], -1.0, ALU.mult)
        self.ts(t1, nl, -1.0, ALU.mult)
        self.tt(t1, t1, nl, ALU.max)
        self.act(t1, t1, AF.Exp, scale=-1.0)
        self.act(t1, t1, AF.Ln, bias=1.0)
        self.ts(t2, nl, 0.0, ALU.max)
        self.tt(t1, t1, t2, ALU.add)
        self.ts(sp8, t1, -8.0, ALU.mult)
        xpad_p = [self.alloc(f"rg_xp{i}", [128, SEQ + 3]) for i in range(2)]
        xpad_s = [self.alloc(f"rg_xs{i}", [128, 16, 11]) for i in range(2)]
        xc = [self.alloc(f"rg_xc{i}", [128, NT]) for i in range(2)]
        wa = self.alloc("rg_wa", [128, 2, 256])
        wx = self.alloc("rg_wx", [128, 2, 256])
        r = self.alloc("rg_r", [128, NT])
        ig = self.alloc("rg_i", [128, NT])
        a = self.alloc("rg_a", [128, NT])
        u = self.alloc("rg_u", [128, NT])
        hh = self.alloc("rg_h", [128, NT])
        gy = self.alloc("rg_gy", [128, NT])
        g2 = self.alloc("rg_g2", [128, NT])
        yb = self.alloc("rg_yb", [128, NT], BF16)
        t16 = self.alloc("rg_t16", [128, 16])
        for n in range(4):
            for c in range(2):
                ch = n * 2 + c
                self.conv_fm(self.FM[1024 + ch * 128:1024 + (ch + 1) * 128, :], self.rcT[l, ch * 128:(ch + 1) * 128, :, :],
                             cw[:, ch, :], xc[c], vec[:, ch, 0:1], xpad_p[c], xpad_s[c])
            self.dma(wa, W['rg_w_a'][l, n].re("(c p) e -> p c e", p=128))
            self.dma(wx, W['rg_w_x'][l, n].re("(c p) e -> p c e", p=128))
            for e in range(2):
                ch = n * 2 + e
                for (wt, dst, bcol) in ((wa, r, 1), (wx, ig, 2)):
                    for (t0, nn) in cfg.groups:
                        ps = self.ps[self.rot('rgps', 4)]
                        for c in range(2):
                            self.mm(ps[:, 0:nn], wt[:, c, e * 128:(e + 1) * 128], xc[c][:, t0:t0 + nn], start=(c == 0), stop=(c == 1))
                        self.act(dst[:, t0:t0 + nn], ps[:, 0:nn], AF.Sigmoid, bias=vec[:, ch, bcol:bcol + 1])
                self.act(a, r, AF.Exp, scale=sp8[:, ch:ch + 1])
                self.tt(u, a, a, ALU.mult)
                self.act(u, u, AF.Sqrt, bias=1.0, scale=-1.0)
                self.tt(ig, ig, xc[e], ALU.mult)
                self.tt(u, u, ig, ALU.mult)
                a_s = a[:, SEQ:SEQ + 128].re("p (i t) -> p i t", i=16, t=8)
                u_s = u[:, SEQ:SEQ + 128].re("p (i t) -> p i t", i=16, t=8)
                self.tt(t16, a_s[:, :, 0], h0[:, ch, :], ALU.mult)
                self.tt(u_s[:, :, 0], u_s[:, :, 0], t16, ALU.add)
                self.memset(a_s[:, :, 0], 0.0)
                self.scan(hh, a, u)
                h_s = hh[:, SEQ:SEQ + 128].re("p (i t) -> p i t", i=16, t=8)
                self.cp(hl[:, ch, 0:1], hh[:, SEQ - 1:SEQ])
                self.cp(hl[:, ch, 1:17], h_s[:, :, 7])
                self.dma(gy, self.FM[2048 + ch * 128:2048 + (ch + 1) * 128, :])
                self.tt(g2, gy, gy, ALU.mult)
                self.ts(g2, g2, 0.044715, ALU.mult, 1.0, ALU.add)
                self.tt(g2, g2, gy, ALU.mult)
                self.act(g2, g2, AF.Sigmoid, scale=1.5957691216057308)
                self.tt(g2, g2, gy, ALU.mult)
                self.tt(yb, g2, hh, ALU.mult)
                self.dma(self.YB[ch * 128:(ch + 1) * 128, :].k(ch), yb)
        self.dma(self.o_rh[l].re("(c p) i -> p c i", p=128), hl)

    def gates(self, l, pre):
        cfg = self.cfg
        NCH, SEQ = cfg.NCH, cfg.SEQ
        gb = self.alloc(pre + "gb", [128, 16])
        self.dma(gb, V(self.W['gbias'].ap[l, :].partition_broadcast(128), 'gbias'))
        nA = self.alloc(pre + "nA", [128, 4])
        self.dma(nA, V(self.W['gd_A_log'].ap[l, :].partition_broadcast(128), 'gd_A_log'))
        self.act(nA, nA, AF.Exp)
        self.ts(nA, nA, -1.0, ALU.mult)
        out = {}
        for nm, L, G, src in (('p', 64, NCH, self.TM[0:SEQ, 3584:3600].re("(c p) g -> p c g", p=64)),
                              ('s', 128, 1, self.TM[SEQ:SEQ + 128, 3584:3600].re("(c p) g -> p c g", p=128))):
            x = self.alloc(pre + "gx" + nm, [L, G, 16])
            self.dma(x, src)
            self.tt(x, x, gb[0:L, :].un(1).bc([L, G, 16]), ALU.add)
            t = self.alloc(pre + "gt" + nm, [L, G, 16])
            self.act(t[:, :, 0:8], x[:, :, 0:8], AF.Tanh, scale=1.0 / 15.0)
            self.ts(t[:, :, 0:8], t[:, :, 0:8], 15.0, ALU.mult)
            self.act(t[:, :, 4:8], t[:, :, 4:8], AF.Exp, scale=-1.0)
            self.act(t[:, :, 4:8], t[:, :, 4:8], AF.Ln, bias=1.0)
            self.ts(t[:, :, 4:8], t[:, :, 4:8], -1.0, ALU.mult)
            self.act(t[:, :, 8:12], x[:, :, 8:12], AF.Sigmoid)
            self.ts(t[:, :, 12:16], x[:, :, 12:16], -1.0, ALU.mult)
            self.tt(t[:, :, 12:16], t[:, :, 12:16], x[:, :, 12:16], ALU.max)
            self.act(t[:, :, 12:16], t[:, :, 12:16], AF.Exp, scale=-1.0)
            self.act(t[:, :, 12:16], t[:, :, 12:16], AF.Ln, bias=1.0)
            self.ts(x[:, :, 12:16], x[:, :, 12:16], 0.0, ALU.max)
            self.tt(t[:, :, 12:16], t[:, :, 12:16], x[:, :, 12:16], ALU.add)
            self.tt(t[:, :, 12:16], t[:, :, 12:16], nA[0:L, :].un(1).bc([L, G, 4]), ALU.mult)
            out[nm] = t
        return out

    def masks(self, mode):
        s = 'P' if mode == 'p' else 'S'
        C = self.C
        return dict(tri=C['tri' + s], negT=C['negT' + s], neg=C['neg' + s], strictT=C['strictT' + s], last=C['last' + s])

    def rowbc(self, ps, col, L, tmp):
        self.cp(tmp[0:L, :, 0:L], col.un(2).bc([L, 4, L]))
        for h in range(4):
            self.mm(ps[0:L, h * L:(h + 1) * L], tmp[0:L, h, 0:L], self.ident[0:L, 0:L])

    def phaseML(self, l):
        cfg = self.cfg
        NT, SEQ, NCH = cfg.NT, cfg.SEQ, cfg.NCH
        al = self.alloc
        qT = al("ml_qT", [128, 4, NT])
        kT = al("ml_kT", [128, 4, NT])
        self.dma(qT, self.FM[0:512, :].re("(h d) t -> d h t", h=4, d=128))
        self.dma(kT, self.FM[512:1024, :].re("(h d) t -> d h t", h=4, d=128))
        G = self.gates(l, "ml_")
        nw = al("ml_nw", [128, 1024])
        self.dma(nw, V(self.W['ml_norm_w'].ap[l, :].partition_broadcast(128), 'ml_norm_w'))
        yaT = [al(f"ml_yaT{i}", [128, 8, 128], BF16) for i in range(2)]
        CT = al("ml_CT", [128, 4, 257])
        self.memset(CT, 0.0)
        m0 = al("ml_m0", [128, 4])
        self.memset(m0, 0.0)
        ktm = al("ml_ktm", [128, 4, 128])
        vaug = al("ml_vaug", [128, 4, 257])
        self.memset(vaug[:, :, 256:257], 1.0)
        otm = al("ml_otm", [128, 1024])
        bc_ = al("ml_bc", [128, 4])
        Bv = al("ml_B", [128, 4])
        big = al("ml_big", [128, 4, 128])
        Rm = al("ml_Rm", [128, 4, 128])
        DT = al("ml_DT", [128, 4, 128])
        ST = al("ml_ST", [128, 4, 128])
        cm = al("ml_cm", [128, 4])
        X12 = al("ml_X12", [128, 12])
        inter = al("ml_inter", [128, 4])
        enm = al("ml_enm", [128, 4])
        lb = al("ml_lb", [128, 12])
        tmpc = al("ml_tmpc", [128, 257])
        nd = al("ml_nd", [128, 4, 257])
        dd = al("ml_dd", [128, 4])
        hh = al("ml_hh", [128, 4, 256])
        sq = al("ml_sq", [128, 4, 256])
        ss = al("ml_ss", [128, 4])
        wsig = al("ml_wsig", [128, 1024])
        ya = al("ml_ya", [128, 1024])
        wend = al("ml_wend", [128, 4])
        kw = al("ml_kw", [128, 4, 128])
        dec = al("ml_dec", [128, 4])
        CTs = al("ml_CTs", [128, 16, 257])
        CTn = al("ml_CTn", [128, 16, 257])
        qTz = al("ml_qTz", [128, 16, 128])
        kwz = al("ml_kwz", [128, 16, 128])
        Wm = al("ml_Wm", [128, 4, 16])
        X64 = al("ml_X64", [128, 4, 16])
        decb = al("ml_decb", [128, 4, 16])
        sm_sb = al("ml_sm", [16, 4])
        sn_sb = al("ml_snsb", [128, 16])
        mo = al("ml_mo", [16, 4])
        ps = self.ps

        def tile(mode, t0, L, ip, lf, part):
            M = self.masks(mode)
            rows = slice(t0, t0 + L)
            if part == 'head':
                self.dma(ktm[0:L], self.TM[rows, 0:512].re("p (h d) -> p h d", h=4, d=128))
                self.dma(vaug[0:L, :, 0:256], self.TM[rows, 512:1536].re("p (h e) -> p h e", h=4, e=256))
                self.dma(otm[0:L], self.TM[rows, 1536:2560])
                self.mm(ps[0][0:L, 0:4], M['tri'][0:L, 0:L], lf)
                self.cp(bc_[0:L], ps[0][0:L, 0:4])
                self.tt(Bv[0:L], ip, bc_[0:L], ALU.subtract)
                self.rowbc(ps[1], Bv[0:L], L, big)
                self.tt(Rm[0:L, :, 0:L], ps[1][0:L, 0:4 * L].re("p (h s) -> p h s", h=4, s=L),
                        M['neg'][0:L, 0:L].un(1).bc([L, 4, L]), ALU.add)
                self.red(cm[0:L], Rm[0:L, :, 0:L], ALU.max)
                self.tt(cm[0:L], cm[0:L], m0[0:L], ALU.max)
                self.ts(X12[0:L, 0:4], cm[0:L], -1.0, ALU.mult)
                self.tt(X12[0:L, 4:8], m0[0:L], cm[0:L], ALU.subtract)
                self.tt(X12[0:L, 8:12], bc_[0:L], cm[0:L], ALU.add)
                self.act(inter[0:L], X12[0:L, 4:8], AF.Exp)
                self.act(enm[0:L], X12[0:L, 8:12], AF.Exp, scale=-1.0)
                self.mm(ps[0][:, 16:28], M['last'][0:L, :], X12[0:L, :])
                self.cp(lb, ps[0][:, 16:28])
                self.rowbc(ps[1], X12[0:L, 0:4], L, big)
                self.tt(Rm[0:L, :, 0:L], ps[1][0:L, 0:4 * L].re("p (h s) -> p h s", h=4, s=L),
                        M['negT'][0:L, 0:L].un(1).bc([L, 4, L]), ALU.add)
                for h in range(4):
                    self.act(DT[0:L, h, 0:L], Rm[0:L, h, 0:L], AF.Exp, bias=Bv[0:L, h:h + 1])
                for h in range(4):
                    self.mm(ps[3][0:L, h * L:(h + 1) * L], kT[:, h, rows], qT[:, h, rows])
                self.tt(ST[0:L, :, 0:L], ps[3][0:L, 0:4 * L].re("p (h s) -> p h s", h=4, s=L), DT[0:L, :, 0:L], ALU.mult)
                for h in range(4):
                    pn = ps[4 + (h % 2) * 2]
                    pc = ps[5 + (h % 2) * 2]
                    self.mm(pn[0:L, 0:257], ST[0:L, h, 0:L], vaug[0:L, h, :])
                    if mode == 'p':
                        self.mm(pc[0:L, 0:257], qT[:, h, rows], CT[:, h, :])
                    else:
                        self.dma(CTs[:, :, 0:256], self.sCT[l, :, h, :, :].re("i d e -> d i e"))
                        self.dma(sn_sb, self.snT[l, h, :, :])
                        self.cp(CTs[:, :, 256], sn_sb)
                        self.tt(qTz, qT[:, h, rows].un(1).bc([128, 16, 128]), self.bm3, ALU.mult)
                        for i in range(16):
                            self.mm(pc[0:L, 0:257], qTz[:, i, :], CTs[:, i, :], start=(i == 0), stop=(i == 15))
                    self.act(tmpc[0:L], pc[0:L, 0:257], AF.Copy, scale=inter[0:L, h:h + 1])
                    self.tt(nd[0:L, h, :], pn[0:L, 0:257], tmpc[0:L], ALU.add)
                    if mode == 's':
                        if h == 0:
                            self.tt(Bv[0:L], Bv[0:L], lb[0:L, 0:4], ALU.add)
                            self.act(wend[0:L], Bv[0:L], AF.Exp)
                            self.tt(Wm, wend.un(2).bc([128, 4, 16]), self.blockind.un(1).bc([128, 4, 16]), ALU.mult)
                            self.tt(X64, inter.un(2).bc([128, 4, 16]), self.lastind.un(1).bc([128, 4, 16]), ALU.mult)
                            self.mm(ps[0][:, 64:128], self.C['ones'], X64.re("p h i -> p (h i)"))
                            self.cp(decb.re("p h i -> p (h i)"), ps[0][:, 64:128])
                        self.tt(kwz, ktm[:, h, :].un(1).bc([128, 16, 128]), Wm[:, h, :].un(2).bc([128, 16, 128]), ALU.mult)
                        for i in range(16):
                            pu = ps[self.rot('mlpu', 2)]
                            self.mm(pu[:, 0:257], kwz[:, i, :], vaug[:, h, :])
                            self.stt(CTn[:, i, :], CTs[:, i, :], decb[:, h, i:i + 1], pu[:, 0:257], ALU.mult, ALU.add)
                        self.dma(self.o_sCT[l, :, h, :, :].re("i d e -> d i e").k(h), CTn)
                if mode == 'p':
                    self.tt(Bv[0:L], Bv[0:L], lb[0:L, 0:4], ALU.add)
                    self.act(wend[0:L], Bv[0:L], AF.Exp)
                    self.tt(kw[0:L], ktm[0:L], wend[0:L].un(2).bc([L, 4, 128]), ALU.mult)
                    self.act(dec, lb[:, 4:8], AF.Exp)
                    for h in range(4):
                        pu = ps[self.rot('mlpu', 2)]
                        self.mm(pu[:, 0:257], kw[0:L, h, :], vaug[0:L, h, :])
                        self.stt(CT[:, h, :], CT[:, h, :], dec[:, h:h + 1], pu[:, 0:257], ALU.mult, ALU.add)
                    self.cp(m0, lb[:, 8:12])
                else:
                    self.mm(ps[0][0:16, 32:36], self.lastind, X12[:, 8:12])
                    self.cp(mo, ps[0][0:16, 32:36])
                    self.dma(self.o_sm[l], mo)

            elif part == 'tailpro':
                self.ts(dd[0:L], nd[0:L, :, 256], -1.0, ALU.mult)
                self.tt(dd[0:L], dd[0:L], nd[0:L, :, 256], ALU.max)
                self.tt(dd[0:L], dd[0:L], enm[0:L], ALU.max)
                self.recip(dd[0:L], dd[0:L])
                self.tt(hh[0:L], nd[0:L, :, 0:256], dd[0:L].un(2).bc([L, 4, 256]), ALU.mult)
                self.act(wsig[0:L], otm[0:L], AF.Sigmoid)
                self.tt(wsig[0:L], wsig[0:L], nw[0:L], ALU.mult)
            else:
                self.tt(sq[0:L], hh[0:L], hh[0:L], ALU.mult)
                self.red(ss[0:L], sq[0:L], ALU.add)
                self.ts(ss[0:L], ss[0:L], 1.0 / 256.0, ALU.mult, 1e-6, ALU.add)
                self.act(ss[0:L], ss[0:L], AF.Sqrt)
                self.recip(ss[0:L], ss[0:L])
                yav = ya[0:L].re("p (h e) -> p h e", h=4, e=256)
                self.tt(yav, hh[0:L], ss[0:L].un(2).bc([L, 4, 256]), ALU.mult)
                self.tt(ya[0:L], ya[0:L], wsig[0:L], ALU.mult)
                for j in range(8):
                    pt = (ps[2] if L == 64 else ps[0]) if (j * L) < 512 else ps[1]
                    c0 = (j * L) % 512
                    self.tr(pt[:, c0:c0 + L], ya[0:L, j * 128:(j + 1) * 128])
                yst = yaT[self.rot('ml_yst', 2)]
                if L == 64:
                    self.cp(yst[:, :, 0:64], ps[2][:, 0:512].re("p (j t) -> p j t", j=8, t=64), eng='act')
                else:
                    self.cp(yst[:, 0:4, :], ps[0][:, 0:512].re("p (j t) -> p j t", j=4, t=128), eng='act')
                    self.cp(yst[:, 4:8, :], ps[1][:, 0:512].re("p (j t) -> p j t", j=4, t=128), eng='act')
                self.dma(self.YA[:, rows].re("(j p) t -> p j t", p=128).k(t0), yst[:, :, 0:L])
        args = lambda c: ('p', c * 64, 64, G['p'][:, c, 0:4], G['p'][:, c, 4:8])
        tile(*args(0), 'head')
        for c in range(NCH):
            tile(*args(c), 'tailpro')
            tl = self.record(tile, *args(c), 'tail')
            hd_ = self.record(tile, *args(c + 1), 'head') if c + 1 < NCH else []
            self.play_merged(hd_, tl)
        self.dma(self.o_pCT[l].re("h d e -> d h e"), CT)
        self.dma(self.o_pm[l:l + 1, :], m0[0:1, :])
        self.dma(sm_sb, self.sm[l])
        self.mm(ps[0][:, 40:44], self.C['blockindT'][0:16, :], sm_sb)
        self.cp(m0, ps[0][:, 40:44])
        for part in ('head', 'tailpro', 'tail'):
            tile('s', SEQ, 128, G['s'][:, 0, 0:4], G['s'][:, 0, 4:8], part)

    def phaseGD(self, l):
        cfg = self.cfg
        NT, SEQ, NCH, NTT = cfg.NT, cfg.SEQ, cfg.NCH, cfg.NTT
        al = self.alloc
        W = self.W
        ps = self.ps
        qT = al("gd_qT", [128, 4, NT])
        kT = al("gd_kT", [128, 4, NT])
        cw = al("gd_cw", [128, 16, 4])
        self.dma(cw, W['gd_conv_wT'][l].re("(c p) j -> p c j", p=128))
        rn_p = al("gd_rnp", [64, NCH, 8])
        rn_s = al("gd_rns", [128, 1, 8])
        mark0 = self.off
        xpad_p = al("gd_xp", [128, SEQ + 3])
        xpad_s = al("gd_xs", [128, 16, 11])
        cv = al("gd_cv", [128, NT])
        sqb = al("gd_sqb", [128, NT])
        stg = [al(f"gd_stg{i}", [128, 128]) for i in range(3)]
        for ch in range(16):
            if ch < 4:
                dst = qT[:, ch, :]
            elif ch < 8:
                dst = kT[:, ch - 4, :]
            else:
                dst = cv
            self.conv_fm(self.FM[3072 + ch * 128:3072 + (ch + 1) * 128, :], self.gcT[l, ch * 128:(ch + 1) * 128, :, :],
                         cw[:, ch, :], cv, None, xpad_p, xpad_s)
            self.act(dst, cv, AF.Silu)
            if ch < 8:
                self.act(sqb, dst, AF.Square)
                for c in range(NCH):
                    self.mm(ps[0][0:64, c * 8 + ch:c * 8 + ch + 1], sqb[:, c * 64:(c + 1) * 64], self.C['ones'][:, 0:1])
                self.mm(ps[1][:, ch:ch + 1], sqb[:, SEQ:SEQ + 128], self.C['ones'][:, 0:1])
            if ch >= 4:
                for tt in range(NTT):
                    pt = ps[2 + self.rot('gdtp', 4)]
                    self.tr(pt[:, 0:128], dst[:, tt * 128:(tt + 1) * 128])
                    st = stg[self.rot('gdstg', 3)]
                    self.evac(st, pt[:, 0:128])
                    self.dma(self.KV[tt * 128:(tt + 1) * 128, (ch - 4) * 128:(ch - 3) * 128].k((tt, ch)), st)
        self.ts(rn_p.re("p c j -> p (c j)"), ps[0][0:64, 0:NCH * 8], 1e-6, ALU.add)
        self.act(rn_p, rn_p, AF.Sqrt)
        self.recip(rn_p, rn_p)
        self.ts(rn_s.re("p c j -> p (c j)"), ps[1][:, 0:8], 1e-6, ALU.add)
        self.act(rn_s, rn_s, AF.Sqrt)
        self.recip(rn_s, rn_s)
        self.P.barrier()
        self.off = mark0
        G = self.gates(l, "gd_")
        gnw = al("gd_gnw", [128, 256])
        self.dma(gnw, V(W['gd_norm_w'].ap[l, :].partition_broadcast(128), 'gd_norm_w'))
        ycT = [al(f"gd_ycT{i}", [128, 8, 128], BF16) for i in range(2)]
        S = al("gd_S", [128, 4, 256])
        self.memset(S, 0.0)
        ktm = al("gd_ktm", [128, 4, 128])
        vtm = al("gd_vtm", [128, 4, 256])
        ztm = al("gd_ztm", [128, 1024])
        gc = al("gd_gc", [128, 4])
        ngc = al("gd_ngc", [128, 4])
        eg = al("gd_eg", [128, 4])
        gl = al("gd_gl", [128, 4])
        egl = al("gd_egl", [128, 4])
        kds = al("gd_kds", [128, 4])
        big = al("gd_big", [128, 4, 128])
        Rm = al("gd_Rm", [128, 4, 128])
        DT = al("gd_DT", [128, 4, 128])
        QKT = al("gd_QKT", [128, 4, 128])
        KKD = al("gd_KKD", [128, 4, 128])
        Am = al("gd_Am", [128, 4, 128])
        TT = al("gd_TT", [128, 4, 128])
        sc4 = al("gd_sc4", [128, 4])
        bv = al("gd_bv", [128, 4, 256])
        bk = al("gd_bk", [128, 4, 128])
        upre = al("gd_upre", [128, 256])
        wT = al("gd_wT", [128, 4, 128])
        u = al("gd_u", [128, 4, 256])
        tmpo = al("gd_tmpo", [128, 256])
        o_ = al("gd_o", [128, 4, 256])
        qs = al("gd_qs", [128, 4])
        egq = al("gd_egq", [128, 4])
        kdec = al("gd_kdec", [128, 4, 128])
        ss = al("gd_ss", [128, 4])
        yc = al("gd_yc", [128, 1024])
        wz = al("gd_wz", [128, 1024])

        def common(mode, t0, L, beta, g, rnq, rnk, need_A):
            M = self.masks(mode)
            rows = slice(t0, t0 + L)
            self.mm(ps[0][0:L, 0:4], M['tri'][0:L, 0:L], g)
            self.cp(gc[0:L], ps[0][0:L, 0:4])
            self.ts(ngc[0:L], gc[0:L], -1.0, ALU.mult)
            self.rowbc(ps[1], gc[0:L], L, big)
            self.tt(Rm[0:L, :, 0:L], ps[1][0:L, 0:4 * L].re("p (h s) -> p h s", h=4, s=L),
                    M['negT'][0:L, 0:L].un(1).bc([L, 4, L]), ALU.add)
            for h in range(4):
                self.act(DT[0:L, h, 0:L], Rm[0:L, h, 0:L], AF.Exp, bias=ngc[0:L, h:h + 1])
            if need_A:
                for h in range(4):
                    self.mm(ps[2][0:L, h * L:(h + 1) * L], kT[:, h, rows], kT[:, h, rows])
                self.tt(KKD[0:L, :, 0:L], ps[2][0:L, 0:4 * L].re("p (h s) -> p h s", h=4, s=L), DT[0:L, :, 0:L], ALU.mult)
                self.tt(KKD[0:L, :, 0:L], KKD[0:L, :, 0:L], M['strictT'][0:L, 0:L].un(1).bc([L, 4, L]), ALU.mult)
                self.tt(KKD[0:L, :, 0:L], KKD[0:L, :, 0:L], rnk.un(2).bc([L, 4, L]), ALU.mult)
                for h in range(4):
                    self.tr(ps[3][0:L, h * L:(h + 1) * L], KKD[0:L, h, 0:L])
                self.tt(sc4[0:L], beta, rnk, ALU.mult)
                self.tt(Am[0:L, :, 0:L], ps[3][0:L, 0:4 * L].re("p (h s) -> p h s", h=4, s=L),
                        sc4[0:L].un(2).bc([L, 4, L]), ALU.mult)

        for c in range(NCH):
            common('p', c * 64, 64, G['p'][:, c, 8:12], G['p'][:, c, 12:16], rn_p[:, c, 0:4], rn_p[:, c, 4:8], True)
            self.dma(self.Ascr[c * 4:(c + 1) * 4, :, :].re("h t s -> t h s").k(c), Am[0:64, :, 0:64])
        NP = NCH * 4
        mark1 = self.off
        Ab = al("gd_Ab", [NP, 64, 64])
        Tt = al("gd_Tt", [NP, 64, 64])
        prod = al("gd_prod", [NP, 32, 64])
        rr = al("gd_rr", [NP, 64])
        self.emit_op('sp', lambda h: h.dma_start(out=Ab.ap, in_=self.Ascr.ap), reads=[("Ascr", c) for c in range(NCH)],
                  writes=[Ab.key], dma=True)
        self.memset(Tt, 0.0)
        self.memset(Tt[:, 0:1, 0], 1.0)
        for t in range(1, 64):
            for jh in range(2):
                js = slice(jh * 32, jh * 32 + 32)
                self.tt(prod[:, :, 0:t], Tt[:, js, 0:t], Ab[:, t, 0:t].un(1).bc([NP, 32, t]), ALU.mult)
                self.red(rr[:, js], prod[:, :, 0:t], ALU.add)
            self.ts(Tt[:, :, t], rr, -1.0, ALU.mult)
            self.ts(Tt[:, t:t + 1, t], Tt[:, t:t + 1, t], 1.0, ALU.add)
        self.dma(V(self.Tscr.ap, 'Tscr_all'), Tt)
        self.P.barrier()
        self.off = mark1
        Ss = al("gd_Ss", [128, 16, 256])
        qTz = al("gd_qTz", [128, 16, 128])
        wTz = qTz
        kdz = al("gd_kdz", [128, 16, 128])
        Nm = al("gd_Nm", [128, 4, 128])
        Mm = al("gd_Mm", [128, 4, 128])
        N2 = al("gd_N2", [128, 4, 128])
        M2 = al("gd_M2", [128, 4, 128])
        N4 = al("gd_N4", [128, 4, 128])
        Q1 = al("gd_Q1", [128, 4, 128])
        X64 = al("gd_X64", [128, 4, 16])
        eglb = al("gd_eglb", [128, 4, 16])
        idb = self.ident.un(1).bc([128, 4, 128])

        def pass2(mode, t0, L, beta, g, rnq, rnk, c):
            M = self.masks(mode)
            rows = slice(t0, t0 + L)
            common(mode, t0, L, beta, g, rnq, rnk, mode == 's')
            self.dma(ktm[0:L], self.KV[rows, 0:512].re("p (h d) -> p h d", h=4, d=128))
            self.dma(vtm[0:L], self.KV[rows, 512:1536].re("p (h e) -> p h e", h=4, e=256))
            self.dma(ztm[0:L], self.TM[rows, 2560:3584])
            if mode == 'p':
                self.dma(TT[0:64, :, 0:64], V(self.Tscr.ap[c * 4:(c + 1) * 4, :, :].rearrange("h s t -> s h t"), 'Tscr_all'))
            else:
                self.ts(Nm, Am, -1.0, ALU.mult)
                for h in range(4):
                    self.tr(ps[2][:, h * 128:(h + 1) * 128], Nm[:, h, :])
                self.cp(Mm, ps[2][:, :].re("p (h s) -> p h s", h=4, s=128))
                for h in range(4):
                    self.mm(ps[3][:, h * 128:(h + 1) * 128], Mm[:, h, :], Nm[:, h, :])
                    self.mm(ps[4][:, h * 128:(h + 1) * 128], Nm[:, h, :], Mm[:, h, :])
                self.cp(N2, ps[3][:, :].re("p (h s) -> p h s", h=4, s=128))
                self.cp(M2, ps[4][:, :].re("p (h s) -> p h s", h=4, s=128), eng='act')
                for h in range(4):
                    self.mm(ps[5][:, h * 128:(h + 1) * 128], M2[:, h, :], N2[:, h, :])
                self.tt(N4, ps[5][:, :].re("p (h s) -> p h s", h=4, s=128), idb, ALU.add)
                self.tt(N2, N2, idb, ALU.add)
                self.tt(Mm, Mm, idb, ALU.add)
                for h in range(4):
                    self.mm(ps[2][:, h * 128:(h + 1) * 128], N2[:, h, :], Mm[:, h, :])
                self.cp(Q1, ps[2][:, :].re("p (h s) -> p h s", h=4, s=128))
                for h in range(4):
                    self.mm(ps[3][:, h * 128:(h + 1) * 128], N4[:, h, :], Q1[:, h, :])
                self.cp(TT, ps[3][:, :].re("p (h s) -> p h s", h=4, s=128))
            for h in range(4):
                self.mm(ps[4][0:L, h * L:(h + 1) * L], kT[:, h, rows], qT[:, h, rows])
            self.tt(QKT[0:L, :, 0:L], ps[4][0:L, 0:4 * L].re("p (h s) -> p h s", h=4, s=L), DT[0:L, :, 0:L], ALU.mult)
            self.tt(QKT[0:L, :, 0:L], QKT[0:L, :, 0:L], rnk.un(2).bc([L, 4, L]), ALU.mult)
            self.act(eg[0:L], gc[0:L], AF.Exp)
            self.mm(ps[0][:, 16:20], M['last'][0:L, :], gc[0:L])
            self.cp(gl, ps[0][:, 16:20])
            self.act(egl, gl, AF.Exp)
            self.tt(kds[0:L], gl[0:L], gc[0:L], ALU.subtract)
            self.act(kds[0:L], kds[0:L], AF.Exp)
            self.tt(kds[0:L], kds[0:L], rnk, ALU.mult)
            self.tt(sc4[0:L], beta, eg[0:L], ALU.mult)
            self.tt(sc4[0:L], sc4[0:L], rnk, ALU.mult)
            self.ts(qs[0:L], rnq, cfg.DK ** -0.5, ALU.mult)
            self.tt(egq[0:L], eg[0:L], qs[0:L], ALU.mult)
            self.tt(bv[0:L], vtm[0:L], beta.un(2).bc([L, 4, 256]), ALU.mult)
            self.tt(bk[0:L], ktm[0:L], sc4[0:L].un(2).bc([L, 4, 128]), ALU.mult)
            self.tt(kdec[0:L], ktm[0:L], kds[0:L].un(2).bc([L, 4, 128]), ALU.mult)
            if mode == 's':
                self.tt(X64, gc.un(2).bc([128, 4, 16]), self.lastind.un(1).bc([128, 4, 16]), ALU.mult)
                self.mm(ps[0][:, 64:128], self.C['ones'], X64.re("p h i -> p (h i)"))
                self.act(eglb.re("p h i -> p (h i)"), ps[0][:, 64:128], AF.Exp)
            for h in range(4):
                self.mm(ps[5][0:L, 0:256], TT[0:L, h, 0:L], bv[0:L, h, :])
                self.cp(upre[0:L], ps[5][0:L, 0:256], eng='act')
                self.mm(ps[6][:, 0:L], bk[0:L, h, :], TT[0:L, h, 0:L])
                self.cp(wT[:, h, 0:L], ps[6][:, 0:L])
                if mode == 'p':
                    self.mm(ps[7][0:L, 0:256], wT[:, h, 0:L], S[:, h, :])
                else:
                    self.dma(Ss, self.gS[l, :, h, :, :].re("i d e -> d i e"))
                    self.tt(wTz, wT[:, h, :].un(1).bc([128, 16, 128]), self.bm3, ALU.mult)
                    for i in range(16):
                        self.mm(ps[7][0:L, 0:256], wTz[:, i, :], Ss[:, i, :], start=(i == 0), stop=(i == 15))
                self.tt(u[0:L, h, :], upre[0:L], ps[7][0:L, 0:256], ALU.subtract)
                if mode == 'p':
                    self.mm(ps[5][0:L, 256:512], qT[:, h, rows], S[:, h, :])
                else:
                    self.tt(qTz, qT[:, h, rows].un(1).bc([128, 16, 128]), self.bm3, ALU.mult)
                    for i in range(16):
                        self.mm(ps[5][0:L, 256:512], qTz[:, i, :], Ss[:, i, :], start=(i == 0), stop=(i == 15))
                self.act(tmpo[0:L], ps[5][0:L, 256:512], AF.Copy, scale=egq[0:L, h:h + 1])
                self.mm(ps[6][0:L, 256:512], QKT[0:L, h, 0:L], u[0:L, h, :])
                self.stt(o_[0:L, h, :], ps[6][0:L, 256:512], qs[0:L, h:h + 1], tmpo[0:L], ALU.mult, ALU.add)
                if mode == 'p':
                    self.mm(ps[7][:, 256:512], kdec[0:L, h, :], u[0:L, h, :])
                    self.stt(S[:, h, :], S[:, h, :], egl[:, h:h + 1], ps[7][:, 256:512], ALU.mult, ALU.add)
                else:
                    self.tt(kdz, kdec[:, h, :].un(1).bc([128, 16, 128]), self.blockind.un(2).bc([128, 16, 128]), ALU.mult)
                    for i in range(16):
                        pu = ps[2 + self.rot('gdpu', 2)]
                        self.mm(pu[:, 0:256], kdz[:, i, :], u[:, h, :])
                        self.stt(Ss[:, i, :], Ss[:, i, :], eglb[:, h, i:i + 1], pu[:, 0:256], ALU.mult, ALU.add)
                    self.dma(self.o_sgS[l, :, h, :, :].re("i d e -> d i e").k(h), Ss)
            sqv = bv
            self.tt(sqv[0:L], o_[0:L], o_[0:L], ALU.mult)
            self.red(ss[0:L], sqv[0:L], ALU.add)
            self.ts(ss[0:L], ss[0:L], 1.0 / 256.0, ALU.mult, 1e-6, ALU.add)
            self.act(ss[0:L], ss[0:L], AF.Sqrt)
            self.recip(ss[0:L], ss[0:L])
            self.act(wz[0:L], ztm[0:L], AF.Silu)
            self.tt(wz[0:L].re("p (h e) -> p h e", h=4, e=256), wz[0:L].re("p (h e) -> p h e", h=4, e=256),
                    gnw[0:L].un(1).bc([L, 4, 256]), ALU.mult)
            ycv = yc[0:L].re("p (h e) -> p h e", h=4, e=256)
            self.tt(ycv, o_[0:L], ss[0:L].un(2).bc([L, 4, 256]), ALU.mult)
            self.tt(yc[0:L], yc[0:L], wz[0:L], ALU.mult)
            for j in range(8):
                pt = ps[0] if (j * L) < 512 else ps[1]
                c0 = (j * L) % 512
                self.tr(pt[:, c0:c0 + L], yc[0:L, j * 128:(j + 1) * 128])
            yst = ycT[self.rot('gd_yst', 2)]
            if L == 64:
                self.cp(yst[:, :, 0:64], ps[0][:, 0:512].re("p (j t) -> p j t", j=8, t=64), eng='act')
            else:
                self.cp(yst[:, 0:4, :], ps[0][:, 0:512].re("p (j t) -> p j t", j=4, t=128), eng='act')
                self.cp(yst[:, 4:8, :], ps[1][:, 0:512].re("p (j t) -> p j t", j=4, t=128), eng='act')
            self.dma(self.YC[:, rows].re("(j p) t -> p j t", p=128).k(t0), yst[:, :, 0:L])

        for c in range(NCH):
            pass2('p', c * 64, 64, G['p'][:, c, 8:12], G['p'][:, c, 12:16], rn_p[:, c, 0:4], rn_p[:, c, 4:8], c)
        self.dma(self.o_pgS[l].re("h d e -> d h e"), S)
        pass2('s', SEQ, 128, G['s'][:, 0, 8:12], G['s'][:, 0, 12:16], rn_s[:, 0, 0:4], rn_s[:, 0, 4:8], None)

    def phaseC1(self, l):
        cfg = self.cfg
        NT, D = cfg.NT, cfg.D
        al = self.alloc
        ys = []
        for nm, src in (('a', self.YA), ('b', self.YB), ('c', self.YC)):
            t = al("c1_y" + nm, [128, 8, NT], BF16)
            self.dma(t, src.re("(j p) t -> p j t", p=128))
            ys.append(t)
        wts = [[al(f"c1_w{x}{i}", [128, 8, 128], BF16) for i in range(2)] for x in range(3)]
        sg = [[al(f"c1_sg{x}{i}", [128, NT], BF16) for i in range(2)] for x in range(3)]
        acc = [al(f"c1_acc{i}", [128, 512]) for i in range(2)]
        tmpb = [al(f"c1_tmp{i}", [128, 512]) for i in range(2)]
        mt = [al(f"c1_mt{i}", [128, NT], BF16) for i in range(2)]
        wn = ('w_up_mlstm', 'w_up_rglru', 'w_up_gdn')
        for j in range(D // 128):
            b = j % 2
            for x in range(3):
                self.dma(wts[x][b], self.W[wn[x]][l, :, j * 128:(j + 1) * 128].re("(k p) n -> p k n", p=128), eng='pool')
                self.dma(sg[x][b], self.MG[x * D + j * 128:x * D + (j + 1) * 128, :])
            for (t0, n) in cfg.groups:
                pss = []
                for x in range(3):
                    ps = self.ps[self.rot('c1ps', 6)]
                    for k in range(8):
                        self.mm(ps[:, 0:n], wts[x][b][:, k, :], ys[x][:, k, t0:t0 + n], start=(k == 0), stop=(k == 7))
                    pss.append(ps)
                a_ = acc[self.rot('c1acc', 2)]
                tm_ = tmpb[self.rot('c1tmp', 2)]
                self.tt(a_[:, 0:n], pss[0][:, 0:n], sg[0][b][:, t0:t0 + n], ALU.mult)
                self.tt(tm_[:, 0:n], pss[1][:, 0:n], sg[1][b][:, t0:t0 + n], ALU.mult)
                self.tt(a_[:, 0:n], a_[:, 0:n], tm_[:, 0:n], ALU.add)
                self.tt(tm_[:, 0:n], pss[2][:, 0:n], sg[2][b][:, t0:t0 + n], ALU.mult)
                self.tt(mt[b][:, t0:t0 + n], a_[:, 0:n], tm_[:, 0:n], ALU.add)
            self.dma(self.MT[j * 128:(j + 1) * 128, :].k(j), mt[b])

    def layernorm(self, z, g, b, out, tmp, s):
        D = self.cfg.D
        self.red(s[:, 0:1], z, ALU.add)
        self.ts(s[:, 0:1], s[:, 0:1], -1.0 / D, ALU.mult)
        self.act(z, z, AF.Identity, bias=s[:, 0:1])
        self.act(tmp, z, AF.Square)
        self.red(s[:, 1:2], tmp, ALU.add)
        self.ts(s[:, 1:2], s[:, 1:2], 1.0 / D, ALU.mult, 1e-5, ALU.add)
        self.act(s[:, 1:2], s[:, 1:2], AF.Sqrt)
        self.recip(s[:, 1:2], s[:, 1:2])
        self.act(z, z, AF.Copy, scale=s[:, 1:2])
        self.tt(z, z, g, ALU.mult, eng='pool')
        self.tt(out, z, b, ALU.add, eng='pool')

    def load_w_bf16(self, dst, src2d, kc):
        N = src2d.ap.shape[1]
        for c0 in range(0, N, 512):
            n = min(512, N - c0)
            self.dma(dst[:, :, c0:c0 + n].k(c0), src2d[:, c0:c0 + n].re("(k p) n -> p k n", p=128), eng='pool')

    def phaseC2(self, l):
        cfg = self.cfg
        NT, D, KC, NTT = cfg.NT, cfg.D, cfg.KC, cfg.NTT
        al = self.alloc
        wout = al("c2_wout", [128, KC, D], BF16)
        self.load_w_bf16(wout, self.W['w_out'][l], KC)
        g = al("c2_g", [128, D])
        b = al("c2_b", [128, D])
        self.dma(g, V(self.W['ln'].ap[l, 0, :].partition_broadcast(128), 'ln'))
        self.dma(b, V(self.W['ln'].ap[l, 1, :].partition_broadcast(128), 'ln'))
        mtl = [al(f"c2_mt{i}", [128, KC, 128], BF16) for i in range(2)]
        xt = [al(f"c2_x{i}", [128, D]) for i in range(2)]
        z = [al(f"c2_z{i}", [128, D]) for i in range(2)]
        tmp = al("c2_tmp", [128, D])
        x1 = [al(f"c2_x1{i}", [128, D]) for i in range(2)]
        s = [al(f"c2_s{i}", [128, 4]) for i in range(2)]
        rkeys = [c0 for c0 in range(0, D, 512)]
        def front(tt):
            bb = tt % 2
            self.dma(mtl[bb], self.MT[:, tt * 128:(tt + 1) * 128].re("(k p) t -> p k t", p=128))
            self.dma(xt[bb], self.x_src(l, tt))
            for cb in range(D // 512):
                ps = self.ps[self.rot('c2ps', 8)]
                for k in range(KC):
                    self.mm(ps, mtl[bb][:, k, :], wout[:, k, cb * 512:(cb + 1) * 512].k(cb * 512), start=(k == 0), stop=(k == KC - 1))
                self.stt(z[bb][:, cb * 512:(cb + 1) * 512], xt[bb][:, cb * 512:(cb + 1) * 512], cfg.alpha, ps, ALU.mult, ALU.add)

        def back(tt):
            bb = tt % 2
            self.layernorm(z[bb], g, b, x1[bb], tmp, s[bb])
            self.dma(self.X1[tt * 128:(tt + 1) * 128, :].k(tt), x1[bb])

        front(0)
        for tt in range(NTT):
            fr = self.record(front, tt + 1) if tt + 1 < NTT else []
            bk = self.record(back, tt)
            self.play_merged(fr, bk)

    def phaseFFN(self, l):
        cfg = self.cfg
        NT, D, KC, NTT = cfg.NT, cfg.D, cfg.KC, cfg.NTT
        al = self.alloc
        moe = (l % 2 == 1)
        j = l // 2
        W = self.W
        if moe:
            NE, DF = cfg.NE, cfg.DFFE
            w1 = lambda e: W['moe_w1'][j, e]
            w3 = lambda e: W['moe_w3'][j, e]
            w2 = lambda e: W['moe_w2'][j, e]
        else:
            NE, DF = 1, cfg.DFF
            w1 = lambda e: W['ffn_w1'][j]
            w3 = lambda e: W['ffn_w3'][j]
            w2 = lambda e: W['ffn_w2'][j]
        x1T = al("ff_x1T", [128, KC, NT], BF16)
        Gbc = None
        if moe:
            Gbc = al("ff_Gbc", [128, NE, NT], BF16)
            rt = al("ff_rt", [128, KC, NE])
            if 'rtdma' not in MOE_DBG:
                self.dma(rt, W['moe_router'][j].re("(k p) e -> p k e", p=128))
            xf = al("ff_xf", [128, KC, 128])
            lg = al("ff_lg", [128, NE])
            lg2 = al("ff_lg2", [128, NE])
            eq1 = al("ff_eq1", [128, NE])
            eq2 = al("ff_eq2", [128, NE])
            gts = al("ff_gts", [128, NE])
            sc = al("ff_sc", [128, 4])
            gtmp = al("ff_gtmp", [128, NE, 128])
        xt = [al(f"ff_xs{i}", [128, D]) for i in range(2)]
        for tt in range(NTT):
            st = xt[tt % 2]
            self.dma(st, self.X1[tt * 128:(tt + 1) * 128, :].k(tt))
            for q in range((KC + 3) // 4):
                nk = min(4, KC - q * 4)
                ps = self.ps[self.rot('xTps', 2)]
                for jj in range(nk):
                    k = q * 4 + jj
                    self.tr(ps[:, jj * 128:(jj + 1) * 128], st[:, k * 128:(k + 1) * 128])
                self.evac(x1T[:, q * 4:q * 4 + nk, tt * 128:(tt + 1) * 128].k(tt), ps[:, 0:nk * 128].re("p (a b) -> p a b", a=nk, b=128))
                if moe and 'xf' not in MOE_DBG:
                    self.cp(xf[:, q * 4:q * 4 + nk, :], ps[:, 0:nk * 128].re("p (a b) -> p a b", a=nk, b=128), eng='act')
            if moe and 'router' in MOE_DBG:
                self.memset(gts, 0.125)
            if moe and 'gbc' in MOE_DBG:
                self.memset(Gbc[:, :, tt * 128:(tt + 1) * 128].k(tt), 1.0)
            if moe and 'router' not in MOE_DBG:
                pl = self.ps[2]
                for k in range(KC):
                    self.mm(pl[:, 0:NE], xf[:, k, :], rt[:, k, :], start=(k == 0), stop=(k == KC - 1))
                self.cp(lg, pl[:, 0:NE])
                self.red(sc[:, 0:1], lg, ALU.max)
                self.ts(eq1, lg, sc[:, 0:1], ALU.is_equal)
                self.stt(lg2, eq1, NEG, lg, ALU.mult, ALU.add)
                self.red(sc[:, 1:2], lg2, ALU.max)
                self.ts(eq2, lg2, sc[:, 1:2], ALU.is_equal)
                self.tt(sc[:, 2:3], sc[:, 1:2], sc[:, 0:1], ALU.subtract)
                self.act(sc[:, 2:3], sc[:, 2:3], AF.Exp)
                self.ts(sc[:, 3:4], sc[:, 2:3], 1.0, ALU.add)
                self.recip(sc[:, 3:4], sc[:, 3:4])
                self.tt(sc[:, 2:3], sc[:, 2:3], sc[:, 3:4], ALU.mult)
                self.ts(eq1, eq1, sc[:, 3:4], ALU.mult)
                self.stt(gts, eq2, sc[:, 2:3], eq1, ALU.mult, ALU.add)
            if moe and 'gbc' not in MOE_DBG:
                self.cp(gtmp, gts.un(2).bc([128, NE, 128]))
                for e0 in range(0, NE, 4):
                    pg = self.ps[3 + self.rot('gbps', 2)]
                    ne = min(4, NE - e0)
                    for e in range(ne):
                        self.mm(pg[:, e * 128:(e + 1) * 128], gtmp[:, e0 + e, :], self.ident)
                    self.cp(Gbc[:, e0:e0 + ne, tt * 128:(tt + 1) * 128].k(tt), pg[:, 0:ne * 128].re("p (a b) -> p a b", a=ne, b=128), eng='act')
        WT = 256 if moe else 512
        w1b = [al(f"ff_w1b{i}", [128, KC, WT], BF16) for i in range(2)]
        w3b = [al(f"ff_w3b{i}", [128, KC, WT], BF16) for i in range(2)]
        sil = [al(f"ff_sil{i}", [128, 512]) for i in range(2)]
        hb = [al(f"ff_hb{i}", [128, 512]) for i in range(2)]
        hst = [al(f"ff_hst{i}", [128, 512], BF16) for i in range(3)]
        xkeys = lambda t0, n: [("ff_x1T", tt) for tt in range(t0 // 128, (t0 + n + 127) // 128)]
        gkeys = lambda t0, n: [("ff_Gbc", tt) for tt in range(t0 // 128, (t0 + n + 127) // 128)]
        for e in range(1 if 'ne1' in MOE_DBG else NE):
            for c0 in range(0, DF, WT):
                nb = min(WT, DF - c0)
                bi = self.rot('ffw', 2)
                self.dma(w1b[bi][:, :, 0:nb], w1(e)[:, c0:c0 + nb].re("(k p) n -> p k n", p=128), eng='pool')
                self.dma(w3b[bi][:, :, 0:nb], w3(e)[:, c0:c0 + nb].re("(k p) n -> p k n", p=128), eng='pool')
                for sub in range(nb // 128):
                    row0 = e * DF + c0 + sub * 128
                    for (t0, n) in cfg.groups:
                        p1 = self.ps[self.rot('ffps', 8)]
                        p3 = self.ps[self.rot('ffps', 8)]
                        for (pp_, wb_) in ((p1, w1b[bi]), (p3, w3b[bi])):
                            for k in range(KC):
                                self.emit_op('pe', lambda h, pp_=pp_, wb_=wb_, k=k, t0=t0, n=n, sub=sub: h.matmul(
                                    pp_.ap[:, 0:n], lhsT=wb_.ap[:, k, sub * 128:(sub + 1) * 128], rhs=x1T.ap[:, k, t0:t0 + n],
                                    start=(k == 0), stop=(k == KC - 1)), reads=[wb_.key] + xkeys(t0, n), writes=[pp_.key], accum=True)
                        si = sil[self.rot('ffsil', 2)]
                        self.act(si[:, 0:n], p1[:, 0:n], AF.Silu)
                        ho = hst[self.rot('ffhst', 3)]
                        if moe:
                            hh_ = hb[self.rot('ffhb', 2)]
                            self.tt(hh_[:, 0:n], si[:, 0:n], p3[:, 0:n], ALU.mult)
                            geng = 'dve' if 'poolmul' in MOE_DBG else 'pool'
                            hnd = (lambda h: h)
                            self.emit_op(geng, lambda h, ho=ho, hh_=hh_, e=e, t0=t0, n=n: h.tensor_tensor(
                                out=ho.ap[:, 0:n], in0=hh_.ap[:, 0:n], in1=Gbc.ap[:, e, t0:t0 + n], op=ALU.mult),
                                reads=[hh_.key] + gkeys(t0, n), writes=[ho.key])
                        else:
                            self.tt(ho[:, 0:n], si[:, 0:n], p3[:, 0:n], ALU.mult)
                        self.dma(self.HT[row0:row0 + 128, t0:t0 + n].k((row0, t0)), ho[:, 0:n])
        self.P.barrier()
        self.off = self.mark
        if self.stop_after == (l, 'phaseFFN_s1'):
            self.stopped = True
            return
        KT = NE * DF // 128
        SEG = 8
        facc = al("ff_facc", [128, NTT, 1024])
        w2b = [al(f"ff_w2b{i}", [128, SEG, 1024], BF16) for i in range(2)]
        hTb = [al(f"ff_hTb{i}", [128, SEG, NT], BF16) for i in range(2)]
        CH = min(1024, D)
        CW = min(512, CH)
        NCI = CH // CW
        for half in range(D // CH):
            first = True
            for e in range(NE):
                kpe = DF // 128
                for k0 in range(0, kpe, SEG):
                    nk = min(SEG, kpe - k0)
                    bi = self.rot('ffw2', 2)
                    r0 = k0 * 128
                    for ci in range(NCI):
                        cc = ci * CW
                        self.dma(w2b[bi][:, 0:nk, cc:cc + CW].k(cc),
                                 w2(e)[r0:r0 + nk * 128, half * CH + cc:half * CH + cc + CW].re("(k p) n -> p k n", p=128), eng='pool')
                    self.dma(hTb[bi][:, 0:nk, :], self.HT[e * DF + r0:e * DF + r0 + nk * 128, :].re("(k p) t -> p k t", p=128))
                    for tt in range(NTT):
                        for ci in range(NCI):
                            pq = self.ps[self.rot('ff2ps', 8)]
                            for k in range(nk):
                                self.mm(pq[:, 0:CW], hTb[bi][:, k, tt * 128:(tt + 1) * 128], w2b[bi][:, k, ci * CW:(ci + 1) * CW].k(ci * CW),
                                        start=(k == 0), stop=(k == nk - 1))
                            dstv = facc[:, tt, ci * CW:(ci + 1) * CW].k((tt, ci))
                            if first:
                                self.evac(dstv, pq[:, 0:CW])
                            else:
                                self.tt(dstv, dstv, pq[:, 0:CW], ALU.add)
                    first = False
            self.emit_op('sp', lambda h, half=half: h.dma_start(
                out=self.FF.ap[:, half * CH:(half + 1) * CH].rearrange("(t p) c -> p t c", p=128), in_=facc.ap[:, :, 0:CH]),
                reads=[("ff_facc", (tt, ci)) for tt in range(NTT) for ci in range(NCI)], writes=[("FF", half)], dma=True)

    def phaseF(self, l):
        cfg = self.cfg
        NT, D, KC, NTT, PD = cfg.NT, cfg.D, cfg.KC, cfg.NTT, cfg.PD
        al = self.alloc
        pgw = al("f_pgw", [128, KC, D], BF16)
        self.load_w_bf16(pgw, self.W['ple_gate_w'][l], KC)
        plw = al("f_plw", [128, PD // 128, D], BF16)
        self.load_w_bf16(plw, self.W['ple_w'][l], PD // 128)
        g = al("f_g", [128, D])
        b = al("f_b", [128, D])
        self.dma(g, V(self.W['ln'].ap[l, 2, :].partition_broadcast(128), 'ln'))
        self.dma(b, V(self.W['ln'].ap[l, 3, :].partition_broadcast(128), 'ln'))
        x1 = [al(f"f_x1{i}", [128, D]) for i in range(2)]
        ff = [al(f"f_ff{i}", [128, D]) for i in range(2)]
        pt = [al(f"f_p{i}", [128, PD]) for i in range(2)]
        x1T = [al(f"f_x1T{i}", [128, KC, 128], BF16) for i in range(2)]
        pT = [al(f"f_pT{i}", [128, PD // 128, 128], BF16) for i in range(2)]
        z = [al(f"f_z{i}", [128, D]) for i in range(2)]
        sgt = [al(f"f_sg{i}", [128, 512]) for i in range(2)]
        tmp = al("f_tmp", [128, D])
        out = [al(f"f_out{i}", [128, D]) for i in range(2)]
        s = [al(f"f_s{i}", [128, 4]) for i in range(2)]
        def front(tt):
            bb = tt % 2
            self.dma(x1[bb], self.X1[tt * 128:(tt + 1) * 128, :].k(tt))
            self.emit_op('sp', lambda h, bb=bb, tt=tt: h.dma_start(out=ff[bb].ap, in_=self.FF.ap[tt * 128:(tt + 1) * 128, :]),
                      reads=[("FF", hf) for hf in range(max(1, D // 1024))], writes=[ff[bb].key], dma=True)
            if tt < NTT - 1:
                self.dma(pt[bb], self.pp[l, tt * 128:(tt + 1) * 128, :])
            else:
                self.dma(pt[bb], self.psm[l])
            for q in range((KC + 3) // 4):
                nk = min(4, KC - q * 4)
                ps = self.ps[self.rot('xTps', 2)]
                for jj in range(nk):
                    k = q * 4 + jj
                    self.tr(ps[:, jj * 128:(jj + 1) * 128], x1[bb][:, k * 128:(k + 1) * 128])
                self.evac(x1T[bb][:, q * 4:q * 4 + nk, :], ps[:, 0:nk * 128].re("p (a b) -> p a b", a=nk, b=128))
            ps = self.ps[self.rot('xTps', 2)]
            for jj in range(PD // 128):
                self.tr(ps[:, jj * 128:(jj + 1) * 128], pt[bb][:, jj * 128:(jj + 1) * 128])
            self.evac(pT[bb], ps[:, 0:PD].re("p (a b) -> p a b", a=PD // 128, b=128))
            for cb in range(D // 512):
                pg = self.ps[2 + self.rot('fps', 6)]
                pl = self.ps[2 + self.rot('fps', 6)]
                for k in range(KC):
                    self.mm(pg, x1T[bb][:, k, :], pgw[:, k, cb * 512:(cb + 1) * 512].k(cb * 512), start=(k == 0), stop=(k == KC - 1))
                for k in range(PD // 128):
                    self.mm(pl, pT[bb][:, k, :], plw[:, k, cb * 512:(cb + 1) * 512].k(cb * 512), start=(k == 0), stop=(k == PD // 128 - 1))
                sg_ = sgt[self.rot('fsg', 2)]
                self.act(sg_, pg, AF.Sigmoid)
                self.tt(sg_, sg_, pl, ALU.mult)
                zc = z[bb][:, cb * 512:(cb + 1) * 512]
                self.stt(zc, x1[bb][:, cb * 512:(cb + 1) * 512], cfg.alpha, ff[bb][:, cb * 512:(cb + 1) * 512], ALU.mult, ALU.add)
                self.tt(zc, zc, sg_, ALU.add, eng='pool')

        def back(tt):
            bb = tt % 2
            self.layernorm(z[bb], g, b, out[bb], tmp, s[bb])
            self.dma(self.x_dst(l, tt), out[bb])

        front(0)
        for tt in range(NTT):
            fr = self.record(front, tt + 1) if tt + 1 < NTT else []
            bk = self.record(back, tt)
            self.play_merged(fr, bk)


def run_cfg(cfg, inp, n_cores, debug=False, stop_after=None):
    DEPTH, D = cfg.DEPTH, cfg.D
    names, carr, c2 = make_consts()
    bld = Builder(cfg, debug=debug, stop_after=stop_after)
    nc = bld.build()
    f = lambda a: np.ascontiguousarray(a, dtype=np.float32)
    shared = {
        'consts': carr, 'consts2': c2,
        'w_in': f(inp['w_in']),
        'gbias': f(np.concatenate([inp['ml_b_i'], inp['ml_b_f'], np.zeros_like(inp['ml_b_i']), inp['gd_dt_bias']], axis=1)),
        'gd_A_log': f(inp['gd_A_log']), 'ml_norm_w': f(inp['ml_norm_w']),
        'rg_conv_wT': f(np.transpose(inp['rg_conv_w'], (0, 2, 1))),
        'rg_vecs': f(np.stack([inp['rg_conv_b'], inp['rg_b_a'], inp['rg_b_x'], inp['rg_lambda']], axis=-1)),
        'rg_w_a': f(inp['rg_w_a']), 'rg_w_x': f(inp['rg_w_x']),
        'gd_conv_wT': f(np.transpose(inp['gd_conv_w'], (0, 2, 1))),
        'gd_norm_w': f(inp['gd_norm_w']),
        'w_up_mlstm': f(inp['w_up_mlstm']), 'w_up_rglru': f(inp['w_up_rglru']), 'w_up_gdn': f(inp['w_up_gdn']),
        'w_out': f(inp['w_out']),
        'ln': f(np.stack([inp['ln1_g'], inp['ln1_b'], inp['ln2_g'], inp['ln2_b']], axis=1)),
        'ffn_w1': f(inp['ffn_w1']), 'ffn_w3': f(inp['ffn_w3']), 'ffn_w2': f(inp['ffn_w2']),
        'moe_router': f(inp['moe_router']), 'moe_w1': f(inp['moe_w1']), 'moe_w3': f(inp['moe_w3']), 'moe_w2': f(inp['moe_w2']),
        'ple_w': f(inp['ple_w']), 'ple_gate_w': f(inp['ple_gate_w']),
    }
    in_maps = []
    for c in range(n_cores):
        sq = c // 2
        sl = slice(16 * c, 16 * c + 16)
        m = dict(shared)
        m['xp'] = f(inp['x_prompt'][sq])
        m['xs'] = f(inp['x_sample'][sl].reshape(128, D))
        m['pp'] = f(inp['p_prompt'][:, sq])
        m['psm'] = f(inp['p_sample'][:, sl].reshape(DEPTH, 128, cfg.PD))
        m['sCT'] = f(np.transpose(inp['state_mlstm_C'][:, sl], (0, 1, 2, 4, 3)))
        m['snT'] = f(np.transpose(inp['state_mlstm_n'][:, sl], (0, 2, 3, 1)))
        m['sm'] = f(inp['state_mlstm_m'][:, sl])
        m['rhT'] = f(np.transpose(inp['state_rglru_h'][:, sl], (0, 2, 1)))
        m['rcT'] = f(np.transpose(inp['state_rglru_conv'][:, sl], (0, 3, 1, 2)))
        m['gS'] = f(inp['state_gdn_S'][:, sl])
        m['gcT'] = f(np.transpose(inp['state_gdn_conv'][:, sl], (0, 3, 1, 2)))
        in_maps.append(m)
    res = run_bass_kernel_spmd(nc, in_maps, core_ids=list(range(n_cores)))
    R = res.results
    if debug:
        return R
    B = n_cores // 2
    NB = n_cores * 16
    SEQ = cfg.SEQ
    y_p = np.zeros((B, SEQ, D), np.float32)
    y_s = np.zeros((NB, 8, D), np.float32)
    pC = np.zeros((DEPTH, B, 4, 256, 128), np.float32)
    pn = np.zeros((DEPTH, B, 4, 128), np.float32)
    pm = np.zeros((DEPTH, B, 4), np.float32)
    prh = np.zeros((DEPTH, B, 1024), np.float32)
    prc = np.zeros((DEPTH, B, 3, 1024), np.float32)
    pgS = np.zeros((DEPTH, B, 4, 128, 256), np.float32)
    pgc = np.zeros((DEPTH, B, 3, 2048), np.float32)
    sC = np.zeros((DEPTH, NB, 4, 256, 128), np.float32)
    sn = np.zeros((DEPTH, NB, 4, 128), np.float32)
    sm = np.zeros((DEPTH, NB, 4), np.float32)
    srh = np.zeros((DEPTH, NB, 1024), np.float32)
    src = np.zeros((DEPTH, NB, 3, 1024), np.float32)
    sgS = np.zeros((DEPTH, NB, 4, 128, 256), np.float32)
    sgc = np.zeros((DEPTH, NB, 3, 2048), np.float32)
    for c in range(n_cores):
        r = R[c]
        sl = slice(16 * c, 16 * c + 16)
        y_s[sl] = r['ys'].reshape(16, 8, D)
        sC[:, sl] = np.transpose(r['o_sCT'][..., 0:256], (0, 1, 2, 4, 3))
        sn[:, sl] = r['o_sCT'][..., 256]
        sm[:, sl] = r['o_sm']
        srh[:, sl] = np.transpose(r['o_rh'][:, :, 1:17], (0, 2, 1))
        oc = r['o_conv'][:, 1].reshape(DEPTH, 16, 8, 3072)[:, :, 5:8, :]
        src[:, sl] = oc[..., 0:1024]
        sgc[:, sl] = oc[..., 1024:3072]
        sgS[:, sl] = r['o_sgS']
        if c % 2 == 0:
            sq = c // 2
            y_p[sq] = r['yp']
            pC[:, sq] = np.transpose(r['o_pCT'][..., 0:256], (0, 1, 3, 2))
            pn[:, sq] = r['o_pCT'][..., 256]
            pm[:, sq] = r['o_pm']
            prh[:, sq] = r['o_rh'][:, :, 0]
            prc[:, sq] = r['o_conv'][:, 0, 125:128, 0:1024]
            pgc[:, sq] = r['o_conv'][:, 0, 125:128, 1024:3072]
            pgS[:, sq] = r['o_pgS']
    return (y_p, y_s, pC, pn, pm, prh, prc, pgS, pgc, sC, sn, sm, srh, src, sgS, sgc)


def kernel(**inputs):
    cfg = Cfg()
    inp = {k: np.asarray(v) for k, v in inputs.items()}
    return run_cfg(cfg, inp, 8)
```

```python
import contextlib
import os
import numpy as np
import concourse.bass as bass
import concourse.mybir as mybir
from concourse.bass_utils import run_bass_kernel_spmd

F32 = mybir.dt.float32
BF16 = mybir.dt.bfloat16
AF = mybir.ActivationFunctionType
ALU = mybir.AluOpType
AX = mybir.AxisListType

ENGS = ('pe', 'act', 'dve', 'pool', 'sp')
SEM_CAP = 30000
N_DMA_SEMS = {'sp': 16, 'act': 4, 'pool': 8}
NEG = -1.0e30
MOE_DBG = set(x for x in os.environ.get('MOE_DBG', '').split(',') if x)


class _Op:
    __slots__ = ('eng', 'fn', 'deps', 'dma', 'needs_inc', 'sem', 'val', 'accum')

    def __init__(self, eng, fn, dma, accum):
        self.eng = eng
        self.fn = fn
        self.dma = dma
        self.accum = accum
        self.deps = ()
        self.needs_inc = False
        self.sem = None
        self.val = 0


class _St:
    __slots__ = ('w', 'wd', 'r', 'rd')

    def __init__(self):
        self.w = {}
        self.wd = []
        self.r = {}
        self.rd = []


class Prog:
    def __init__(self, nc):
        self.nc = nc
        self.ops = {e: [] for e in ENGS}
        self.res = {}
        self.last = {}
        self.dmas = []
        self.pending = {e: [] for e in ENGS}

    def barrier(self):
        deps = list(self.last.values()) + list(self.dmas)
        for e in ENGS:
            self.pending[e] = list(deps)
        self.dmas = []
        self.res = {}

    def op(self, eng, fn, reads=(), writes=(), dma=False, accum=False):
        o = _Op(eng, fn, dma, accum)
        deps = []
        res = self.res
        for k in reads:
            st = res.get(k)
            if st is not None:
                deps.extend(st.w.values())
                deps.extend(st.wd)
                if isinstance(k, str) and k.startswith('ps') and k[2:].isdigit():
                    for re_, ro in st.r.items():
                        if re_ != eng:
                            deps.append(ro)
        for k in writes:
            st = res.get(k)
            if st is not None:
                for we, wo in st.w.items():
                    if accum and wo.accum and we == eng:
                        continue
                    deps.append(wo)
                deps.extend(st.wd)
                deps.extend(st.r.values())
                deps.extend(st.rd)
        if self.pending[eng]:
            deps.extend(self.pending[eng])
            self.pending[eng] = []
        for k in reads:
            st = res.get(k)
            if st is None:
                st = res[k] = _St()
            if dma:
                st.rd.append(o)
            else:
                st.r[eng] = o
        for k in writes:
            st = res.get(k)
            if st is None:
                st = res[k] = _St()
            if dma:
                st.w = {}
                st.wd = [o]
            else:
                st.w = {eng: o}
                st.wd = []
            st.r = {}
            st.rd = []
        dd = []
        seen = set()
        for d in deps:
            if d is o or id(d) in seen:
                continue
            seen.add(id(d))
            dd.append(d)
            d.needs_inc = True
        o.deps = dd
        self.ops[eng].append(o)
        if dma:
            self.dmas.append(o)
        else:
            self.last[eng] = o
        return o

    def emit(self, stack):
        nc = self.nc
        eng_sems = {}
        for e in ENGS:
            n_inc = sum(1 for o in self.ops[e] if (o.needs_inc and not o.dma))
            n_s = max(1, (n_inc + SEM_CAP - 1) // SEM_CAP)
            eng_sems[e] = [stack.enter_context(nc.semaphore(f"c_{e}_{i}")) for i in range(n_s)]
        dma_sems = {}
        for e in ('sp', 'act', 'pool'):
            if any(o.dma for o in self.ops[e]):
                dma_sems[e] = [stack.enter_context(nc.semaphore(f"d_{e}_{i}")) for i in range(N_DMA_SEMS[e])]
        final_dma = {}
        for e in ENGS:
            cnt = 0
            dcnt = 0
            dvals = {}
            for o in self.ops[e]:
                if o.dma:
                    ss = dma_sems[e]
                    s = ss[dcnt % len(ss)]
                    dcnt += 1
                    o.sem = s
                    o.val = dvals.get(id(s), 0) + 16
                    dvals[id(s)] = o.val
                    final_dma[id(s)] = (s, o.val)
                elif o.needs_inc:
                    o.sem = eng_sems[e][cnt // SEM_CAP]
                    o.val = cnt % SEM_CAP + 1
                    cnt += 1
        block = stack.enter_context(nc.Block())
        handles = {'pe': 'tensor', 'act': 'scalar', 'dve': 'vector', 'pool': 'gpsimd', 'sp': 'sync'}

        def make(e):
            def body(h):
                waited = {}
                for o in self.ops[e]:
                    for d in o.deps:
                        key = id(d.sem)
                        if waited.get(key, 0) >= d.val:
                            continue
                        h.wait_ge(d.sem, d.val)
                        waited[key] = d.val
                    if o.dma:
                        key = id(o.sem)
                        if o.val > 16 and waited.get(key, 0) < o.val - 16:
                            h.wait_ge(o.sem, o.val - 16)
                            waited[key] = o.val - 16
                        o.fn(h).then_inc(o.sem, 16)
                    else:
                        ins = o.fn(h)
                        if o.needs_inc:
                            ins.then_inc(o.sem, 1)
                if e == 'sp':
                    for (s, v) in final_dma.values():
                        if waited.get(id(s), 0) < v:
                            h.wait_ge(s, v)
            return body

        for e in ENGS:
            getattr(block, handles[e])(make(e))


class V:
    __slots__ = ('ap', 'key')

    def __init__(self, ap, key):
        self.ap = ap
        self.key = key

    def __getitem__(self, idx):
        return V(self.ap[idx], self.key)

    def bc(self, shape):
        return V(self.ap.broadcast_to(list(shape)), self.key)

    def un(self, axis):
        return V(self.ap.unsqueeze(axis), self.key)

    def k(self, sub):
        return V(self.ap, (self.key, sub))

    def re(self, pat, **kw):
        return V(self.ap.rearrange(pat, **kw), self.key)


class Cfg:
    def __init__(self, D=2048, SEQ=2048, DFF=5632, DFFE=2816, NE=8, PD=256, DEPTH=2):
        self.D, self.SEQ, self.DFF, self.DFFE, self.NE, self.PD, self.DEPTH = D, SEQ, DFF, DFFE, NE, PD, DEPTH
        self.H, self.DK, self.DV, self.LW = 4, 128, 256, 1024
        self.NS, self.TS = 16, 8
        self.KC = D // 128
        self.NT = SEQ + 128
        self.NTT = self.NT // 128
        self.NCH = SEQ // 64
        self.GC = 2048
        o = 0
        self.off = {}
        for name, n in (('mlq', 512), ('mlk', 512), ('mlv', 1024), ('mli', 4), ('mlf', 4), ('mlo', 1024),
                        ('rgx', 1024), ('rgy', 1024), ('gdqkv', 2048), ('gdb', 4), ('gda', 4), ('gdz', 1024),
                        ('mg', 3 * D)):
            self.off[name] = o
            o += n
        self.DIN = o
        self.groups = []
        t = 0
        while t < self.NT:
            n = min(512, self.NT - t)
            self.groups.append((t, n))
            t += n
        self.alpha = (2 * DEPTH) ** 0.25


def make_consts():
    idx = np.arange(128)
    c = {}
    c['ident'] = np.eye(128, dtype=np.float32)
    for nm, B in (('P', 128), ('S', 8)):
        same = (idx[:, None] // B) == (idx[None, :] // B)
        le = idx[:, None] <= idx[None, :]
        c['tri' + nm] = (same & le).astype(np.float32)
        c['negT' + nm] = np.where(same & le, 0.0, NEG).astype(np.float32)
        c['neg' + nm] = np.where(same & le, 0.0, NEG).astype(np.float32).T.copy()
        c['strictT' + nm] = (same & (idx[:, None] < idx[None, :])).astype(np.float32)
    lastP = np.zeros((128, 128), np.float32)
    lastP[63, :] = 1.0
    c['lastP'] = lastP
    lastS = np.zeros((128, 128), np.float32)
    for m in range(128):
        lastS[8 * (m // 8) + 7, m] = 1.0
    c['lastS'] = lastS
    c['ones'] = np.ones((128, 128), np.float32)
    bT = np.zeros((128, 128), np.float32)
    for m in range(128):
        bT[m // 8, m] = 1.0
    c['blockindT'] = bT
    names = ['ident', 'triP', 'negTP', 'negP', 'strictTP', 'lastP', 'triS', 'negTS', 'negS', 'strictTS', 'lastS',
             'ones', 'blockindT']
    arr = np.stack([c[n] for n in names]).astype(np.float32)
    blockind = np.zeros((128, 16), np.float32)
    lastind = np.zeros((128, 16), np.float32)
    for t in range(128):
        blockind[t, t // 8] = 1.0
    for i in range(16):
        lastind[8 * i + 7, i] = 1.0
    bm3 = np.zeros((128, 16, 128), np.float32)
    for i in range(16):
        bm3[:, i, 8 * i:8 * i + 8] = 1.0
    c2 = np.concatenate([blockind, lastind, bm3.reshape(128, 2048)], axis=1).astype(np.float32)
    return names, arr, c2


class Builder:
    def __init__(self, cfg, debug=False, stop_after=None):
        self.cfg = cfg
        self.debug = debug
        self.stop_after = stop_after
        self.nc = bass.Bass("TRN2", target_bir_lowering=False)
        self.P = Prog(self.nc)
        self.AW = 50176
        self.off = 0
        self.mark = 0
        self.dr = {}
        self.rr = {}
        self.rec = None
        self.stopped = False


    def emit_op(self, *a, **kw):
        if self.rec is not None:
            self.rec.append((a, kw))
        else:
            self.P.op(*a, **kw)

    def record(self, fn, *args):
        assert self.rec is None
        self.rec = []
        fn(*args)
        r, self.rec = self.rec, None
        return r

    def play_merged(self, A, B):
        na, nb = len(A), len(B)
        ia = ib = 0
        while ia < na or ib < nb:
            if ib >= nb or (ia < na and ia * nb <= ib * na):
                a, kw = A[ia]
                ia += 1
            else:
                a, kw = B[ib]
                ib += 1
            self.P.op(*a, **kw)

    def dram(self, name, shape, dt=F32, kind="Internal"):
        if self.debug and kind == "Internal":
            kind = "ExternalOutput"
        t = self.nc.dram_tensor(name, list(shape), dt, kind=kind)
        v = V(t.ap(), name)
        self.dr[name] = v
        return v

    def alloc(self, name, shape, dt=F32):
        p = shape[0]
        n = int(np.prod(shape[1:]))
        words = n if dt == F32 else (n + 1) // 2
        off = self.off
        self.off += words
        assert self.off <= self.AW, f"SBUF arena overflow at {name}: {self.off}"
        ap = self.arena[0:p, off:off + words]
        if dt != F32:
            ap = ap.bitcast(dt)[:, 0:n]
        if len(shape) == 3:
            ap = ap.rearrange("p (a b) -> p a b", a=shape[1], b=shape[2])
        elif len(shape) == 4:
            ap = ap.rearrange("p (a b c) -> p a b c", a=shape[1], b=shape[2], c=shape[3])
        return V(ap, name)

    def phase_end(self):
        self.P.barrier()
        self.off = self.mark

    def mm(self, out, lhsT, rhs, start=True, stop=True):
        self.emit_op('pe', lambda h: h.matmul(out.ap, lhsT=lhsT.ap, rhs=rhs.ap, start=start, stop=stop),
                  reads=[lhsT.key, rhs.key], writes=[out.key], accum=True)

    def tr(self, out, in_):
        n = in_.ap.shape[0]
        idt = self.ident[0:n, 0:n]
        self.emit_op('pe', lambda h: h.transpose(out=out.ap, in_=in_.ap, identity=idt.ap),
                  reads=[in_.key, idt.key], writes=[out.key], accum=True)

    def act(self, out, in_, func, bias=None, scale=None):
        reads = [in_.key]
        kw = {}
        if bias is not None:
            if isinstance(bias, V):
                reads.append(bias.key)
                kw['bias'] = bias.ap
            else:
                kw['bias'] = float(bias)
        if scale is not None:
            if isinstance(scale, V):
                reads.append(scale.key)
                kw['scale'] = scale.ap
            else:
                kw['scale'] = float(scale)
        self.emit_op('act', lambda h: h.activation(out=out.ap, in_=in_.ap, func=func, **kw), reads=reads, writes=[out.key])

    def tt(self, out, a, b, op, eng='dve'):
        self.emit_op(eng, lambda h: h.tensor_tensor(out=out.ap, in0=a.ap, in1=b.ap, op=op), reads=[a.key, b.key],
                  writes=[out.key])

    def ts(self, out, a, s1, op0, s2=None, op1=None, eng='dve'):
        reads = [a.key]
        a1 = s1
        if isinstance(s1, V):
            reads.append(s1.key)
            a1 = s1.ap
        a2 = s2
        if isinstance(s2, V):
            reads.append(s2.key)
            a2 = s2.ap
        if op1 is None:
            self.emit_op(eng, lambda h: h.tensor_scalar(out=out.ap, in0=a.ap, scalar1=a1, scalar2=None, op0=op0),
                      reads=reads, writes=[out.key])
        else:
            self.emit_op(eng, lambda h: h.tensor_scalar(out=out.ap, in0=a.ap, scalar1=a1, scalar2=a2, op0=op0, op1=op1),
                      reads=reads, writes=[out.key])

    def stt(self, out, in0, scalar, in1, op0, op1):
        reads = [in0.key, in1.key]
        sc = scalar
        if isinstance(scalar, V):
            reads.append(scalar.key)
            sc = scalar.ap
        self.emit_op('dve', lambda h: h.scalar_tensor_tensor(out=out.ap, in0=in0.ap, scalar=sc, in1=in1.ap, op0=op0, op1=op1),
                  reads=reads, writes=[out.key])

    def red(self, out, in_, op):
        self.emit_op('dve', lambda h: h.tensor_reduce(out=out.ap, in_=in_.ap, axis=AX.X, op=op), reads=[in_.key],
                  writes=[out.key])

    def cp(self, out, in_, eng='dve'):
        if eng == 'act':
            self.emit_op('act', lambda h: h.activation(out=out.ap, in_=in_.ap, func=AF.Copy), reads=[in_.key], writes=[out.key])
        else:
            self.emit_op(eng, lambda h: h.tensor_copy(out=out.ap, in_=in_.ap), reads=[in_.key], writes=[out.key])

    def memset(self, out, val, eng='dve'):
        self.emit_op(eng, lambda h: h.memset(out.ap, val), writes=[out.key])

    def recip(self, out, in_):
        self.emit_op('dve', lambda h: h.reciprocal(out=out.ap, in_=in_.ap), reads=[in_.key], writes=[out.key])

    def scan(self, out, a, u):
        self.emit_op('dve', lambda h: h.tensor_tensor_scan(out=out.ap, data0=a.ap, data1=u.ap, initial=0.0, op0=ALU.mult,
                                                        op1=ALU.add), reads=[a.key, u.key], writes=[out.key])

    def dma(self, out, in_, eng='sp'):
        self.emit_op(eng, lambda h: h.dma_start(out=out.ap, in_=in_.ap), reads=[in_.key], writes=[out.key], dma=True)

    def rot(self, name, n):
        i = self.rr.get(name, 0)
        self.rr[name] = i + 1
        return i % n

    def evac(self, out, in_, i=None):
        if i is None:
            i = self.rot('evac', 2)
        self.cp(out, in_, eng=('dve' if i % 2 == 0 else 'act'))

    def build(self):
        cfg = self.cfg
        nc = self.nc
        D, SEQ, NT, KC, DEPTH = cfg.D, cfg.SEQ, cfg.NT, cfg.KC, cfg.DEPTH
        NCH = cfg.NCH
        dr = self.dram
        EI, EO = "ExternalInput", "ExternalOutput"
        self.xp = dr("xp", [SEQ, D], kind=EI)
        self.xs = dr("xs", [128, D], kind=EI)
        self.pp = dr("pp", [DEPTH, SEQ, cfg.PD], kind=EI)
        self.psm = dr("psm", [DEPTH, 128, cfg.PD], kind=EI)
        self.sCT = dr("sCT", [DEPTH, 16, 4, 128, 256], kind=EI)
        self.snT = dr("snT", [DEPTH, 4, 128, 16], kind=EI)
        self.sm = dr("sm", [DEPTH, 16, 4], kind=EI)
        self.rhT = dr("rhT", [DEPTH, 1024, 16], kind=EI)
        self.rcT = dr("rcT", [DEPTH, 1024, 16, 3], kind=EI)
        self.gS = dr("gS", [DEPTH, 16, 4, 128, 256], kind=EI)
        self.gcT = dr("gcT", [DEPTH, 2048, 16, 3], kind=EI)
        self.consts = dr("consts", [13, 128, 128], kind=EI)
        self.consts2 = dr("consts2", [128, 32 + 2048], kind=EI)
        W = {}
        W['w_in'] = dr("w_in", [DEPTH, D, cfg.DIN], kind=EI)
        W['gbias'] = dr("gbias", [DEPTH, 16], kind=EI)
        W['gd_A_log'] = dr("gd_A_log", [DEPTH, 4], kind=EI)
        W['ml_norm_w'] = dr("ml_norm_w", [DEPTH, 1024], kind=EI)
        W['rg_conv_wT'] = dr("rg_conv_wT", [DEPTH, 1024, 4], kind=EI)
        W['rg_vecs'] = dr("rg_vecs", [DEPTH, 1024, 4], kind=EI)
        W['rg_w_a'] = dr("rg_w_a", [DEPTH, 4, 256, 256], kind=EI)
        W['rg_w_x'] = dr("rg_w_x", [DEPTH, 4, 256, 256], kind=EI)
        W['gd_conv_wT'] = dr("gd_conv_wT", [DEPTH, 2048, 4], kind=EI)
        W['gd_norm_w'] = dr("gd_norm_w", [DEPTH, 256], kind=EI)
        W['w_up_mlstm'] = dr("w_up_mlstm", [DEPTH, 1024, D], kind=EI)
        W['w_up_rglru'] = dr("w_up_rglru", [DEPTH, 1024, D], kind=EI)
        W['w_up_gdn'] = dr("w_up_gdn", [DEPTH, 1024, D], kind=EI)
        W['w_out'] = dr("w_out", [DEPTH, D, D], kind=EI)
        W['ln'] = dr("ln", [DEPTH, 4, D], kind=EI)
        n_dense = (DEPTH + 1) // 2
        n_moe = DEPTH // 2
        W['ffn_w1'] = dr("ffn_w1", [n_dense, D, cfg.DFF], kind=EI)
        W['ffn_w3'] = dr("ffn_w3", [n_dense, D, cfg.DFF], kind=EI)
        W['ffn_w2'] = dr("ffn_w2", [n_dense, cfg.DFF, D], kind=EI)
        W['moe_router'] = dr("moe_router", [max(n_moe, 1), D, cfg.NE], kind=EI)
        W['moe_w1'] = dr("moe_w1", [max(n_moe, 1), cfg.NE, D, cfg.DFFE], kind=EI)
        W['moe_w3'] = dr("moe_w3", [max(n_moe, 1), cfg.NE, D, cfg.DFFE], kind=EI)
        W['moe_w2'] = dr("moe_w2", [max(n_moe, 1), cfg.NE, cfg.DFFE, D], kind=EI)
        W['ple_w'] = dr("ple_w", [DEPTH, cfg.PD, D], kind=EI)
        W['ple_gate_w'] = dr("ple_gate_w", [DEPTH, D, D], kind=EI)
        self.W = W
        self.yp = dr("yp", [SEQ, D], kind=EO)
        self.ys = dr("ys", [128, D], kind=EO)
        self.o_pCT = dr("o_pCT", [DEPTH, 4, 128, 257], kind=EO)
        self.o_pm = dr("o_pm", [DEPTH, 4], kind=EO)
        self.o_rh = dr("o_rh", [DEPTH, 1024, 17], kind=EO)
        self.o_conv = dr("o_conv", [DEPTH, 2, 128, 3072], kind=EO)
        self.o_pgS = dr("o_pgS", [DEPTH, 4, 128, 256], kind=EO)
        self.o_sCT = dr("o_sCT", [DEPTH, 16, 4, 128, 257], kind=EO)
        self.o_sm = dr("o_sm", [DEPTH, 16, 4], kind=EO)
        self.o_sgS = dr("o_sgS", [DEPTH, 16, 4, 128, 256], kind=EO)
        self.FM = dr("FM", [5120, NT])
        self.MG = dr("MG", [3 * D, NT], BF16)
        self.TM = dr("TM", [NT, 3600])
        self.KV = dr("KV", [NT, 1536])
        self.YA = dr("YA", [1024, NT], BF16)
        self.YB = dr("YB", [1024, NT], BF16)
        self.YC = dr("YC", [1024, NT], BF16)
        self.MT = dr("MT", [D, NT], BF16)
        self.X1 = dr("X1", [NT, D])
        self.XC = dr("XC", [NT, D])
        self.FF = dr("FF", [NT, D])
        self.HT = dr("HT", [max(cfg.DFF, cfg.NE * cfg.DFFE), NT], BF16)
        self.Ascr = dr("Ascr", [NCH * 4, 64, 64])
        self.Tscr = dr("Tscr", [NCH * 4, 64, 64])

        with contextlib.ExitStack() as stack:
            self.arena = stack.enter_context(nc.sbuf_tensor("arena", [128, self.AW], F32))
            self.ps = [V(stack.enter_context(nc.psum_tensor(f"ps{i}", [128, 512], F32))[:, :], f"ps{i}") for i in range(8)]
            cn, _, _ = make_consts()
            self.C = {}
            call = self.alloc("call", [128, 13, 128])
            self.dma(call, self.consts.re("c p n -> p c n"))
            for i, n in enumerate(cn):
                self.C[n] = call[:, i, :]
            self.ident = self.C['ident']
            c2 = self.alloc("c2", [128, 32 + 2048])
            self.dma(c2, self.consts2)
            self.blockind = c2[:, 0:16]
            self.lastind = c2[:, 16:32]
            self.bm3 = c2[:, 32:32 + 2048].re("p (i t) -> p i t", i=16, t=128)
            self.mark = self.off
            for l in range(DEPTH):
                self.layer(l)
            self.P.emit(stack)
        return nc

    def x_src(self, l, tt):
        cfg = self.cfg
        if l == 0:
            if tt < cfg.NTT - 1:
                return self.xp[tt * 128:(tt + 1) * 128, :].k(tt)
            return self.xs[:, :].k(tt)
        return self.XC[tt * 128:(tt + 1) * 128, :].k(tt)

    def x_dst(self, l, tt):
        cfg = self.cfg
        if l == cfg.DEPTH - 1:
            if tt < cfg.NTT - 1:
                return self.yp[tt * 128:(tt + 1) * 128, :].k(tt)
            return self.ys[:, :].k(tt)
        return self.XC[tt * 128:(tt + 1) * 128, :].k(tt)

    def layer(self, l):
        for ph in (self.phaseA, self.phaseRG, self.phaseML, self.phaseGD, self.phaseC1, self.phaseC2, self.phaseFFN,
                   self.phaseF):
            if self.stop_after is not None and self.stopped:
                return
            ph(l)
            self.phase_end()
            if self.stop_after == (l, ph.__name__):
                self.stopped = True

    def make_xT(self, xT, src_fn, want_f32=None):
        cfg = self.cfg
        KC = cfg.KC
        xt = [self.alloc(f"xt_stage{i}", [128, cfg.D]) for i in range(2)]
        for tt in range(cfg.NTT):
            st = xt[tt % 2]
            self.dma(st, src_fn(tt))
            for q in range(KC // 4 if KC >= 4 else 1):
                nk = min(4, KC)
                ps = self.ps[self.rot('xTps', 2)]
                for j in range(nk):
                    k = q * 4 + j
                    self.tr(ps[:, j * 128:(j + 1) * 128], st[:, k * 128:(k + 1) * 128])
                dst = xT[:, q * 4:q * 4 + nk, tt * 128:(tt + 1) * 128].k(tt)
                self.evac(dst, ps[:, 0:nk * 128].re("p (a b) -> p a b", a=nk, b=128))

    def phaseA(self, l):
        cfg = self.cfg
        KC, NT, NTT, D = cfg.KC, cfg.NT, cfg.NTT, cfg.D
        xT = self.alloc("xT", [128, KC, NT], BF16)
        self.make_xT(xT, lambda tt: self.x_src(l, tt))
        wbuf = [self.alloc(f"wA{i}", [128, KC, 512], BF16) for i in range(2)]
        stf = [self.alloc(f"stA{i}", [128, 512]) for i in range(4)]
        stb = [self.alloc(f"stAb{i}", [128, 512], BF16) for i in range(2)]
        wg = self.alloc("wAg", [128, KC, 16], BF16)
        win = self.W['w_in']
        o = cfg.off
        segs = [
            (o['mlq'], 512, [('fm', 0, 'q')]),
            (o['mlk'], 512, [('fm', 512, 'c'), ('tm', 0, 'c')]),
            (o['mlv'], 1024, [('tm', 512, 'c')]),
            (o['mlo'], 1024, [('tm', 1536, 'c')]),
            (o['rgx'], 1024, [('fm', 1024, 'c'), ('cv', 0, 'c')]),
            (o['rgy'], 1024, [('fm', 2048, 'c')]),
            (o['gdqkv'], 2048, [('fm', 3072, 'c'), ('cv', 1024, 'c')]),
            (o['gdz'], 1024, [('tm', 2560, 'c')]),
            (o['mg'], 3 * D, [('mg', 0, 's')]),
        ]
        xTr = lambda t0, n: [("xT", tt) for tt in range(t0 // 128, (t0 + n + 127) // 128)]

        def fm_block(wt, sub, row0, kind, dst):
            for (t0, n) in cfg.groups:
                ps = self.ps[2 + self.rot('Aps', 6)]
                for k in range(KC):
                    self.emit_op('pe', lambda h, ps=ps, k=k, t0=t0, n=n: h.matmul(
                        ps.ap[:, 0:n], lhsT=wt.ap[:, k, sub * 128:(sub + 1) * 128], rhs=xT.ap[:, k, t0:t0 + n],
                        start=(k == 0), stop=(k == KC - 1)), reads=[wt.key] + xTr(t0, n), writes=[ps.key], accum=True)
                if kind == 's':
                    st = stb[self.rot('stb', 2)]
                    self.act(st[:, 0:n], ps[:, 0:n], AF.Sigmoid)
                elif kind == 'q':
                    st = stf[self.rot('stf', 4)]
                    self.act(st[:, 0:n], ps[:, 0:n], AF.Copy, scale=cfg.DK ** -0.5)
                else:
                    st = stf[self.rot('stf', 4)]
                    self.evac(st[:, 0:n], ps[:, 0:n])
                self.dma(dst[row0:row0 + 128, t0:t0 + n].k((row0, t0)), st[:, 0:n])

        def tm_block(wt, nc_, col0, dst, tiles, dst_rows=None):
            for ti, tt in enumerate(tiles):
                ps = self.ps[2 + self.rot('Aps', 6)]
                for k in range(KC):
                    self.emit_op('pe', lambda h, ps=ps, k=k, tt=tt: h.matmul(
                        ps.ap[:, 0:nc_], lhsT=xT.ap[:, k, tt * 128:(tt + 1) * 128], rhs=wt.ap[:, k, 0:nc_],
                        start=(k == 0), stop=(k == KC - 1)), reads=[wt.key, ("xT", tt)], writes=[ps.key], accum=True)
                st = stf[self.rot('stf', 4)]
                self.evac(st[:, 0:nc_], ps[:, 0:nc_])
                if dst_rows is None:
                    self.dma(dst[tt * 128:(tt + 1) * 128, col0:col0 + nc_].k((tt, col0)), st[:, 0:nc_])
                else:
                    self.dma(dst_rows(ti)[:, col0:col0 + nc_].k((ti, col0)), st[:, 0:nc_])

        for (c0, ncols, outs) in segs:
            for b0 in range(0, ncols, 512):
                nb = min(512, ncols - b0)
                wt = wbuf[self.rot('wA', 2)]
                self.dma(wt[:, :, 0:nb], win[l, :, c0 + b0:c0 + b0 + nb].re("(k p) n -> p k n", p=128), eng='pool')
                for (mode, base, kind) in outs:
                    if mode == 'fm':
                        for sub in range(nb // 128):
                            fm_block(wt, sub, base + b0 + sub * 128, kind, self.FM)
                    elif mode == 'mg':
                        for sub in range(nb // 128):
                            fm_block(wt, sub, base + b0 + sub * 128, kind, self.MG)
                    elif mode == 'tm':
                        tm_block(wt, nb, base + b0, self.TM, list(range(NTT)))
                    elif mode == 'cv':
                        tm_block(wt, nb, base + b0, None, [NTT - 2, NTT - 1],
                                 dst_rows=lambda ti: self.o_conv[l, ti, :, :])
        self.dma(wg[:, :, 0:8], win[l, :, o['mli']:o['mli'] + 8].re("(k p) n -> p k n", p=128), eng='pool')
        self.dma(wg[:, :, 8:16], win[l, :, o['gdb']:o['gdb'] + 8].re("(k p) n -> p k n", p=128), eng='pool')
        tm_block(wg, 16, 3584, self.TM, list(range(NTT)))

    def conv_fm(self, raw_rows, bufT, cw, out, bias, xpad_p, xpad_s):
        cfg = self.cfg
        SEQ = cfg.SEQ
        self.memset(xpad_p[:, 0:3], 0.0)
        self.dma(xpad_p[:, 3:3 + SEQ], raw_rows[:, 0:SEQ])
        self.dma(xpad_s[:, :, 3:11], raw_rows[:, SEQ:SEQ + 128].re("p (i t) -> p i t", i=16, t=8))
        self.dma(xpad_s[:, :, 0:3], bufT)
        op = out[:, 0:SEQ]
        os_ = out[:, SEQ:SEQ + 128].re("p (i t) -> p i t", i=16, t=8)
        for (o_, xp_, sl) in ((op, xpad_p, lambda j: xpad_p[:, j:j + SEQ]), (os_, xpad_s, lambda j: xpad_s[:, :, j:j + 8])):
            if bias is None:
                self.ts(o_, sl(0), cw[:, 0:1], ALU.mult)
            else:
                self.ts(o_, sl(0), cw[:, 0:1], ALU.mult, bias, ALU.add)
            for j in range(1, 4):
                self.stt(o_, sl(j), cw[:, j:j + 1], o_, ALU.mult, ALU.add)

    def phaseRG(self, l):
        cfg = self.cfg
        NT, SEQ = cfg.NT, cfg.SEQ
        W = self.W
        cw = self.alloc("rg_cw", [128, 8, 4])
        self.dma(cw, W['rg_conv_wT'][l].re("(c p) j -> p c j", p=128))
        vec = self.alloc("rg_vec", [128, 8, 4])
        self.dma(vec, W['rg_vecs'][l].re("(c p) j -> p c j", p=128))
        h0 = self.alloc("rg_h0", [128, 8, 16])
        self.dma(h0, self.rhT[l].re("(c p) i -> p c i", p=128))
        hl = self.alloc("rg_hl", [128, 8, 17])
        nl = self.alloc("rg_nl", [128, 8])
        t1 = self.alloc("rg_t1", [128, 8])
        t2 = self.alloc("rg_t2", [128, 8])
        sp8 = self.alloc("rg_sp8", [128, 8])
        self.ts(nl, vec[:, :, 3], -1.0, ALU.mult)
        self.ts(t1, nl, -1.0, ALU.mult)
        self.tt(t1, t1, nl, ALU.max)
        self.act(t1, t1, AF.Exp, scale=-1.0)
        self.act(t1, t1, AF.Ln, bias=1.0)
        self.ts(t2, nl, 0.0, ALU.max)
        self.tt(t1, t1, t2, ALU.add)
        self.ts(sp8, t1, -8.0, ALU.mult)
        xpad_p = [self.alloc(f"rg_xp{i}", [128, SEQ + 3]) for i in range(2)]
        xpad_s = [self.alloc(f"rg_xs{i}", [128, 16, 11]) for i in range(2)]
        xc = [self.alloc(f"rg_xc{i}", [128, NT]) for i in range(2)]
        wa = self.alloc("rg_wa", [128, 2, 256])
        wx = self.alloc("rg_wx", [128, 2, 256])
        r = self.alloc("rg_r", [128, NT])
        ig = self.alloc("rg_i", [128, NT])
        a = self.alloc("rg_a", [128, NT])
        u = self.alloc("rg_u", [128, NT])
        hh = self.alloc("rg_h", [128, NT])
        gy = self.alloc("rg_gy", [128, NT])
        g2 = self.alloc("rg_g2", [128, NT])
        yb = self.alloc("rg_yb", [128, NT], BF16)
        t16 = self.alloc("rg_t16", [128, 16])
        for n in range(4):
            for c in range(2):
                ch = n * 2 + c
                self.conv_fm(self.FM[1024 + ch * 128:1024 + (ch + 1) * 128, :], self.rcT[l, ch * 128:(ch + 1) * 128, :, :],
                             cw[:, ch, :], xc[c], vec[:, ch, 0:1], xpad_p[c], xpad_s[c])
            self.dma(wa, W['rg_w_a'][l, n].re("(c p) e -> p c e", p=128))
            self.dma(wx, W['rg_w_x'][l, n].re("(c p) e -> p c e", p=128))
            for e in range(2):
                ch = n * 2 + e
                for (wt, dst, bcol) in ((wa, r, 1), (wx, ig, 2)):
                    for (t0, nn) in cfg.groups:
                        ps = self.ps[self.rot('rgps', 4)]
                        for c in range(2):
                            self.mm(ps[:, 0:nn], wt[:, c, e * 128:(e + 1) * 128], xc[c][:, t0:t0 + nn], start=(c == 0), stop=(c == 1))
                        self.act(dst[:, t0:t0 + nn], ps[:, 0:nn], AF.Sigmoid, bias=vec[:, ch, bcol:bcol + 1])
                self.act(a, r, AF.Exp, scale=sp8[:, ch:ch + 1])
                self.tt(u, a, a, ALU.mult)
                self.act(u, u, AF.Sqrt, bias=1.0, scale=-1.0)
                self.tt(ig, ig, xc[e], ALU.mult)
                self.tt(u, u, ig, ALU.mult)
                a_s = a[:, SEQ:SEQ + 128].re("p (i t) -> p i t", i=16, t=8)
                u_s = u[:, SEQ:SEQ + 128].re("p (i t) -> p i t", i=16, t=8)
                self.tt(t16, a_s[:, :, 0], h0[:, ch, :], ALU.mult)
                self.tt(u_s[:, :, 0], u_s[:, :, 0], t16, ALU.add)
                self.memset(a_s[:, :, 0], 0.0)
                self.scan(hh, a, u)
                h_s = hh[:, SEQ:SEQ + 128].re("p (i t) -> p i t", i=16, t=8)
                self.cp(hl[:, ch, 0:1], hh[:, SEQ - 1:SEQ])
                self.cp(hl[:, ch, 1:17], h_s[:, :, 7])
                self.dma(gy, self.FM[2048 + ch * 128:2048 + (ch + 1) * 128, :])
                self.tt(g2, gy, gy, ALU.mult)
                self.ts(g2, g2, 0.044715, ALU.mult, 1.0, ALU.add)
                self.tt(g2, g2, gy, ALU.mult)
                self.act(g2, g2, AF.Sigmoid, scale=1.5957691216057308)
                self.tt(g2, g2, gy, ALU.mult)
                self.tt(yb, g2, hh, ALU.mult)
                self.dma(self.YB[ch * 128:(ch + 1) * 128, :].k(ch), yb)
        self.dma(self.o_rh[l].re("(c p) i -> p c i", p=128), hl)

    def gates(self, l, pre):
        cfg = self.cfg
        NCH, SEQ = cfg.NCH, cfg.SEQ
        gb = self.alloc(pre + "gb", [128, 16])
        self.dma(gb, V(self.W['gbias'].ap[l, :].partition_broadcast(128), 'gbias'))
        nA = self.alloc(pre + "nA", [128, 4])
        self.dma(nA, V(self.W['gd_A_log'].ap[l, :].partition_broadcast(128), 'gd_A_log'))
        self.act(nA, nA, AF.Exp)
        self.ts(nA, nA, -1.0, ALU.mult)
        out = {}
        for nm, L, G, src in (('p', 64, NCH, self.TM[0:SEQ, 3584:3600].re("(c p) g -> p c g", p=64)),
                              ('s', 128, 1, self.TM[SEQ:SEQ + 128, 3584:3600].re("(c p) g -> p c g", p=128))):
            x = self.alloc(pre + "gx" + nm, [L, G, 16])
            self.dma(x, src)
            self.tt(x, x, gb[0:L, :].un(1).bc([L, G, 16]), ALU.add)
            t = self.alloc(pre + "gt" + nm, [L, G, 16])
            self.act(t[:, :, 0:8], x[:, :, 0:8], AF.Tanh, scale=1.0 / 15.0)
            self.ts(t[:, :, 0:8], t[:, :, 0:8], 15.0, ALU.mult)
            self.act(t[:, :, 4:8], t[:, :, 4:8], AF.Exp, scale=-1.0)
            self.act(t[:, :, 4:8], t[:, :, 4:8], AF.Ln, bias=1.0)
            self.ts(t[:, :, 4:8], t[:, :, 4:8], -1.0, ALU.mult)
            self.act(t[:, :, 8:12], x[:, :, 8:12], AF.Sigmoid)
            self.ts(t[:, :, 12:16], x[:, :, 12:16], -1.0, ALU.mult)
            self.tt(t[:, :, 12:16], t[:, :, 12:16], x[:, :, 12:16], ALU.max)
            self.act(t[:, :, 12:16], t[:, :, 12:16], AF.Exp, scale=-1.0)
            self.act(t[:, :, 12:16], t[:, :, 12:16], AF.Ln, bias=1.0)
            self.ts(x[:, :, 12:16], x[:, :, 12:16], 0.0, ALU.max)
            self.tt(t[:, :, 12:16], t[:, :, 12:16], x[:, :, 12:16], ALU.add)
            self.tt(t[:, :, 12:16], t[:, :, 12:16], nA[0:L, :].un(1).bc([L, G, 4]), ALU.mult)
            out[nm] = t
        return out

    def masks(self, mode):
        s = 'P' if mode == 'p' else 'S'
        C = self.C
        return dict(tri=C['tri' + s], negT=C['negT' + s], neg=C['neg' + s], strictT=C['strictT' + s], last=C['last' + s])

    def rowbc(self, ps, col, L, tmp):
        self.cp(tmp[0:L, :, 0:L], col.un(2).bc([L, 4, L]))
        for h in range(4):
            self.mm(ps[0:L, h * L:(h + 1) * L], tmp[0:L, h, 0:L], self.ident[0:L, 0:L])

    def phaseML(self, l):
        cfg = self.cfg
        NT, SEQ, NCH = cfg.NT, cfg.SEQ, cfg.NCH
        al = self.alloc
        qT = al("ml_qT", [128, 4, NT])
        kT = al("ml_kT", [128, 4, NT])
        self.dma(qT, self.FM[0:512, :].re("(h d) t -> d h t", h=4, d=128))
        self.dma(kT, self.FM[512:1024, :].re("(h d) t -> d h t", h=4, d=128))
        G = self.gates(l, "ml_")
        nw = al("ml_nw", [128, 1024])
        self.dma(nw, V(self.W['ml_norm_w'].ap[l, :].partition_broadcast(128), 'ml_norm_w'))
        yaT = [al(f"ml_yaT{i}", [128, 8, 128], BF16) for i in range(2)]
        CT = al("ml_CT", [128, 4, 257])
        self.memset(CT, 0.0)
        m0 = al("ml_m0", [128, 4])
        self.memset(m0, 0.0)
        ktm = al("ml_ktm", [128, 4, 128])
        vaug = al("ml_vaug", [128, 4, 257])
        self.memset(vaug[:, :, 256:257], 1.0)
        otm = al("ml_otm", [128, 1024])
        bc_ = al("ml_bc", [128, 4])
        Bv = al("ml_B", [128, 4])
        big = al("ml_big", [128, 4, 128])
        Rm = al("ml_Rm", [128, 4, 128])
        DT = al("ml_DT", [128, 4, 128])
        ST = al("ml_ST", [128, 4, 128])
        cm = al("ml_cm", [128, 4])
        X12 = al("ml_X12", [128, 12])
        inter = al("ml_inter", [128, 4])
        enm = al("ml_enm", [128, 4])
        lb = al("ml_lb", [128, 12])
        tmpc = al("ml_tmpc", [128, 257])
        nd = al("ml_nd", [128, 4, 257])
        dd = al("ml_dd", [128, 4])
        hh = al("ml_hh", [128, 4, 256])
        sq = al("ml_sq", [128, 4, 256])
        ss = al("ml_ss", [128, 4])
        wsig = al("ml_wsig", [128, 1024])
        ya = al("ml_ya", [128, 1024])
        wend = al("ml_wend", [128, 4])
        kw = al("ml_kw", [128, 4, 128])
        dec = al("ml_dec", [128, 4])
        CTs = al("ml_CTs", [128, 16, 257])
        CTn = al("ml_CTn", [128, 16, 257])
        qTz = al("ml_qTz", [128, 16, 128])
        kwz = al("ml_kwz", [128, 16, 128])
        Wm = al("ml_Wm", [128, 4, 16])
        X64 = al("ml_X64", [128, 4, 16])
        decb = al("ml_decb", [128, 4, 16])
        sm_sb = al("ml_sm", [16, 4])
        sn_sb = al("ml_snsb", [128, 16])
        mo = al("ml_mo", [16, 4])
        ps = self.ps

        def tile(mode, t0, L, ip, lf, part):
            M = self.masks(mode)
            rows = slice(t0, t0 + L)
            if part == 'head':
                self.dma(ktm[0:L], self.TM[rows, 0:512].re("p (h d) -> p h d", h=4, d=128))
                self.dma(vaug[0:L, :, 0:256], self.TM[rows, 512:1536].re("p (h e) -> p h e", h=4, e=256))
                self.dma(otm[0:L], self.TM[rows, 1536:2560])
                self.mm(ps[0][0:L, 0:4], M['tri'][0:L, 0:L], lf)
                self.cp(bc_[0:L], ps[0][0:L, 0:4])
                self.tt(Bv[0:L], ip, bc_[0:L], ALU.subtract)
                self.rowbc(ps[1], Bv[0:L], L, big)
                self.tt(Rm[0:L, :, 0:L], ps[1][0:L, 0:4 * L].re("p (h s) -> p h s", h=4, s=L),
                        M['neg'][0:L, 0:L].un(1).bc([L, 4, L]), ALU.add)
                self.red(cm[0:L], Rm[0:L, :, 0:L], ALU.max)
                self.tt(cm[0:L], cm[0:L], m0[0:L], ALU.max)
                self.ts(X12[0:L, 0:4], cm[0:L], -1.0, ALU.mult)
                self.tt(X12[0:L, 4:8], m0[0:L], cm[0:L], ALU.subtract)
                self.tt(X12[0:L, 8:12], bc_[0:L], cm[0:L], ALU.add)
                self.act(inter[0:L], X12[0:L, 4:8], AF.Exp)
                self.act(enm[0:L], X12[0:L, 8:12], AF.Exp, scale=-1.0)
                self.mm(ps[0][:, 16:28], M['last'][0:L, :], X12[0:L, :])
                self.cp(lb, ps[0][:, 16:28])
                self.rowbc(ps[1], X12[0:L, 0:4], L, big)
                self.tt(Rm[0:L, :, 0:L], ps[1][0:L, 0:4 * L].re("p (h s) -> p h s", h=4, s=L),
                        M['negT'][0:L, 0:L].un(1).bc([L, 4, L]), ALU.add)
                for h in range(4):
                    self.act(DT[0:L, h, 0:L], Rm[0:L, h, 0:L], AF.Exp, bias=Bv[0:L, h:h + 1])
                for h in range(4):
                    self.mm(ps[3][0:L, h * L:(h + 1) * L], kT[:, h, rows], qT[:, h, rows])
                self.tt(ST[0:L, :, 0:L], ps[3][0:L, 0:4 * L].re("p (h s) -> p h s", h=4, s=L), DT[0:L, :, 0:L], ALU.mult)
                for h in range(4):
                    pn = ps[4 + (h % 2) * 2]
                    pc = ps[5 + (h % 2) * 2]
                    self.mm(pn[0:L, 0:257], ST[0:L, h, 0:L], vaug[0:L, h, :])
                    if mode == 'p':
                        self.mm(pc[0:L, 0:257], qT[:, h, rows], CT[:, h, :])
                    else:
                        self.dma(CTs[:, :, 0:256], self.sCT[l, :, h, :, :].re("i d e -> d i e"))
                        self.dma(sn_sb, self.snT[l, h, :, :])
                        self.cp(CTs[:, :, 256], sn_sb)
                        self.tt(qTz, qT[:, h, rows].un(1).bc([128, 16, 128]), self.bm3, ALU.mult)
                        for i in range(16):
                            self.mm(pc[0:L, 0:257], qTz[:, i, :], CTs[:, i, :], start=(i == 0), stop=(i == 15))
                    self.act(tmpc[0:L], pc[0:L, 0:257], AF.Copy, scale=inter[0:L, h:h + 1])
                    self.tt(nd[0:L, h, :], pn[0:L, 0:257], tmpc[0:L], ALU.add)
                    if mode == 's':
                        if h == 0:
                            self.tt(Bv[0:L], Bv[0:L], lb[0:L, 0:4], ALU.add)
                            self.act(wend[0:L], Bv[0:L], AF.Exp)
                            self.tt(Wm, wend.un(2).bc([128, 4, 16]), self.blockind.un(1).bc([128, 4, 16]), ALU.mult)
                            self.tt(X64, inter.un(2).bc([128, 4, 16]), self.lastind.un(1).bc([128, 4, 16]), ALU.mult)
                            self.mm(ps[0][:, 64:128], self.C['ones'], X64.re("p h i -> p (h i)"))
                            self.cp(decb.re("p h i -> p (h i)"), ps[0][:, 64:128])
                        self.tt(kwz, ktm[:, h, :].un(1).bc([128, 16, 128]), Wm[:, h, :].un(2).bc([128, 16, 128]), ALU.mult)
                        for i in range(16):
                            pu = ps[self.rot('mlpu', 2)]
                            self.mm(pu[:, 0:257], kwz[:, i, :], vaug[:, h, :])
                            self.stt(CTn[:, i, :], CTs[:, i, :], decb[:, h, i:i + 1], pu[:, 0:257], ALU.mult, ALU.add)
                        self.dma(self.o_sCT[l, :, h, :, :].re("i d e -> d i e").k(h), CTn)
                if mode == 'p':
                    self.tt(Bv[0:L], Bv[0:L], lb[0:L, 0:4], ALU.add)
                    self.act(wend[0:L], Bv[0:L], AF.Exp)
                    self.tt(kw[0:L], ktm[0:L], wend[0:L].un(2).bc([L, 4, 128]), ALU.mult)
                    self.act(dec, lb[:, 4:8], AF.Exp)
                    for h in range(4):
                        pu = ps[self.rot('mlpu', 2)]
                        self.mm(pu[:, 0:257], kw[0:L, h, :], vaug[0:L, h, :])
                        self.stt(CT[:, h, :], CT[:, h, :], dec[:, h:h + 1], pu[:, 0:257], ALU.mult, ALU.add)
                    self.cp(m0, lb[:, 8:12])
                else:
                    self.mm(ps[0][0:16, 32:36], self.lastind, X12[:, 8:12])
                    self.cp(mo, ps[0][0:16, 32:36])
                    self.dma(self.o_sm[l], mo)

            elif part == 'tailpro':
                self.ts(dd[0:L], nd[0:L, :, 256], -1.0, ALU.mult)
                self.tt(dd[0:L], dd[0:L], nd[0:L, :, 256], ALU.max)
                self.tt(dd[0:L], dd[0:L], enm[0:L], ALU.max)
                self.recip(dd[0:L], dd[0:L])
                self.tt(hh[0:L], nd[0:L, :, 0:256], dd[0:L].un(2).bc([L, 4, 256]), ALU.mult)
                self.act(wsig[0:L], otm[0:L], AF.Sigmoid)
                self.tt(wsig[0:L], wsig[0:L], nw[0:L], ALU.mult)
            else:
                self.tt(sq[0:L], hh[0:L], hh[0:L], ALU.mult)
                self.red(ss[0:L], sq[0:L], ALU.add)
                self.ts(ss[0:L], ss[0:L], 1.0 / 256.0, ALU.mult, 1e-6, ALU.add)
                self.act(ss[0:L], ss[0:L], AF.Sqrt)
                self.recip(ss[0:L], ss[0:L])
                yav = ya[0:L].re("p (h e) -> p h e", h=4, e=256)
                self.tt(yav, hh[0:L], ss[0:L].un(2).bc([L, 4, 256]), ALU.mult)
                self.tt(ya[0:L], ya[0:L], wsig[0:L], ALU.mult)
                for j in range(8):
                    pt = (ps[2] if L == 64 else ps[0]) if (j * L) < 512 else ps[1]
                    c0 = (j * L) % 512
                    self.tr(pt[:, c0:c0 + L], ya[0:L, j * 128:(j + 1) * 128])
                yst = yaT[self.rot('ml_yst', 2)]
                if L == 64:
                    self.cp(yst[:, :, 0:64], ps[2][:, 0:512].re("p (j t) -> p j t", j=8, t=64), eng='act')
                else:
                    self.cp(yst[:, 0:4, :], ps[0][:, 0:512].re("p (j t) -> p j t", j=4, t=128), eng='act')
                    self.cp(yst[:, 4:8, :], ps[1][:, 0:512].re("p (j t) -> p j t", j=4, t=128), eng='act')
                self.dma(self.YA[:, rows].re("(j p) t -> p j t", p=128).k(t0), yst[:, :, 0:L])
        args = lambda c: ('p', c * 64, 64, G['p'][:, c, 0:4], G['p'][:, c, 4:8])
        tile(*args(0), 'head')
        for c in range(NCH):
            tile(*args(c), 'tailpro')
            tl = self.record(tile, *args(c), 'tail')
            hd_ = self.record(tile, *args(c + 1), 'head') if c + 1 < NCH else []
            self.play_merged(hd_, tl)
        self.dma(self.o_pCT[l].re("h d e -> d h e"), CT)
        self.dma(self.o_pm[l:l + 1, :], m0[0:1, :])
        self.dma(sm_sb, self.sm[l])
        self.mm(ps[0][:, 40:44], self.C['blockindT'][0:16, :], sm_sb)
        self.cp(m0, ps[0][:, 40:44])
        for part in ('head', 'tailpro', 'tail'):
            tile('s', SEQ, 128, G['s'][:, 0, 0:4], G['s'][:, 0, 4:8], part)

    def phaseGD(self, l):
        cfg = self.cfg
        NT, SEQ, NCH, NTT = cfg.NT, cfg.SEQ, cfg.NCH, cfg.NTT
        al = self.alloc
        W = self.W
        ps = self.ps
        qT = al("gd_qT", [128, 4, NT])
        kT = al("gd_kT", [128, 4, NT])
        cw = al("gd_cw", [128, 16, 4])
        self.dma(cw, W['gd_conv_wT'][l].re("(c p) j -> p c j", p=128))
        rn_p = al("gd_rnp", [64, NCH, 8])
        rn_s = al("gd_rns", [128, 1, 8])
        mark0 = self.off
        xpad_p = al("gd_xp", [128, SEQ + 3])
        xpad_s = al("gd_xs", [128, 16, 11])
        cv = al("gd_cv", [128, NT])
        sqb = al("gd_sqb", [128, NT])
        stg = [al(f"gd_stg{i}", [128, 128]) for i in range(3)]
        for ch in range(16):
            if ch < 4:
                dst = qT[:, ch, :]
            elif ch < 8:
                dst = kT[:, ch - 4, :]
            else:
                dst = cv
            self.conv_fm(self.FM[3072 + ch * 128:3072 + (ch + 1) * 128, :], self.gcT[l, ch * 128:(ch + 1) * 128, :, :],
                         cw[:, ch, :], cv, None, xpad_p, xpad_s)
            self.act(dst, cv, AF.Silu)
            if ch < 8:
                self.act(sqb, dst, AF.Square)
                for c in range(NCH):
                    self.mm(ps[0][0:64, c * 8 + ch:c * 8 + ch + 1], sqb[:, c * 64:(c + 1) * 64], self.C['ones'][:, 0:1])
                self.mm(ps[1][:, ch:ch + 1], sqb[:, SEQ:SEQ + 128], self.C['ones'][:, 0:1])
            if ch >= 4:
                for tt in range(NTT):
                    pt = ps[2 + self.rot('gdtp', 4)]
                    self.tr(pt[:, 0:128], dst[:, tt * 128:(tt + 1) * 128])
                    st = stg[self.rot('gdstg', 3)]
                    self.evac(st, pt[:, 0:128])
                    self.dma(self.KV[tt * 128:(tt + 1) * 128, (ch - 4) * 128:(ch - 3) * 128].k((tt, ch)), st)
        self.ts(rn_p.re("p c j -> p (c j)"), ps[0][0:64, 0:NCH * 8], 1e-6, ALU.add)
        self.act(rn_p, rn_p, AF.Sqrt)
        self.recip(rn_p, rn_p)
        self.ts(rn_s.re("p c j -> p (c j)"), ps[1][:, 0:8], 1e-6, ALU.add)
        self.act(rn_s, rn_s, AF.Sqrt)
        self.recip(rn_s, rn_s)
        self.P.barrier()
        self.off = mark0
        G = self.gates(l, "gd_")
        gnw = al("gd_gnw", [128, 256])
        self.dma(gnw, V(W['gd_norm_w'].ap[l, :].partition_broadcast(128), 'gd_norm_w'))
        ycT = [al(f"gd_ycT{i}", [128, 8, 128], BF16) for i in range(2)]
        S = al("gd_S", [128, 4, 256])
        self.memset(S, 0.0)
        ktm = al("gd_ktm", [128, 4, 128])
        vtm = al("gd_vtm", [128, 4, 256])
        ztm = al("gd_ztm", [128, 1024])
        gc = al("gd_gc", [128, 4])
        ngc = al("gd_ngc", [128, 4])
        eg = al("gd_eg", [128, 4])
        gl = al("gd_gl", [128, 4])
        egl = al("gd_egl", [128, 4])
        kds = al("gd_kds", [128, 4])
        big = al("gd_big", [128, 4, 128])
        Rm = al("gd_Rm", [128, 4, 128])
        DT = al("gd_DT", [128, 4, 128])
        QKT = al("gd_QKT", [128, 4, 128])
        KKD = al("gd_KKD", [128, 4, 128])
        Am = al("gd_Am", [128, 4, 128])
        TT = al("gd_TT", [128, 4, 128])
        sc4 = al("gd_sc4", [128, 4])
        bv = al("gd_bv", [128, 4, 256])
        bk = al("gd_bk", [128, 4, 128])
        upre = al("gd_upre", [128, 256])
        wT = al("gd_wT", [128, 4, 128])
        u = al("gd_u", [128, 4, 256])
        tmpo = al("gd_tmpo", [128, 256])
        o_ = al("gd_o", [128, 4, 256])
        qs = al("gd_qs", [128, 4])
        egq = al("gd_egq", [128, 4])
        kdec = al("gd_kdec", [128, 4, 128])
        ss = al("gd_ss", [128, 4])
        yc = al("gd_yc", [128, 1024])
        wz = al("gd_wz", [128, 1024])

        set0 = dict(gc=gc, ngc=ngc, eg=eg, gl=gl, egl=egl, kds=kds, big=big, Rm=Rm, DT=DT, QKT=QKT, TT=TT, sc4=sc4, bv=bv, bk=bk, qs=qs, egq=egq, kdec=kdec, ktm=ktm, vtm=vtm, ztm=ztm)

        def common(mode, t0, L, beta, g, rnq, rnk, need_A, bs=None):
            bs = set0 if bs is None else bs
            gc, ngc, big, Rm, DT, sc4 = (bs[n] for n in ('gc', 'ngc', 'big', 'Rm', 'DT', 'sc4'))
            M = self.masks(mode)
            rows = slice(t0, t0 + L)
            self.mm(ps[0][0:L, 0:4], M['tri'][0:L, 0:L], g)
            self.cp(gc[0:L], ps[0][0:L, 0:4])
            self.ts(ngc[0:L], gc[0:L], -1.0, ALU.mult)
            self.rowbc(ps[1], gc[0:L], L, big)
            self.tt(Rm[0:L, :, 0:L], ps[1][0:L, 0:4 * L].re("p (h s) -> p h s", h=4, s=L),
                    M['negT'][0:L, 0:L].un(1).bc([L, 4, L]), ALU.add)
            for h in range(4):
                self.act(DT[0:L, h, 0:L], Rm[0:L, h, 0:L], AF.Exp, bias=ngc[0:L, h:h + 1])
            if need_A:
                for h in range(4):
                    self.mm(ps[2][0:L, h * L:(h + 1) * L], kT[:, h, rows], kT[:, h, rows])
                self.tt(KKD[0:L, :, 0:L], ps[2][0:L, 0:4 * L].re("p (h s) -> p h s", h=4, s=L), DT[0:L, :, 0:L], ALU.mult)
                self.tt(KKD[0:L, :, 0:L], KKD[0:L, :, 0:L], M['strictT'][0:L, 0:L].un(1).bc([L, 4, L]), ALU.mult)
                self.tt(KKD[0:L, :, 0:L], KKD[0:L, :, 0:L], rnk.un(2).bc([L, 4, L]), ALU.mult)
                for h in range(4):
                    self.tr(ps[3][0:L, h * L:(h + 1) * L], KKD[0:L, h, 0:L])
                self.tt(sc4[0:L], beta, rnk, ALU.mult)
                self.tt(Am[0:L, :, 0:L], ps[3][0:L, 0:4 * L].re("p (h s) -> p h s", h=4, s=L),
                        sc4[0:L].un(2).bc([L, 4, L]), ALU.mult)

        for c in range(NCH):
            common('p', c * 64, 64, G['p'][:, c, 8:12], G['p'][:, c, 12:16], rn_p[:, c, 0:4], rn_p[:, c, 4:8], True)
            self.dma(self.Ascr[c * 4:(c + 1) * 4, :, :].re("h t s -> t h s").k(c), Am[0:64, :, 0:64])
        NP = NCH * 4
        mark1 = self.off
        Ab = al("gd_Ab", [NP, 64, 64])
        Tt = al("gd_Tt", [NP, 64, 64])
        prod = al("gd_prod", [NP, 32, 64])
        rr = al("gd_rr", [NP, 64])
        self.emit_op('sp', lambda h: h.dma_start(out=Ab.ap, in_=self.Ascr.ap), reads=[("Ascr", c) for c in range(NCH)],
                  writes=[Ab.key], dma=True)
        self.memset(Tt, 0.0)
        self.memset(Tt[:, 0:1, 0], 1.0)
        for t in range(1, 64):
            for jh in range(2):
                js = slice(jh * 32, jh * 32 + 32)
                self.tt(prod[:, :, 0:t], Tt[:, js, 0:t], Ab[:, t, 0:t].un(1).bc([NP, 32, t]), ALU.mult)
                self.red(rr[:, js], prod[:, :, 0:t], ALU.add)
            self.ts(Tt[:, :, t], rr, -1.0, ALU.mult)
            self.ts(Tt[:, t:t + 1, t], Tt[:, t:t + 1, t], 1.0, ALU.add)
        self.dma(V(self.Tscr.ap, 'Tscr_all'), Tt)
        self.P.barrier()
        self.off = mark1
        def pass2(mode, t0, L, beta, g, rnq, rnk, c, part, bs):
            M = self.masks(mode)
            rows = slice(t0, t0 + L)
            (gc, ngc, eg, gl, egl, kds, big, Rm, DT, QKT, TT, sc4, bv, bk, qs, egq, kdec, ktm, vtm, ztm) = (bs[n] for n in SETN)
            if part == 'pre':
                common(mode, t0, L, beta, g, rnq, rnk, mode == 's', bs)
                self.dma(ktm[0:L], self.KV[rows, 0:512].re("p (h d) -> p h d", h=4, d=128))
                self.dma(vtm[0:L], self.KV[rows, 512:1536].re("p (h e) -> p h e", h=4, e=256))
                self.dma(ztm[0:L], self.TM[rows, 2560:3584])
                if mode == 'p':
                    self.dma(TT[0:64, :, 0:64], V(self.Tscr.ap[c * 4:(c + 1) * 4, :, :].rearrange("h s t -> s h t"), 'Tscr_all'))
                else:
                    self.ts(Nm, Am, -1.0, ALU.mult)
                    for h in range(4):
                        self.tr(ps[2][:, h * 128:(h + 1) * 128], Nm[:, h, :])
                    self.cp(Mm, ps[2][:, :].re("p (h s) -> p h s", h=4, s=128))
                    for h in range(4):
                        self.mm(ps[3][:, h * 128:(h + 1) * 128], Mm[:, h, :], Nm[:, h, :])
                        self.mm(ps[4][:, h * 128:(h + 1) * 128], Nm[:, h, :], Mm[:, h, :])
                    self.cp(N2, ps[3][:, :].re("p (h s) -> p h s", h=4, s=128))
                    self.cp(M2, ps[4][:, :].re("p (h s) -> p h s", h=4, s=128), eng='act')
                    for h in range(4):
                        self.mm(ps[5][:, h * 128:(h + 1) * 128], M2[:, h, :], N2[:, h, :])
                    self.tt(N4, ps[5][:, :].re("p (h s) -> p h s", h=4, s=128), idb, ALU.add)
                    self.tt(N2, N2, idb, ALU.add)
                    self.tt(Mm, Mm, idb, ALU.add)
                    for h in range(4):
                        self.mm(ps[2][:, h * 128:(h + 1) * 128], N2[:, h, :], Mm[:, h, :])
                    self.cp(Q1, ps[2][:, :].re("p (h s) -> p h s", h=4, s=128))
                    for h in range(4):
                        self.mm(ps[3][:, h * 128:(h + 1) * 128], N4[:, h, :], Q1[:, h, :])
                    self.cp(TT, ps[3][:, :].re("p (h s) -> p h s", h=4, s=128))
                for h in range(4):
                    self.mm(ps[4][0:L, h * L:(h + 1) * L], kT[:, h, rows], qT[:, h, rows])
                self.tt(QKT[0:L, :, 0:L], ps[4][0:L, 0:4 * L].re("p (h s) -> p h s", h=4, s=L), DT[0:L, :, 0:L], ALU.mult)
                self.tt(QKT[0:L, :, 0:L], QKT[0:L, :, 0:L], rnk.un(2).bc([L, 4, L]), ALU.mult)
                self.act(eg[0:L], gc[0:L], AF.Exp)
                self.mm(ps[0][:, 16:20], M['last'][0:L, :], gc[0:L])
                self.cp(gl, ps[0][:, 16:20])
                self.act(egl, gl, AF.Exp)
                self.tt(kds[0:L], gl[0:L], gc[0:L], ALU.subtract)
                self.act(kds[0:L], kds[0:L], AF.Exp)
                self.tt(kds[0:L], kds[0:L], rnk, ALU.mult)
                self.tt(sc4[0:L], beta, eg[0:L], ALU.mult)
                self.tt(sc4[0:L], sc4[0:L], rnk, ALU.mult)
                self.ts(qs[0:L], rnq, cfg.DK ** -0.5, ALU.mult)
                self.tt(egq[0:L], eg[0:L], qs[0:L], ALU.mult)
                self.tt(bv[0:L], vtm[0:L], beta.un(2).bc([L, 4, 256]), ALU.mult)
                self.tt(bk[0:L], ktm[0:L], sc4[0:L].un(2).bc([L, 4, 128]), ALU.mult)
                self.tt(kdec[0:L], ktm[0:L], kds[0:L].un(2).bc([L, 4, 128]), ALU.mult)
                if mode == 's':
                    self.tt(X64, gc.un(2).bc([128, 4, 16]), self.lastind.un(1).bc([128, 4, 16]), ALU.mult)
                    self.mm(ps[0][:, 64:128], self.C['ones'], X64.re("p h i -> p (h i)"))
                    self.act(eglb.re("p h i -> p (h i)"), ps[0][:, 64:128], AF.Exp)
            elif part == 'dep':
                for h in range(4):
                    self.mm(ps[5][0:L, 0:256], TT[0:L, h, 0:L], bv[0:L, h, :])
                    self.cp(upre[0:L], ps[5][0:L, 0:256], eng='act')
                    self.mm(ps[6][:, 0:L], bk[0:L, h, :], TT[0:L, h, 0:L])
                    self.cp(wT[:, h, 0:L], ps[6][:, 0:L])
                    if mode == 'p':
                        self.mm(ps[7][0:L, 0:256], wT[:, h, 0:L], S[:, h, :])
                    else:
                        self.dma(Ss, self.gS[l, :, h, :, :].re("i d e -> d i e"))
                        self.tt(wTz, wT[:, h, :].un(1).bc([128, 16, 128]), self.bm3, ALU.mult)
                        for i in range(16):
                            self.mm(ps[7][0:L, 0:256], wTz[:, i, :], Ss[:, i, :], start=(i == 0), stop=(i == 15))
                    self.tt(u[0:L, h, :], upre[0:L], ps[7][0:L, 0:256], ALU.subtract)
                    if mode == 'p':
                        self.mm(ps[5][0:L, 256:512], qT[:, h, rows], S[:, h, :])
                    else:
                        self.tt(qTz, qT[:, h, rows].un(1).bc([128, 16, 128]), self.bm3, ALU.mult)
                        for i in range(16):
                            self.mm(ps[5][0:L, 256:512], qTz[:, i, :], Ss[:, i, :], start=(i == 0), stop=(i == 15))
                    self.act(tmpo[0:L], ps[5][0:L, 256:512], AF.Copy, scale=egq[0:L, h:h + 1])
                    self.mm(ps[6][0:L, 256:512], QKT[0:L, h, 0:L], u[0:L, h, :])
                    self.stt(o_[0:L, h, :], ps[6][0:L, 256:512], qs[0:L, h:h + 1], tmpo[0:L], ALU.mult, ALU.add)
                    if mode == 'p':
                        self.mm(ps[7][:, 256:512], kdec[0:L, h, :], u[0:L, h, :])
                        self.stt(S[:, h, :], S[:, h, :], egl[:, h:h + 1], ps[7][:, 256:512], ALU.mult, ALU.add)
                    else:
                        self.tt(kdz, kdec[:, h, :].un(1).bc([128, 16, 128]), self.blockind.un(2).bc([128, 16, 128]), ALU.mult)
                        for i in range(16):
                            pu = ps[2 + self.rot('gdpu', 2)]
                            self.mm(pu[:, 0:256], kdz[:, i, :], u[:, h, :])
                            self.stt(Ss[:, i, :], Ss[:, i, :], eglb[:, h, i:i + 1], pu[:, 0:256], ALU.mult, ALU.add)
                        self.dma(self.o_sgS[l, :, h, :, :].re("i d e -> d i e").k(h), Ss)
            elif part == 'tailpro':
                ycv = yc[0:L].re("p (h e) -> p h e", h=4, e=256)
                self.tt(ycv, o_[0:L], o_[0:L], ALU.mult)
                self.red(ss[0:L], ycv, ALU.add)
                self.ts(ss[0:L], ss[0:L], 1.0 / 256.0, ALU.mult, 1e-6, ALU.add)
                self.act(ss[0:L], ss[0:L], AF.Sqrt)
                self.recip(ss[0:L], ss[0:L])
                self.act(wz[0:L], ztm[0:L], AF.Silu)
                self.tt(wz[0:L].re("p (h e) -> p h e", h=4, e=256), wz[0:L].re("p (h e) -> p h e", h=4, e=256),
                        gnw[0:L].un(1).bc([L, 4, 256]), ALU.mult)
                self.tt(ycv, o_[0:L], ss[0:L].un(2).bc([L, 4, 256]), ALU.mult)
            else:
                self.tt(yc[0:L], yc[0:L], wz[0:L], ALU.mult)
                for j in range(8):
                    pt = (ps[2] if L == 64 else ps[0]) if (j * L) < 512 else ps[1]
                    c0 = (j * L) % 512
                    self.tr(pt[:, c0:c0 + L], yc[0:L, j * 128:(j + 1) * 128])
                yst = ycT[self.rot('gd_yst', 2)]
                if L == 64:
                    self.cp(yst[:, :, 0:64], ps[2][:, 0:512].re("p (j t) -> p j t", j=8, t=64), eng='act')
                else:
                    self.cp(yst[:, 0:4, :], ps[0][:, 0:512].re("p (j t) -> p j t", j=4, t=128), eng='act')
                    self.cp(yst[:, 4:8, :], ps[1][:, 0:512].re("p (j t) -> p j t", j=4, t=128), eng='act')
                self.dma(self.YC[:, rows].re("(j p) t -> p j t", p=128).k(t0), yst[:, :, 0:L])

        SETN = ('gc', 'ngc', 'eg', 'gl', 'egl', 'kds', 'big', 'Rm', 'DT', 'QKT', 'TT', 'sc4', 'bv', 'bk', 'qs', 'egq', 'kdec', 'ktm', 'vtm', 'ztm')
        set1 = {'gc': al('gd2_gc', [128, 4]), 'ngc': al('gd2_ngc', [128, 4]), 'eg': al('gd2_eg', [128, 4]), 'gl': al('gd2_gl', [128, 4]), 'egl': al('gd2_egl', [128, 4]), 'kds': al('gd2_kds', [128, 4]), 'big': al('gd2_big', [128, 4, 128]), 'Rm': al('gd2_Rm', [128, 4, 128]), 'DT': al('gd2_DT', [128, 4, 128]), 'QKT': al('gd2_QKT', [128, 4, 128]), 'TT': al('gd2_TT', [128, 4, 128]), 'sc4': al('gd2_sc4', [128, 4]), 'bv': al('gd2_bv', [128, 4, 256]), 'bk': al('gd2_bk', [128, 4, 128]), 'qs': al('gd2_qs', [128, 4]), 'egq': al('gd2_egq', [128, 4]), 'kdec': al('gd2_kdec', [128, 4, 128]), 'ktm': al('gd2_ktm', [128, 4, 128]), 'vtm': al('gd2_vtm', [128, 4, 256]), 'ztm': al('gd2_ztm', [128, 1024])}
        sets = (set0, set1)
        pa = lambda c: ('p', c * 64, 64, G['p'][:, c, 8:12], G['p'][:, c, 12:16], rn_p[:, c, 0:4], rn_p[:, c, 4:8], c)
        pass2(*pa(0), 'pre', sets[0])
        for c in range(NCH):
            A = []
            for part in ('dep', 'tailpro', 'tail'):
                A += self.record(pass2, *pa(c), part, sets[c % 2])
            Bq = self.record(pass2, *pa(c + 1), 'pre', sets[(c + 1) % 2]) if c + 1 < NCH else []
            self.play_merged(A, Bq)
        self.dma(self.o_pgS[l].re("h d e -> d h e"), S)
        self.P.barrier()
        self.off = mark1
        Ss = al("gd_Ss", [128, 16, 256])
        qTz = al("gd_qTz", [128, 16, 128])
        wTz = qTz
        kdz = al("gd_kdz", [128, 16, 128])
        Nm = al("gd_Nm", [128, 4, 128])
        Mm = al("gd_Mm", [128, 4, 128])
        N2 = al("gd_N2", [128, 4, 128])
        M2 = al("gd_M2", [128, 4, 128])
        N4 = al("gd_N4", [128, 4, 128])
        Q1 = al("gd_Q1", [128, 4, 128])
        X64 = al("gd_X64", [128, 4, 16])
        eglb = al("gd_eglb", [128, 4, 16])
        idb = self.ident.un(1).bc([128, 4, 128])

        for part in ('pre', 'dep', 'tailpro', 'tail'):
            pass2('s', SEQ, 128, G['s'][:, 0, 8:12], G['s'][:, 0, 12:16], rn_s[:, 0, 0:4], rn_s[:, 0, 4:8], None, part, set0)

    def phaseC1(self, l):
        cfg = self.cfg
        NT, D = cfg.NT, cfg.D
        al = self.alloc
        ys = []
        for nm, src in (('a', self.YA), ('b', self.YB), ('c', self.YC)):
            t = al("c1_y" + nm, [128, 8, NT], BF16)
            self.dma(t, src.re("(j p) t -> p j t", p=128))
            ys.append(t)
        wts = [[al(f"c1_w{x}{i}", [128, 8, 128], BF16) for i in range(2)] for x in range(3)]
        sg = [[al(f"c1_sg{x}{i}", [128, NT], BF16) for i in range(2)] for x in range(3)]
        acc = [al(f"c1_acc{i}", [128, 512]) for i in range(2)]
        tmpb = [al(f"c1_tmp{i}", [128, 512]) for i in range(2)]
        mt = [al(f"c1_mt{i}", [128, NT], BF16) for i in range(2)]
        wn = ('w_up_mlstm', 'w_up_rglru', 'w_up_gdn')
        for j in range(D // 128):
            b = j % 2
            for x in range(3):
                self.dma(wts[x][b], self.W[wn[x]][l, :, j * 128:(j + 1) * 128].re("(k p) n -> p k n", p=128), eng='pool')
                self.dma(sg[x][b], self.MG[x * D + j * 128:x * D + (j + 1) * 128, :])
            for (t0, n) in cfg.groups:
                pss = []
                for x in range(3):
                    ps = self.ps[self.rot('c1ps', 6)]
                    for k in range(8):
                        self.mm(ps[:, 0:n], wts[x][b][:, k, :], ys[x][:, k, t0:t0 + n], start=(k == 0), stop=(k == 7))
                    pss.append(ps)
                a_ = acc[self.rot('c1acc', 2)]
                tm_ = tmpb[self.rot('c1tmp', 2)]
                self.tt(a_[:, 0:n], pss[0][:, 0:n], sg[0][b][:, t0:t0 + n], ALU.mult)
                self.tt(tm_[:, 0:n], pss[1][:, 0:n], sg[1][b][:, t0:t0 + n], ALU.mult)
                self.tt(a_[:, 0:n], a_[:, 0:n], tm_[:, 0:n], ALU.add)
                self.tt(tm_[:, 0:n], pss[2][:, 0:n], sg[2][b][:, t0:t0 + n], ALU.mult)
                self.tt(mt[b][:, t0:t0 + n], a_[:, 0:n], tm_[:, 0:n], ALU.add)
            self.dma(self.MT[j * 128:(j + 1) * 128, :].k(j), mt[b])

    def layernorm(self, z, g, b, out, tmp, s):
        D = self.cfg.D
        self.red(s[:, 0:1], z, ALU.add)
        self.ts(s[:, 0:1], s[:, 0:1], -1.0 / D, ALU.mult)
        self.act(z, z, AF.Identity, bias=s[:, 0:1])
        self.act(tmp, z, AF.Square)
        self.red(s[:, 1:2], tmp, ALU.add)
        self.ts(s[:, 1:2], s[:, 1:2], 1.0 / D, ALU.mult, 1e-5, ALU.add)
        self.act(s[:, 1:2], s[:, 1:2], AF.Sqrt)
        self.recip(s[:, 1:2], s[:, 1:2])
        self.act(z, z, AF.Copy, scale=s[:, 1:2])
        self.tt(z, z, g, ALU.mult, eng='pool')
        self.tt(out, z, b, ALU.add, eng='pool')

    def load_w_bf16(self, dst, src2d, kc):
        N = src2d.ap.shape[1]
        for c0 in range(0, N, 512):
            n = min(512, N - c0)
            self.dma(dst[:, :, c0:c0 + n].k(c0), src2d[:, c0:c0 + n].re("(k p) n -> p k n", p=128), eng='pool')

    def phaseC2(self, l):
        cfg = self.cfg
        NT, D, KC, NTT = cfg.NT, cfg.D, cfg.KC, cfg.NTT
        al = self.alloc
        wout = al("c2_wout", [128, KC, D], BF16)
        self.load_w_bf16(wout, self.W['w_out'][l], KC)
        g = al("c2_g", [128, D])
        b = al("c2_b", [128, D])
        self.dma(g, V(self.W['ln'].ap[l, 0, :].partition_broadcast(128), 'ln'))
        self.dma(b, V(self.W['ln'].ap[l, 1, :].partition_broadcast(128), 'ln'))
        mtl = [al(f"c2_mt{i}", [128, KC, 128], BF16) for i in range(2)]
        xt = [al(f"c2_x{i}", [128, D]) for i in range(2)]
        z = [al(f"c2_z{i}", [128, D]) for i in range(2)]
        tmp = al("c2_tmp", [128, D])
        x1 = [al(f"c2_x1{i}", [128, D]) for i in range(2)]
        s = [al(f"c2_s{i}", [128, 4]) for i in range(2)]
        rkeys = [c0 for c0 in range(0, D, 512)]
        def front(tt):
            bb = tt % 2
            self.dma(mtl[bb], self.MT[:, tt * 128:(tt + 1) * 128].re("(k p) t -> p k t", p=128))
            self.dma(xt[bb], self.x_src(l, tt))
            for cb in range(D // 512):
                ps = self.ps[self.rot('c2ps', 8)]
                for k in range(KC):
                    self.mm(ps, mtl[bb][:, k, :], wout[:, k, cb * 512:(cb + 1) * 512].k(cb * 512), start=(k == 0), stop=(k == KC - 1))
                self.stt(z[bb][:, cb * 512:(cb + 1) * 512], xt[bb][:, cb * 512:(cb + 1) * 512], cfg.alpha, ps, ALU.mult, ALU.add)

        def back(tt):
            bb = tt % 2
            self.layernorm(z[bb], g, b, x1[bb], tmp, s[bb])
            self.dma(self.X1[tt * 128:(tt + 1) * 128, :].k(tt), x1[bb])

        front(0)
        for tt in range(NTT):
            fr = self.record(front, tt + 1) if tt + 1 < NTT else []
            bk = self.record(back, tt)
            self.play_merged(fr, bk)

    def phaseFFN(self, l):
        cfg = self.cfg
        NT, D, KC, NTT = cfg.NT, cfg.D, cfg.KC, cfg.NTT
        al = self.alloc
        moe = (l % 2 == 1)
        j = l // 2
        W = self.W
        if moe:
            NE, DF = cfg.NE, cfg.DFFE
            w1 = lambda e: W['moe_w1'][j, e]
            w3 = lambda e: W['moe_w3'][j, e]
            w2 = lambda e: W['moe_w2'][j, e]
        else:
            NE, DF = 1, cfg.DFF
            w1 = lambda e: W['ffn_w1'][j]
            w3 = lambda e: W['ffn_w3'][j]
            w2 = lambda e: W['ffn_w2'][j]
        x1T = al("ff_x1T", [128, KC, NT], BF16)
        Gbc = None
        if moe:
            Gbc = al("ff_Gbc", [128, NE, NT], BF16)
            rt = al("ff_rt", [128, KC, NE])
            if 'rtdma' not in MOE_DBG:
                self.dma(rt, W['moe_router'][j].re("(k p) e -> p k e", p=128))
            xf = al("ff_xf", [128, KC, 128])
            lg = al("ff_lg", [128, NE])
            lg2 = al("ff_lg2", [128, NE])
            eq1 = al("ff_eq1", [128, NE])
            eq2 = al("ff_eq2", [128, NE])
            gts = al("ff_gts", [128, NE])
            sc = al("ff_sc", [128, 4])
            gtmp = al("ff_gtmp", [128, NE, 128])
        xt = [al(f"ff_xs{i}", [128, D]) for i in range(2)]
        for tt in range(NTT):
            st = xt[tt % 2]
            self.dma(st, self.X1[tt * 128:(tt + 1) * 128, :].k(tt))
            for q in range((KC + 3) // 4):
                nk = min(4, KC - q * 4)
                ps = self.ps[self.rot('xTps', 2)]
                for jj in range(nk):
                    k = q * 4 + jj
                    self.tr(ps[:, jj * 128:(jj + 1) * 128], st[:, k * 128:(k + 1) * 128])
                self.evac(x1T[:, q * 4:q * 4 + nk, tt * 128:(tt + 1) * 128].k(tt), ps[:, 0:nk * 128].re("p (a b) -> p a b", a=nk, b=128))
                if moe and 'xf' not in MOE_DBG:
                    self.cp(xf[:, q * 4:q * 4 + nk, :], ps[:, 0:nk * 128].re("p (a b) -> p a b", a=nk, b=128), eng='act')
            if moe and 'router' in MOE_DBG:
                self.memset(gts, 0.125)
            if moe and 'gbc' in MOE_DBG:
                self.memset(Gbc[:, :, tt * 128:(tt + 1) * 128].k(tt), 1.0)
            if moe and 'router' not in MOE_DBG:
                pl = self.ps[2]
                for k in range(KC):
                    self.mm(pl[:, 0:NE], xf[:, k, :], rt[:, k, :], start=(k == 0), stop=(k == KC - 1))
                self.cp(lg, pl[:, 0:NE])
                self.red(sc[:, 0:1], lg, ALU.max)
                self.ts(eq1, lg, sc[:, 0:1], ALU.is_equal)
                self.stt(lg2, eq1, NEG, lg, ALU.mult, ALU.add)
                self.red(sc[:, 1:2], lg2, ALU.max)
                self.ts(eq2, lg2, sc[:, 1:2], ALU.is_equal)
                self.tt(sc[:, 2:3], sc[:, 1:2], sc[:, 0:1], ALU.subtract)
                self.act(sc[:, 2:3], sc[:, 2:3], AF.Exp)
                self.ts(sc[:, 3:4], sc[:, 2:3], 1.0, ALU.add)
                self.recip(sc[:, 3:4], sc[:, 3:4])
                self.tt(sc[:, 2:3], sc[:, 2:3], sc[:, 3:4], ALU.mult)
                self.ts(eq1, eq1, sc[:, 3:4], ALU.mult)
                self.stt(gts, eq2, sc[:, 2:3], eq1, ALU.mult, ALU.add)
            if moe and 'gbc' not in MOE_DBG:
                self.cp(gtmp, gts.un(2).bc([128, NE, 128]))
                for e0 in range(0, NE, 4):
                    pg = self.ps[3 + self.rot('gbps', 2)]
                    ne = min(4, NE - e0)
                    for e in range(ne):
                        self.mm(pg[:, e * 128:(e + 1) * 128], gtmp[:, e0 + e, :], self.ident)
                    self.cp(Gbc[:, e0:e0 + ne, tt * 128:(tt + 1) * 128].k(tt), pg[:, 0:ne * 128].re("p (a b) -> p a b", a=ne, b=128), eng='act')
        WT = 256 if moe else 512
        w1b = [al(f"ff_w1b{i}", [128, KC, WT], BF16) for i in range(2)]
        w3b = [al(f"ff_w3b{i}", [128, KC, WT], BF16) for i in range(2)]
        sil = [al(f"ff_sil{i}", [128, 512]) for i in range(2)]
        hb = [al(f"ff_hb{i}", [128, 512]) for i in range(2)]
        hst = [al(f"ff_hst{i}", [128, 512], BF16) for i in range(3)]
        xkeys = lambda t0, n: [("ff_x1T", tt) for tt in range(t0 // 128, (t0 + n + 127) // 128)]
        gkeys = lambda t0, n: [("ff_Gbc", tt) for tt in range(t0 // 128, (t0 + n + 127) // 128)]
        for e in range(1 if 'ne1' in MOE_DBG else NE):
            for c0 in range(0, DF, WT):
                nb = min(WT, DF - c0)
                bi = self.rot('ffw', 2)
                self.dma(w1b[bi][:, :, 0:nb], w1(e)[:, c0:c0 + nb].re("(k p) n -> p k n", p=128), eng='pool')
                self.dma(w3b[bi][:, :, 0:nb], w3(e)[:, c0:c0 + nb].re("(k p) n -> p k n", p=128), eng='pool')
                for sub in range(nb // 128):
                    row0 = e * DF + c0 + sub * 128
                    for (t0, n) in cfg.groups:
                        p1 = self.ps[self.rot('ffps', 8)]
                        p3 = self.ps[self.rot('ffps', 8)]
                        for (pp_, wb_) in ((p1, w1b[bi]), (p3, w3b[bi])):
                            for k in range(KC):
                                self.emit_op('pe', lambda h, pp_=pp_, wb_=wb_, k=k, t0=t0, n=n, sub=sub: h.matmul(
                                    pp_.ap[:, 0:n], lhsT=wb_.ap[:, k, sub * 128:(sub + 1) * 128], rhs=x1T.ap[:, k, t0:t0 + n],
                                    start=(k == 0), stop=(k == KC - 1)), reads=[wb_.key] + xkeys(t0, n), writes=[pp_.key], accum=True)
                        si = sil[self.rot('ffsil', 2)]
                        self.act(si[:, 0:n], p1[:, 0:n], AF.Silu)
                        ho = hst[self.rot('ffhst', 3)]
                        if moe:
                            hh_ = hb[self.rot('ffhb', 2)]
                            self.tt(hh_[:, 0:n], si[:, 0:n], p3[:, 0:n], ALU.mult)
                            geng = 'dve' if 'poolmul' in MOE_DBG else 'pool'
                            hnd = (lambda h: h)
                            self.emit_op(geng, lambda h, ho=ho, hh_=hh_, e=e, t0=t0, n=n: h.tensor_tensor(
                                out=ho.ap[:, 0:n], in0=hh_.ap[:, 0:n], in1=Gbc.ap[:, e, t0:t0 + n], op=ALU.mult),
                                reads=[hh_.key] + gkeys(t0, n), writes=[ho.key])
                        else:
                            self.tt(ho[:, 0:n], si[:, 0:n], p3[:, 0:n], ALU.mult)
                        self.dma(self.HT[row0:row0 + 128, t0:t0 + n].k((row0, t0)), ho[:, 0:n])
        self.P.barrier()
        self.off = self.mark
        if self.stop_after == (l, 'phaseFFN_s1'):
            self.stopped = True
            return
        KT = NE * DF // 128
        SEG = 8
        facc = al("ff_facc", [128, NTT, 1024])
        w2b = [al(f"ff_w2b{i}", [128, SEG, 1024], BF16) for i in range(2)]
        hTb = [al(f"ff_hTb{i}", [128, SEG, NT], BF16) for i in range(2)]
        CH = min(1024, D)
        CW = min(512, CH)
        NCI = CH // CW
        for half in range(D // CH):
            first = True
            for e in range(NE):
                kpe = DF // 128
                for k0 in range(0, kpe, SEG):
                    nk = min(SEG, kpe - k0)
                    bi = self.rot('ffw2', 2)
                    r0 = k0 * 128
                    for ci in range(NCI):
                        cc = ci * CW
                        self.dma(w2b[bi][:, 0:nk, cc:cc + CW].k(cc),
                                 w2(e)[r0:r0 + nk * 128, half * CH + cc:half * CH + cc + CW].re("(k p) n -> p k n", p=128), eng='pool')
                    self.dma(hTb[bi][:, 0:nk, :], self.HT[e * DF + r0:e * DF + r0 + nk * 128, :].re("(k p) t -> p k t", p=128))
                    for tt in range(NTT):
                        for ci in range(NCI):
                            pq = self.ps[self.rot('ff2ps', 8)]
                            for k in range(nk):
                                self.mm(pq[:, 0:CW], hTb[bi][:, k, tt * 128:(tt + 1) * 128], w2b[bi][:, k, ci * CW:(ci + 1) * CW].k(ci * CW),
                                        start=(k == 0), stop=(k == nk - 1))
                            dstv = facc[:, tt, ci * CW:(ci + 1) * CW].k((tt, ci))
                            if first:
                                self.evac(dstv, pq[:, 0:CW])
                            else:
                                self.tt(dstv, dstv, pq[:, 0:CW], ALU.add)
                    first = False
            self.emit_op('sp', lambda h, half=half: h.dma_start(
                out=self.FF.ap[:, half * CH:(half + 1) * CH].rearrange("(t p) c -> p t c", p=128), in_=facc.ap[:, :, 0:CH]),
                reads=[("ff_facc", (tt, ci)) for tt in range(NTT) for ci in range(NCI)], writes=[("FF", half)], dma=True)

    def phaseF(self, l):
        cfg = self.cfg
        NT, D, KC, NTT, PD = cfg.NT, cfg.D, cfg.KC, cfg.NTT, cfg.PD
        al = self.alloc
        pgw = al("f_pgw", [128, KC, D], BF16)
        self.load_w_bf16(pgw, self.W['ple_gate_w'][l], KC)
        plw = al("f_plw", [128, PD // 128, D], BF16)
        self.load_w_bf16(plw, self.W['ple_w'][l], PD // 128)
        g = al("f_g", [128, D])
        b = al("f_b", [128, D])
        self.dma(g, V(self.W['ln'].ap[l, 2, :].partition_broadcast(128), 'ln'))
        self.dma(b, V(self.W['ln'].ap[l, 3, :].partition_broadcast(128), 'ln'))
        x1 = [al(f"f_x1{i}", [128, D]) for i in range(2)]
        ff = [al(f"f_ff{i}", [128, D]) for i in range(2)]
        pt = [al(f"f_p{i}", [128, PD]) for i in range(2)]
        x1T = [al(f"f_x1T{i}", [128, KC, 128], BF16) for i in range(2)]
        pT = [al(f"f_pT{i}", [128, PD // 128, 128], BF16) for i in range(2)]
        z = [al(f"f_z{i}", [128, D]) for i in range(2)]
        sgt = [al(f"f_sg{i}", [128, 512]) for i in range(2)]
        tmp = al("f_tmp", [128, D])
        out = [al(f"f_out{i}", [128, D]) for i in range(2)]
        s = [al(f"f_s{i}", [128, 4]) for i in range(2)]
        def front(tt):
            bb = tt % 2
            self.dma(x1[bb], self.X1[tt * 128:(tt + 1) * 128, :].k(tt))
            self.emit_op('sp', lambda h, bb=bb, tt=tt: h.dma_start(out=ff[bb].ap, in_=self.FF.ap[tt * 128:(tt + 1) * 128, :]),
                      reads=[("FF", hf) for hf in range(max(1, D // 1024))], writes=[ff[bb].key], dma=True)
            if tt < NTT - 1:
                self.dma(pt[bb], self.pp[l, tt * 128:(tt + 1) * 128, :])
            else:
                self.dma(pt[bb], self.psm[l])
            for q in range((KC + 3) // 4):
                nk = min(4, KC - q * 4)
                ps = self.ps[self.rot('xTps', 2)]
                for jj in range(nk):
                    k = q * 4 + jj
                    self.tr(ps[:, jj * 128:(jj + 1) * 128], x1[bb][:, k * 128:(k + 1) * 128])
                self.evac(x1T[bb][:, q * 4:q * 4 + nk, :], ps[:, 0:nk * 128].re("p (a b) -> p a b", a=nk, b=128))
            ps = self.ps[self.rot('xTps', 2)]
            for jj in range(PD // 128):
                self.tr(ps[:, jj * 128:(jj + 1) * 128], pt[bb][:, jj * 128:(jj + 1) * 128])
            self.evac(pT[bb], ps[:, 0:PD].re("p (a b) -> p a b", a=PD // 128, b=128))
            for cb in range(D // 512):
                pg = self.ps[2 + self.rot('fps', 6)]
                pl = self.ps[2 + self.rot('fps', 6)]
                for k in range(KC):
                    self.mm(pg, x1T[bb][:, k, :], pgw[:, k, cb * 512:(cb + 1) * 512].k(cb * 512), start=(k == 0), stop=(k == KC - 1))
                for k in range(PD // 128):
                    self.mm(pl, pT[bb][:, k, :], plw[:, k, cb * 512:(cb + 1) * 512].k(cb * 512), start=(k == 0), stop=(k == PD // 128 - 1))
                sg_ = sgt[self.rot('fsg', 2)]
                self.act(sg_, pg, AF.Sigmoid)
                self.tt(sg_, sg_, pl, ALU.mult)
                zc = z[bb][:, cb * 512:(cb + 1) * 512]
                self.stt(zc, x1[bb][:, cb * 512:(cb + 1) * 512], cfg.alpha, ff[bb][:, cb * 512:(cb + 1) * 512], ALU.mult, ALU.add)
                self.tt(zc, zc, sg_, ALU.add, eng='pool')

        def back(tt):
            bb = tt % 2
            self.layernorm(z[bb], g, b, out[bb], tmp, s[bb])
            self.dma(self.x_dst(l, tt), out[bb])

        front(0)
        for tt in range(NTT):
            fr = self.record(front, tt + 1) if tt + 1 < NTT else []
            bk = self.record(back, tt)
            self.play_merged(fr, bk)


def run_cfg(cfg, inp, n_cores, debug=False, stop_after=None):
    DEPTH, D = cfg.DEPTH, cfg.D
    names, carr, c2 = make_consts()
    bld = Builder(cfg, debug=debug, stop_after=stop_after)
    nc = bld.build()
    f = lambda a: np.ascontiguousarray(a, dtype=np.float32)
    shared = {
        'consts': carr, 'consts2': c2,
        'w_in': f(inp['w_in']),
        'gbias': f(np.concatenate([inp['ml_b_i'], inp['ml_b_f'], np.zeros_like(inp['ml_b_i']), inp['gd_dt_bias']], axis=1)),
        'gd_A_log': f(inp['gd_A_log']), 'ml_norm_w': f(inp['ml_norm_w']),
        'rg_conv_wT': f(np.transpose(inp['rg_conv_w'], (0, 2, 1))),
        'rg_vecs': f(np.stack([inp['rg_conv_b'], inp['rg_b_a'], inp['rg_b_x'], inp['rg_lambda']], axis=-1)),
        'rg_w_a': f(inp['rg_w_a']), 'rg_w_x': f(inp['rg_w_x']),
        'gd_conv_wT': f(np.transpose(inp['gd_conv_w'], (0, 2, 1))),
        'gd_norm_w': f(inp['gd_norm_w']),
        'w_up_mlstm': f(inp['w_up_mlstm']), 'w_up_rglru': f(inp['w_up_rglru']), 'w_up_gdn': f(inp['w_up_gdn']),
        'w_out': f(inp['w_out']),
        'ln': f(np.stack([inp['ln1_g'], inp['ln1_b'], inp['ln2_g'], inp['ln2_b']], axis=1)),
        'ffn_w1': f(inp['ffn_w1']), 'ffn_w3': f(inp['ffn_w3']), 'ffn_w2': f(inp['ffn_w2']),
        'moe_router': f(inp['moe_router']), 'moe_w1': f(inp['moe_w1']), 'moe_w3': f(inp['moe_w3']), 'moe_w2': f(inp['moe_w2']),
        'ple_w': f(inp['ple_w']), 'ple_gate_w': f(inp['ple_gate_w']),
    }
    in_maps = []
    for c in range(n_cores):
        sq = c // 2
        sl = slice(16 * c, 16 * c + 16)
        m = dict(shared)
        m['xp'] = f(inp['x_prompt'][sq])
        m['xs'] = f(inp['x_sample'][sl].reshape(128, D))
        m['pp'] = f(inp['p_prompt'][:, sq])
        m['psm'] = f(inp['p_sample'][:, sl].reshape(DEPTH, 128, cfg.PD))
        m['sCT'] = f(np.transpose(inp['state_mlstm_C'][:, sl], (0, 1, 2, 4, 3)))
        m['snT'] = f(np.transpose(inp['state_mlstm_n'][:, sl], (0, 2, 3, 1)))
        m['sm'] = f(inp['state_mlstm_m'][:, sl])
        m['rhT'] = f(np.transpose(inp['state_rglru_h'][:, sl], (0, 2, 1)))
        m['rcT'] = f(np.transpose(inp['state_rglru_conv'][:, sl], (0, 3, 1, 2)))
        m['gS'] = f(inp['state_gdn_S'][:, sl])
        m['gcT'] = f(np.transpose(inp['state_gdn_conv'][:, sl], (0, 3, 1, 2)))
        in_maps.append(m)
    res = run_bass_kernel_spmd(nc, in_maps, core_ids=list(range(n_cores)))
    R = res.results
    if debug:
        return R
    B = n_cores // 2
    NB = n_cores * 16
    SEQ = cfg.SEQ
    y_p = np.zeros((B, SEQ, D), np.float32)
    y_s = np.zeros((NB, 8, D), np.float32)
    pC = np.zeros((DEPTH, B, 4, 256, 128), np.float32)
    pn = np.zeros((DEPTH, B, 4, 128), np.float32)
    pm = np.zeros((DEPTH, B, 4), np.float32)
    prh = np.zeros((DEPTH, B, 1024), np.float32)
    prc = np.zeros((DEPTH, B, 3, 1024), np.float32)
    pgS = np.zeros((DEPTH, B, 4, 128, 256), np.float32)
    pgc = np.zeros((DEPTH, B, 3, 2048), np.float32)
    sC = np.zeros((DEPTH, NB, 4, 256, 128), np.float32)
    sn = np.zeros((DEPTH, NB, 4, 128), np.float32)
    sm = np.zeros((DEPTH, NB, 4), np.float32)
    srh = np.zeros((DEPTH, NB, 1024), np.float32)
    src = np.zeros((DEPTH, NB, 3, 1024), np.float32)
    sgS = np.zeros((DEPTH, NB, 4, 128, 256), np.float32)
    sgc = np.zeros((DEPTH, NB, 3, 2048), np.float32)
    for c in range(n_cores):
        r = R[c]
        sl = slice(16 * c, 16 * c + 16)
        y_s[sl] = r['ys'].reshape(16, 8, D)
        sC[:, sl] = np.transpose(r['o_sCT'][..., 0:256], (0, 1, 2, 4, 3))
        sn[:, sl] = r['o_sCT'][..., 256]
        sm[:, sl] = r['o_sm']
        srh[:, sl] = np.transpose(r['o_rh'][:, :, 1:17], (0, 2, 1))
        oc = r['o_conv'][:, 1].reshape(DEPTH, 16, 8, 3072)[:, :, 5:8, :]
        src[:, sl] = oc[..., 0:1024]
        sgc[:, sl] = oc[..., 1024:3072]
        sgS[:, sl] = r['o_sgS']
        if c % 2 == 0:
            sq = c // 2
            y_p[sq] = r['yp']
            pC[:, sq] = np.transpose(r['o_pCT'][..., 0:256], (0, 1, 3, 2))
            pn[:, sq] = r['o_pCT'][..., 256]
            pm[:, sq] = r['o_pm']
            prh[:, sq] = r['o_rh'][:, :, 0]
            prc[:, sq] = r['o_conv'][:, 0, 125:128, 0:1024]
            pgc[:, sq] = r['o_conv'][:, 0, 125:128, 1024:3072]
            pgS[:, sq] = r['o_pgS']
    return (y_p, y_s, pC, pn, pm, prh, prc, pgS, pgc, sC, sn, sm, srh, src, sgS, sgc)


def kernel(**inputs):
    cfg = Cfg()
    inp = {k: np.asarray(v) for k, v in inputs.items()}
    return run_cfg(cfg, inp, 8)
```

```python
import contextlib
import os
import numpy as np
import concourse.bass as bass
import concourse.mybir as mybir
from concourse.bass_utils import run_bass_kernel_spmd

F32 = mybir.dt.float32
BF16 = mybir.dt.bfloat16
AF = mybir.ActivationFunctionType
ALU = mybir.AluOpType
AX = mybir.AxisListType

ENGS = ('pe', 'act', 'dve', 'pool', 'sp')
SEM_CAP = 30000
N_DMA_SEMS = {'sp': 16, 'act': 4, 'pool': 8}
NEG = -1.0e30
MOE_DBG = set(x for x in os.environ.get('MOE_DBG', '').split(',') if x)


class _Op:
    __slots__ = ('eng', 'fn', 'deps', 'dma', 'needs_inc', 'sem', 'val', 'accum')

    def __init__(self, eng, fn, dma, accum):
        self.eng = eng
        self.fn = fn
        self.dma = dma
        self.accum = accum
        self.deps = ()
        self.needs_inc = False
        self.sem = None
        self.val = 0


class _St:
    __slots__ = ('w', 'wd', 'r', 'rd')

    def __init__(self):
        self.w = {}
        self.wd = []
        self.r = {}
        self.rd = []


class Prog:
    def __init__(self, nc):
        self.nc = nc
        self.ops = {e: [] for e in ENGS}
        self.res = {}
        self.last = {}
        self.dmas = []
        self.pending = {e: [] for e in ENGS}

    def barrier(self):
        deps = list(self.last.values()) + list(self.dmas)
        for e in ENGS:
            self.pending[e] = list(deps)
        self.dmas = []
        self.res = {}

    def op(self, eng, fn, reads=(), writes=(), dma=False, accum=False):
        o = _Op(eng, fn, dma, accum)
        deps = []
        res = self.res
        for k in reads:
            st = res.get(k)
            if st is not None:
                deps.extend(st.w.values())
                deps.extend(st.wd)
                if isinstance(k, str) and k.startswith('ps') and k[2:].isdigit():
                    for re_, ro in st.r.items():
                        if re_ != eng:
                            deps.append(ro)
        for k in writes:
            st = res.get(k)
            if st is not None:
                for we, wo in st.w.items():
                    if accum and wo.accum and we == eng:
                        continue
                    deps.append(wo)
                deps.extend(st.wd)
                deps.extend(st.r.values())
                deps.extend(st.rd)
        if self.pending[eng]:
            deps.extend(self.pending[eng])
            self.pending[eng] = []
        for k in reads:
            st = res.get(k)
            if st is None:
                st = res[k] = _St()
            if dma:
                st.rd.append(o)
            else:
                st.r[eng] = o
        for k in writes:
            st = res.get(k)
            if st is None:
                st = res[k] = _St()
            if dma:
                st.w = {}
                st.wd = [o]
            else:
                st.w = {eng: o}
                st.wd = []
            st.r = {}
            st.rd = []
        dd = []
        seen = set()
        for d in deps:
            if d is o or id(d) in seen:
                continue
            seen.add(id(d))
            dd.append(d)
            d.needs_inc = True
        o.deps = dd
        self.ops[eng].append(o)
        if dma:
            self.dmas.append(o)
        else:
            self.last[eng] = o
        return o

    def emit(self, stack):
        nc = self.nc
        eng_sems = {}
        for e in ENGS:
            n_inc = sum(1 for o in self.ops[e] if (o.needs_inc and not o.dma))
            n_s = max(1, (n_inc + SEM_CAP - 1) // SEM_CAP)
            eng_sems[e] = [stack.enter_context(nc.semaphore(f"c_{e}_{i}")) for i in range(n_s)]
        dma_sems = {}
        for e in ('sp', 'act', 'pool'):
            if any(o.dma for o in self.ops[e]):
                dma_sems[e] = [stack.enter_context(nc.semaphore(f"d_{e}_{i}")) for i in range(N_DMA_SEMS[e])]
        final_dma = {}
        for e in ENGS:
            cnt = 0
            dcnt = 0
            dvals = {}
            for o in self.ops[e]:
                if o.dma:
                    ss = dma_sems[e]
                    s = ss[dcnt % len(ss)]
                    dcnt += 1
                    o.sem = s
                    o.val = dvals.get(id(s), 0) + 16
                    dvals[id(s)] = o.val
                    final_dma[id(s)] = (s, o.val)
                elif o.needs_inc:
                    o.sem = eng_sems[e][cnt // SEM_CAP]
                    o.val = cnt % SEM_CAP + 1
                    cnt += 1
        block = stack.enter_context(nc.Block())
        handles = {'pe': 'tensor', 'act': 'scalar', 'dve': 'vector', 'pool': 'gpsimd', 'sp': 'sync'}

        def make(e):
            def body(h):
                waited = {}
                for o in self.ops[e]:
                    for d in o.deps:
                        key = id(d.sem)
                        if waited.get(key, 0) >= d.val:
                            continue
                        h.wait_ge(d.sem, d.val)
                        waited[key] = d.val
                    if o.dma:
                        key = id(o.sem)
                        if o.val > 16 and waited.get(key, 0) < o.val - 16:
                            h.wait_ge(o.sem, o.val - 16)
                            waited[key] = o.val - 16
                        o.fn(h).then_inc(o.sem, 16)
                    else:
                        ins = o.fn(h)
                        if o.needs_inc:
                            ins.then_inc(o.sem, 1)
                if e == 'sp':
                    for (s, v) in final_dma.values():
                        if waited.get(id(s), 0) < v:
                            h.wait_ge(s, v)
            return body

        for e in ENGS:
            getattr(block, handles[e])(make(e))


class V:
    __slots__ = ('ap', 'key')

    def __init__(self, ap, key):
        self.ap = ap
        self.key = key

    def __getitem__(self, idx):
        return V(self.ap[idx], self.key)

    def bc(self, shape):
        return V(self.ap.broadcast_to(list(shape)), self.key)

    def un(self, axis):
        return V(self.ap.unsqueeze(axis), self.key)

    def k(self, sub):
        return V(self.ap, (self.key, sub))

    def re(self, pat, **kw):
        return V(self.ap.rearrange(pat, **kw), self.key)


class Cfg:
    def __init__(self, D=2048, SEQ=2048, DFF=5632, DFFE=2816, NE=8, PD=256, DEPTH=2):
        self.D, self.SEQ, self.DFF, self.DFFE, self.NE, self.PD, self.DEPTH = D, SEQ, DFF, DFFE, NE, PD, DEPTH
        self.H, self.DK, self.DV, self.LW = 4, 128, 256, 1024
        self.NS, self.TS = 16, 8
        self.KC = D // 128
        self.NT = SEQ + 128
        self.NTT = self.NT // 128
        self.NCH = SEQ // 64
        self.GC = 2048
        o = 0
        self.off = {}
        for name, n in (('mlq', 512), ('mlk', 512), ('mlv', 1024), ('mli', 4), ('mlf', 4), ('mlo', 1024),
                        ('rgx', 1024), ('rgy', 1024), ('gdqkv', 2048), ('gdb', 4), ('gda', 4), ('gdz', 1024),
                        ('mg', 3 * D)):
            self.off[name] = o
            o += n
        self.DIN = o
        self.groups = []
        t = 0
        while t < self.NT:
            n = min(512, self.NT - t)
            self.groups.append((t, n))
            t += n
        self.alpha = (2 * DEPTH) ** 0.25


def make_consts():
    idx = np.arange(128)
    c = {}
    c['ident'] = np.eye(128, dtype=np.float32)
    for nm, B in (('P', 128), ('S', 8)):
        same = (idx[:, None] // B) == (idx[None, :] // B)
        le = idx[:, None] <= idx[None, :]
        c['tri' + nm] = (same & le).astype(np.float32)
        c['negT' + nm] = np.where(same & le, 0.0, NEG).astype(np.float32)
        c['neg' + nm] = np.where(same & le, 0.0, NEG).astype(np.float32).T.copy()
        c['strictT' + nm] = (same & (idx[:, None] < idx[None, :])).astype(np.float32)
    lastP = np.zeros((128, 128), np.float32)
    lastP[63, :] = 1.0
    c['lastP'] = lastP
    lastS = np.zeros((128, 128), np.float32)
    for m in range(128):
        lastS[8 * (m // 8) + 7, m] = 1.0
    c['lastS'] = lastS
    c['ones'] = np.ones((128, 128), np.float32)
    bT = np.zeros((128, 128), np.float32)
    for m in range(128):
        bT[m // 8, m] = 1.0
    c['blockindT'] = bT
    names = ['ident', 'triP', 'negTP', 'negP', 'strictTP', 'lastP', 'triS', 'negTS', 'negS', 'strictTS', 'lastS',
             'ones', 'blockindT']
    arr = np.stack([c[n] for n in names]).astype(np.float32)
    blockind = np.zeros((128, 16), np.float32)
    lastind = np.zeros((128, 16), np.float32)
    for t in range(128):
        blockind[t, t // 8] = 1.0
    for i in range(16):
        lastind[8 * i + 7, i] = 1.0
    bm3 = np.zeros((128, 16, 128), np.float32)
    for i in range(16):
        bm3[:, i, 8 * i:8 * i + 8] = 1.0
    c2 = np.concatenate([blockind, lastind, bm3.reshape(128, 2048)], axis=1).astype(np.float32)
    return names, arr, c2


class Builder:
    def __init__(self, cfg, debug=False, stop_after=None):
        self.cfg = cfg
        self.debug = debug
        self.stop_after = stop_after
        self.nc = bass.Bass("TRN2", target_bir_lowering=False)
        self.P = Prog(self.nc)
        self.AW = 50176
        self.off = 0
        self.mark = 0
        self.dr = {}
        self.rr = {}
        self.rec = None
        self.stopped = False


    def emit_op(self, *a, **kw):
        if self.rec is not None:
            self.rec.append((a, kw))
        else:
            self.P.op(*a, **kw)

    def record(self, fn, *args):
        assert self.rec is None
        self.rec = []
        fn(*args)
        r, self.rec = self.rec, None
        return r

    def play_merged(self, A, B):
        na, nb = len(A), len(B)
        ia = ib = 0
        while ia < na or ib < nb:
            if ib >= nb or (ia < na and ia * nb <= ib * na):
                a, kw = A[ia]
                ia += 1
            else:
                a, kw = B[ib]
                ib += 1
            self.P.op(*a, **kw)

    def dram(self, name, shape, dt=F32, kind="Internal"):
        if self.debug and kind == "Internal":
            kind = "ExternalOutput"
        t = self.nc.dram_tensor(name, list(shape), dt, kind=kind)
        v = V(t.ap(), name)
        self.dr[name] = v
        return v

    def alloc(self, name, shape, dt=F32):
        p = shape[0]
        n = int(np.prod(shape[1:]))
        words = n if dt == F32 else (n + 1) // 2
        off = self.off
        self.off += words
        assert self.off <= self.AW, f"SBUF arena overflow at {name}: {self.off}"
        ap = self.arena[0:p, off:off + words]
        if dt != F32:
            ap = ap.bitcast(dt)[:, 0:n]
        if len(shape) == 3:
            ap = ap.rearrange("p (a b) -> p a b", a=shape[1], b=shape[2])
        elif len(shape) == 4:
            ap = ap.rearrange("p (a b c) -> p a b c", a=shape[1], b=shape[2], c=shape[3])
        return V(ap, name)

    def phase_end(self):
        self.P.barrier()
        self.off = self.mark

    def mm(self, out, lhsT, rhs, start=True, stop=True):
        self.emit_op('pe', lambda h: h.matmul(out.ap, lhsT=lhsT.ap, rhs=rhs.ap, start=start, stop=stop),
                  reads=[lhsT.key, rhs.key], writes=[out.key], accum=True)

    def tr(self, out, in_):
        n = in_.ap.shape[0]
        idt = self.ident[0:n, 0:n]
        self.emit_op('pe', lambda h: h.transpose(out=out.ap, in_=in_.ap, identity=idt.ap),
                  reads=[in_.key, idt.key], writes=[out.key], accum=True)

    def act(self, out, in_, func, bias=None, scale=None):
        reads = [in_.key]
        kw = {}
        if bias is not None:
            if isinstance(bias, V):
                reads.append(bias.key)
                kw['bias'] = bias.ap
            else:
                kw['bias'] = float(bias)
        if scale is not None:
            if isinstance(scale, V):
                reads.append(scale.key)
                kw['scale'] = scale.ap
            else:
                kw['scale'] = float(scale)
        self.emit_op('act', lambda h: h.activation(out=out.ap, in_=in_.ap, func=func, **kw), reads=reads, writes=[out.key])

    def tt(self, out, a, b, op, eng='dve'):
        self.emit_op(eng, lambda h: h.tensor_tensor(out=out.ap, in0=a.ap, in1=b.ap, op=op), reads=[a.key, b.key],
                  writes=[out.key])

    def ts(self, out, a, s1, op0, s2=None, op1=None, eng='dve'):
        reads = [a.key]
        a1 = s1
        if isinstance(s1, V):
            reads.append(s1.key)
            a1 = s1.ap
        a2 = s2
        if isinstance(s2, V):
            reads.append(s2.key)
            a2 = s2.ap
        if op1 is None:
            self.emit_op(eng, lambda h: h.tensor_scalar(out=out.ap, in0=a.ap, scalar1=a1, scalar2=None, op0=op0),
                      reads=reads, writes=[out.key])
        else:
            self.emit_op(eng, lambda h: h.tensor_scalar(out=out.ap, in0=a.ap, scalar1=a1, scalar2=a2, op0=op0, op1=op1),
                      reads=reads, writes=[out.key])

    def stt(self, out, in0, scalar, in1, op0, op1):
        reads = [in0.key, in1.key]
        sc = scalar
        if isinstance(scalar, V):
            reads.append(scalar.key)
            sc = scalar.ap
        self.emit_op('dve', lambda h: h.scalar_tensor_tensor(out=out.ap, in0=in0.ap, scalar=sc, in1=in1.ap, op0=op0, op1=op1),
                  reads=reads, writes=[out.key])

    def red(self, out, in_, op):
        self.emit_op('dve', lambda h: h.tensor_reduce(out=out.ap, in_=in_.ap, axis=AX.X, op=op), reads=[in_.key],
                  writes=[out.key])

    def cp(self, out, in_, eng='dve'):
        if eng == 'act':
            self.emit_op('act', lambda h: h.activation(out=out.ap, in_=in_.ap, func=AF.Copy), reads=[in_.key], writes=[out.key])
        else:
            self.emit_op(eng, lambda h: h.tensor_copy(out=out.ap, in_=in_.ap), reads=[in_.key], writes=[out.key])

    def memset(self, out, val, eng='dve'):
        self.emit_op(eng, lambda h: h.memset(out.ap, val), writes=[out.key])

    def recip(self, out, in_):
        self.emit_op('dve', lambda h: h.reciprocal(out=out.ap, in_=in_.ap), reads=[in_.key], writes=[out.key])

    def scan(self, out, a, u):
        self.emit_op('dve', lambda h: h.tensor_tensor_scan(out=out.ap, data0=a.ap, data1=u.ap, initial=0.0, op0=ALU.mult,
                                                        op1=ALU.add), reads=[a.key, u.key], writes=[out.key])

    def dma(self, out, in_, eng='sp'):
        self.emit_op(eng, lambda h: h.dma_start(out=out.ap, in_=in_.ap), reads=[in_.key], writes=[out.key], dma=True)

    def rot(self, name, n):
        i = self.rr.get(name, 0)
        self.rr[name] = i + 1
        return i % n

    def evac(self, out, in_, i=None):
        if i is None:
            i = self.rot('evac', 2)
        self.cp(out, in_, eng=('dve' if i % 2 == 0 else 'act'))

    def build(self):
        cfg = self.cfg
        nc = self.nc
        D, SEQ, NT, KC, DEPTH = cfg.D, cfg.SEQ, cfg.NT, cfg.KC, cfg.DEPTH
        NCH = cfg.NCH
        dr = self.dram
        EI, EO = "ExternalInput", "ExternalOutput"
        self.xp = dr("xp", [SEQ, D], kind=EI)
        self.xs = dr("xs", [128, D], kind=EI)
        self.pp = dr("pp", [DEPTH, SEQ, cfg.PD], kind=EI)
        self.psm = dr("psm", [DEPTH, 128, cfg.PD], kind=EI)
        self.sCT = dr("sCT", [DEPTH, 16, 4, 128, 256], kind=EI)
        self.snT = dr("snT", [DEPTH, 4, 128, 16], kind=EI)
        self.sm = dr("sm", [DEPTH, 16, 4], kind=EI)
        self.rhT = dr("rhT", [DEPTH, 1024, 16], kind=EI)
        self.rcT = dr("rcT", [DEPTH, 1024, 16, 3], kind=EI)
        self.gS = dr("gS", [DEPTH, 16, 4, 128, 256], kind=EI)
        self.gcT = dr("gcT", [DEPTH, 2048, 16, 3], kind=EI)
        self.consts = dr("consts", [13, 128, 128], kind=EI)
        self.consts2 = dr("consts2", [128, 32 + 2048], kind=EI)
        W = {}
        W['w_in'] = dr("w_in", [DEPTH, D, cfg.DIN], kind=EI)
        W['gbias'] = dr("gbias", [DEPTH, 16], kind=EI)
        W['gd_A_log'] = dr("gd_A_log", [DEPTH, 4], kind=EI)
        W['ml_norm_w'] = dr("ml_norm_w", [DEPTH, 1024], kind=EI)
        W['rg_conv_wT'] = dr("rg_conv_wT", [DEPTH, 1024, 4], kind=EI)
        W['rg_vecs'] = dr("rg_vecs", [DEPTH, 1024, 4], kind=EI)
        W['rg_w_a'] = dr("rg_w_a", [DEPTH, 4, 256, 256], kind=EI)
        W['rg_w_x'] = dr("rg_w_x", [DEPTH, 4, 256, 256], kind=EI)
        W['gd_conv_wT'] = dr("gd_conv_wT", [DEPTH, 2048, 4], kind=EI)
        W['gd_norm_w'] = dr("gd_norm_w", [DEPTH, 256], kind=EI)
        W['w_up_mlstm'] = dr("w_up_mlstm", [DEPTH, 1024, D], kind=EI)
        W['w_up_rglru'] = dr("w_up_rglru", [DEPTH, 1024, D], kind=EI)
        W['w_up_gdn'] = dr("w_up_gdn", [DEPTH, 1024, D], kind=EI)
        W['w_out'] = dr("w_out", [DEPTH, D, D], kind=EI)
        W['ln'] = dr("ln", [DEPTH, 4, D], kind=EI)
        n_dense = (DEPTH + 1) // 2
        n_moe = DEPTH // 2
        W['ffn_w1'] = dr("ffn_w1", [n_dense, D, cfg.DFF], kind=EI)
        W['ffn_w3'] = dr("ffn_w3", [n_dense, D, cfg.DFF], kind=EI)
        W['ffn_w2'] = dr("ffn_w2", [n_dense, cfg.DFF, D], kind=EI)
        W['moe_router'] = dr("moe_router", [max(n_moe, 1), D, cfg.NE], kind=EI)
        W['moe_w1'] = dr("moe_w1", [max(n_moe, 1), cfg.NE, D, cfg.DFFE], kind=EI)
        W['moe_w3'] = dr("moe_w3", [max(n_moe, 1), cfg.NE, D, cfg.DFFE], kind=EI)
        W['moe_w2'] = dr("moe_w2", [max(n_moe, 1), cfg.NE, cfg.DFFE, D], kind=EI)
        W['ple_w'] = dr("ple_w", [DEPTH, cfg.PD, D], kind=EI)
        W['ple_gate_w'] = dr("ple_gate_w", [DEPTH, D, D], kind=EI)
        self.W = W
        self.yp = dr("yp", [SEQ, D], kind=EO)
        self.ys = dr("ys", [128, D], kind=EO)
        self.o_pCT = dr("o_pCT", [DEPTH, 4, 128, 257], kind=EO)
        self.o_pm = dr("o_pm", [DEPTH, 4], kind=EO)
        self.o_rh = dr("o_rh", [DEPTH, 1024, 17], kind=EO)
        self.o_conv = dr("o_conv", [DEPTH, 2, 128, 3072], kind=EO)
        self.o_pgS = dr("o_pgS", [DEPTH, 4, 128, 256], kind=EO)
        self.o_sCT = dr("o_sCT", [DEPTH, 16, 4, 128, 257], kind=EO)
        self.o_sm = dr("o_sm", [DEPTH, 16, 4], kind=EO)
        self.o_sgS = dr("o_sgS", [DEPTH, 16, 4, 128, 256], kind=EO)
        self.FM = dr("FM", [5120, NT])
        self.MG = dr("MG", [3 * D, NT], BF16)
        self.TM = dr("TM", [NT, 3600])
        self.KV = dr("KV", [NT, 1536])
        self.YA = dr("YA", [1024, NT], BF16)
        self.YB = dr("YB", [1024, NT], BF16)
        self.YC = dr("YC", [1024, NT], BF16)
        self.MT = dr("MT", [D, NT], BF16)
        self.X1 = dr("X1", [NT, D])
        self.XC = dr("XC", [NT, D])
        self.FF = dr("FF", [NT, D])
        self.HT = dr("HT", [max(cfg.DFF, cfg.NE * cfg.DFFE), NT], BF16)
        self.Ascr = dr("Ascr", [NCH * 4, 64, 64])
        self.Tscr = dr("Tscr", [NCH * 4, 64, 64])

        with contextlib.ExitStack() as stack:
            self.arena = stack.enter_context(nc.sbuf_tensor("arena", [128, self.AW], F32))
            self.ps = [V(stack.enter_context(nc.psum_tensor(f"ps{i}", [128, 512], F32))[:, :], f"ps{i}") for i in range(8)]
            cn, _, _ = make_consts()
            self.C = {}
            call = self.alloc("call", [128, 13, 128])
            self.dma(call, self.consts.re("c p n -> p c n"))
            for i, n in enumerate(cn):
                self.C[n] = call[:, i, :]
            self.ident = self.C['ident']
            c2 = self.alloc("c2", [128, 32 + 2048])
            self.dma(c2, self.consts2)
            self.blockind = c2[:, 0:16]
            self.lastind = c2[:, 16:32]
            self.bm3 = c2[:, 32:32 + 2048].re("p (i t) -> p i t", i=16, t=128)
            self.mark = self.off
            for l in range(DEPTH):
                self.layer(l)
            self.P.emit(stack)
        return nc

    def x_src(self, l, tt):
        cfg = self.cfg
        if l == 0:
            if tt < cfg.NTT - 1:
                return self.xp[tt * 128:(tt + 1) * 128, :].k(tt)
            return self.xs[:, :].k(tt)
        return self.XC[tt * 128:(tt + 1) * 128, :].k(tt)

    def x_dst(self, l, tt):
        cfg = self.cfg
        if l == cfg.DEPTH - 1:
            if tt < cfg.NTT - 1:
                return self.yp[tt * 128:(tt + 1) * 128, :].k(tt)
            return self.ys[:, :].k(tt)
        return self.XC[tt * 128:(tt + 1) * 128, :].k(tt)

    def layer(self, l):
        for ph in (self.phaseA, self.phaseRG, self.phaseML, self.phaseGD, self.phaseC1, self.phaseC2, self.phaseFFN,
                   self.phaseF):
            if self.stop_after is not None and self.stopped:
                return
            ph(l)
            self.phase_end()
            if self.stop_after == (l, ph.__name__):
                self.stopped = True

    def make_xT(self, xT, src_fn, want_f32=None):
        cfg = self.cfg
        KC = cfg.KC
        xt = [self.alloc(f"xt_stage{i}", [128, cfg.D]) for i in range(2)]
        for tt in range(cfg.NTT):
            st = xt[tt % 2]
            self.dma(st, src_fn(tt))
            for q in range(KC // 4 if KC >= 4 else 1):
                nk = min(4, KC)
                ps = self.ps[self.rot('xTps', 2)]
                for j in range(nk):
                    k = q * 4 + j
                    self.tr(ps[:, j * 128:(j + 1) * 128], st[:, k * 128:(k + 1) * 128])
                dst = xT[:, q * 4:q * 4 + nk, tt * 128:(tt + 1) * 128].k(tt)
                self.evac(dst, ps[:, 0:nk * 128].re("p (a b) -> p a b", a=nk, b=128))

    def phaseA(self, l):
        cfg = self.cfg
        KC, NT, NTT, D = cfg.KC, cfg.NT, cfg.NTT, cfg.D
        xT = self.alloc("xT", [128, KC, NT], BF16)
        self.make_xT(xT, lambda tt: self.x_src(l, tt))
        wbuf = [self.alloc(f"wA{i}", [128, KC, 512], BF16) for i in range(2)]
        stf = [self.alloc(f"stA{i}", [128, 512]) for i in range(4)]
        stb = [self.alloc(f"stAb{i}", [128, 512], BF16) for i in range(2)]
        wg = self.alloc("wAg", [128, KC, 16], BF16)
        win = self.W['w_in']
        o = cfg.off
        segs = [
            (o['mlq'], 512, [('fm', 0, 'q')]),
            (o['mlk'], 512, [('fm', 512, 'c'), ('tm', 0, 'c')]),
            (o['mlv'], 1024, [('tm', 512, 'c')]),
            (o['mlo'], 1024, [('tm', 1536, 'c')]),
            (o['rgx'], 1024, [('fm', 1024, 'c'), ('cv', 0, 'c')]),
            (o['rgy'], 1024, [('fm', 2048, 'c')]),
            (o['gdqkv'], 2048, [('fm', 3072, 'c'), ('cv', 1024, 'c')]),
            (o['gdz'], 1024, [('tm', 2560, 'c')]),
            (o['mg'], 3 * D, [('mg', 0, 's')]),
        ]
        xTr = lambda t0, n: [("xT", tt) for tt in range(t0 // 128, (t0 + n + 127) // 128)]

        def fm_block(wt, sub, row0, kind, dst):
            for (t0, n) in cfg.groups:
                ps = self.ps[2 + self.rot('Aps', 6)]
                for k in range(KC):
                    self.emit_op('pe', lambda h, ps=ps, k=k, t0=t0, n=n: h.matmul(
                        ps.ap[:, 0:n], lhsT=wt.ap[:, k, sub * 128:(sub + 1) * 128], rhs=xT.ap[:, k, t0:t0 + n],
                        start=(k == 0), stop=(k == KC - 1)), reads=[wt.key] + xTr(t0, n), writes=[ps.key], accum=True)
                if kind == 's':
                    st = stb[self.rot('stb', 2)]
                    self.act(st[:, 0:n], ps[:, 0:n], AF.Sigmoid)
                elif kind == 'q':
                    st = stf[self.rot('stf', 4)]
                    self.act(st[:, 0:n], ps[:, 0:n], AF.Copy, scale=cfg.DK ** -0.5)
                else:
                    st = stf[self.rot('stf', 4)]
                    self.evac(st[:, 0:n], ps[:, 0:n])
                self.dma(dst[row0:row0 + 128, t0:t0 + n].k((row0, t0)), st[:, 0:n])

        def tm_block(wt, nc_, col0, dst, tiles, dst_rows=None):
            for ti, tt in enumerate(tiles):
                ps = self.ps[2 + self.rot('Aps', 6)]
                for k in range(KC):
                    self.emit_op('pe', lambda h, ps=ps, k=k, tt=tt: h.matmul(
                        ps.ap[:, 0:nc_], lhsT=xT.ap[:, k, tt * 128:(tt + 1) * 128], rhs=wt.ap[:, k, 0:nc_],
                        start=(k == 0), stop=(k == KC - 1)), reads=[wt.key, ("xT", tt)], writes=[ps.key], accum=True)
                st = stf[self.rot('stf', 4)]
                self.evac(st[:, 0:nc_], ps[:, 0:nc_])
                if dst_rows is None:
                    self.dma(dst[tt * 128:(tt + 1) * 128, col0:col0 + nc_].k((tt, col0)), st[:, 0:nc_])
                else:
                    self.dma(dst_rows(ti)[:, col0:col0 + nc_].k((ti, col0)), st[:, 0:nc_])

        for (c0, ncols, outs) in segs:
            for b0 in range(0, ncols, 512):
                nb = min(512, ncols - b0)
                wt = wbuf[self.rot('wA', 2)]
                self.dma(wt[:, :, 0:nb], win[l, :, c0 + b0:c0 + b0 + nb].re("(k p) n -> p k n", p=128), eng='pool')
                for (mode, base, kind) in outs:
                    if mode == 'fm':
                        for sub in range(nb // 128):
                            fm_block(wt, sub, base + b0 + sub * 128, kind, self.FM)
                    elif mode == 'mg':
                        for sub in range(nb // 128):
                            fm_block(wt, sub, base + b0 + sub * 128, kind, self.MG)
                    elif mode == 'tm':
                        tm_block(wt, nb, base + b0, self.TM, list(range(NTT)))
                    elif mode == 'cv':
                        tm_block(wt, nb, base + b0, None, [NTT - 2, NTT - 1],
                                 dst_rows=lambda ti: self.o_conv[l, ti, :, :])
        self.dma(wg[:, :, 0:8], win[l, :, o['mli']:o['mli'] + 8].re("(k p) n -> p k n", p=128), eng='pool')
        self.dma(wg[:, :, 8:16], win[l, :, o['gdb']:o['gdb'] + 8].re("(k p) n -> p k n", p=128), eng='pool')
        tm_block(wg, 16, 3584, self.TM, list(range(NTT)))

    def conv_fm(self, raw_rows, bufT, cw, out, bias, xpad_p, xpad_s):
        cfg = self.cfg
        SEQ = cfg.SEQ
        self.memset(xpad_p[:, 0:3], 0.0)
        self.dma(xpad_p[:, 3:3 + SEQ], raw_rows[:, 0:SEQ])
        self.dma(xpad_s[:, :, 3:11], raw_rows[:, SEQ:SEQ + 128].re("p (i t) -> p i t", i=16, t=8))
        self.dma(xpad_s[:, :, 0:3], bufT)
        op = out[:, 0:SEQ]
        os_ = out[:, SEQ:SEQ + 128].re("p (i t) -> p i t", i=16, t=8)
        for (o_, xp_, sl) in ((op, xpad_p, lambda j: xpad_p[:, j:j + SEQ]), (os_, xpad_s, lambda j: xpad_s[:, :, j:j + 8])):
            if bias is None:
                self.ts(o_, sl(0), cw[:, 0:1], ALU.mult)
            else:
                self.ts(o_, sl(0), cw[:, 0:1], ALU.mult, bias, ALU.add)
            for j in range(1, 4):
                self.stt(o_, sl(j), cw[:, j:j + 1], o_, ALU.mult, ALU.add)

    def phaseRG(self, l):
        cfg = self.cfg
        NT, SEQ = cfg.NT, cfg.SEQ
        W = self.W
        cw = self.alloc("rg_cw", [128, 8, 4])
        self.dma(cw, W['rg_conv_wT'][l].re("(c p) j -> p c j", p=128))
        vec = self.alloc("rg_vec", [128, 8, 4])
        self.dma(vec, W['rg_vecs'][l].re("(c p) j -> p c j", p=128))
        h0 = self.alloc("rg_h0", [128, 8, 16])
        self.dma(h0, self.rhT[l].re("(c p) i -> p c i", p=128))
        hl = self.alloc("rg_hl", [128, 8, 17])
        nl = self.alloc("rg_nl", [128, 8])
        t1 = self.alloc("rg_t1", [128, 8])
        t2 = self.alloc("rg_t2", [128, 8])
        sp8 = self.alloc("rg_sp8", [128, 8])
        self.ts(nl, vec[:, :, 3], -1.0, ALU.mult)
        self.ts(t1, nl, -1.0, ALU.mult)
        self.tt(t1, t1, nl, ALU.max)
        self.act(t1, t1, AF.Exp, scale=-1.0)
        self.act(t1, t1, AF.Ln, bias=1.0)
        self.ts(t2, nl, 0.0, ALU.max)
        self.tt(t1, t1, t2, ALU.add)
        self.ts(sp8, t1, -8.0, ALU.mult)
        xpad_p = [self.alloc(f"rg_xp{i}", [128, SEQ + 3]) for i in range(2)]
        xpad_s = [self.alloc(f"rg_xs{i}", [128, 16, 11]) for i in range(2)]
        xc = [self.alloc(f"rg_xc{i}", [128, NT]) for i in range(2)]
        wa = self.alloc("rg_wa", [128, 2, 256])
        wx = self.alloc("rg_wx", [128, 2, 256])
        r = self.alloc("rg_r", [128, NT])
        ig = self.alloc("rg_i", [128, NT])
        a = self.alloc("rg_a", [128, NT])
        u = self.alloc("rg_u", [128, NT])
        hh = self.alloc("rg_h", [128, NT])
        gy = self.alloc("rg_gy", [128, NT])
        g2 = self.alloc("rg_g2", [128, NT])
        yb = self.alloc("rg_yb", [128, NT], BF16)
        t16 = self.alloc("rg_t16", [128, 16])
        for n in range(4):
            for c in range(2):
                ch = n * 2 + c
                self.conv_fm(self.FM[1024 + ch * 128:1024 + (ch + 1) * 128, :], self.rcT[l, ch * 128:(ch + 1) * 128, :, :],
                             cw[:, ch, :], xc[c], vec[:, ch, 0:1], xpad_p[c], xpad_s[c])
            self.dma(wa, W['rg_w_a'][l, n].re("(c p) e -> p c e", p=128))
            self.dma(wx, W['rg_w_x'][l, n].re("(c p) e -> p c e", p=128))
            for e in range(2):
                ch = n * 2 + e
                for (wt, dst, bcol) in ((wa, r, 1), (wx, ig, 2)):
                    for (t0, nn) in cfg.groups:
                        ps = self.ps[self.rot('rgps', 4)]
                        for c in range(2):
                            self.mm(ps[:, 0:nn], wt[:, c, e * 128:(e + 1) * 128], xc[c][:, t0:t0 + nn], start=(c == 0), stop=(c == 1))
                        self.act(dst[:, t0:t0 + nn], ps[:, 0:nn], AF.Sigmoid, bias=vec[:, ch, bcol:bcol + 1])
                self.act(a, r, AF.Exp, scale=sp8[:, ch:ch + 1])
                self.tt(u, a, a, ALU.mult)
                self.act(u, u, AF.Sqrt, bias=1.0, scale=-1.0)
                self.tt(ig, ig, xc[e], ALU.mult)
                self.tt(u, u, ig, ALU.mult)
                a_s = a[:, SEQ:SEQ + 128].re("p (i t) -> p i t", i=16, t=8)
                u_s = u[:, SEQ:SEQ + 128].re("p (i t) -> p i t", i=16, t=8)
                self.tt(t16, a_s[:, :, 0], h0[:, ch, :], ALU.mult)
                self.tt(u_s[:, :, 0], u_s[:, :, 0], t16, ALU.add)
                self.memset(a_s[:, :, 0], 0.0)
                self.scan(hh, a, u)
                h_s = hh[:, SEQ:SEQ + 128].re("p (i t) -> p i t", i=16, t=8)
                self.cp(hl[:, ch, 0:1], hh[:, SEQ - 1:SEQ])
                self.cp(hl[:, ch, 1:17], h_s[:, :, 7])
                self.dma(gy, self.FM[2048 + ch * 128:2048 + (ch + 1) * 128, :])
                self.tt(g2, gy, gy, ALU.mult)
                self.ts(g2, g2, 0.044715, ALU.mult, 1.0, ALU.add)
                self.tt(g2, g2, gy, ALU.mult)
                self.act(g2, g2, AF.Sigmoid, scale=1.5957691216057308)
                self.tt(g2, g2, gy, ALU.mult)
                self.tt(yb, g2, hh, ALU.mult)
                self.dma(self.YB[ch * 128:(ch + 1) * 128, :].k(ch), yb)
        self.dma(self.o_rh[l].re("(c p) i -> p c i", p=128), hl)

    def gates(self, l, pre):
        cfg = self.cfg
        NCH, SEQ = cfg.NCH, cfg.SEQ
        gb = self.alloc(pre + "gb", [128, 16])
        self.dma(gb, V(self.W['gbias'].ap[l, :].partition_broadcast(128), 'gbias'))
        nA = self.alloc(pre + "nA", [128, 4])
        self.dma(nA, V(self.W['gd_A_log'].ap[l, :].partition_broadcast(128), 'gd_A_log'))
        self.act(nA, nA, AF.Exp)
        self.ts(nA, nA, -1.0, ALU.mult)
        out = {}
        for nm, L, G, src in (('p', 64, NCH, self.TM[0:SEQ, 3584:3600].re("(c p) g -> p c g", p=64)),
                              ('s', 128, 1, self.TM[SEQ:SEQ + 128, 3584:3600].re("(c p) g -> p c g", p=128))):
            x = self.alloc(pre + "gx" + nm, [L, G, 16])
            self.dma(x, src)
            self.tt(x, x, gb[0:L, :].un(1).bc([L, G, 16]), ALU.add)
            t = self.alloc(pre + "gt" + nm, [L, G, 16])
            self.act(t[:, :, 0:8], x[:, :, 0:8], AF.Tanh, scale=1.0 / 15.0)
            self.ts(t[:, :, 0:8], t[:, :, 0:8], 15.0, ALU.mult)
            self.act(t[:, :, 4:8], t[:, :, 4:8], AF.Exp, scale=-1.0)
            self.act(t[:, :, 4:8], t[:, :, 4:8], AF.Ln, bias=1.0)
            self.ts(t[:, :, 4:8], t[:, :, 4:8], -1.0, ALU.mult)
            self.act(t[:, :, 8:12], x[:, :, 8:12], AF.Sigmoid)
            self.ts(t[:, :, 12:16], x[:, :, 12:16], -1.0, ALU.mult)
            self.tt(t[:, :, 12:16], t[:, :, 12:16], x[:, :, 12:16], ALU.max)
            self.act(t[:, :, 12:16], t[:, :, 12:16], AF.Exp, scale=-1.0)
            self.act(t[:, :, 12:16], t[:, :, 12:16], AF.Ln, bias=1.0)
            self.ts(x[:, :, 12:16], x[:, :, 12:16], 0.0, ALU.max)
            self.tt(t[:, :, 12:16], t[:, :, 12:16], x[:, :, 12:16], ALU.add)
            self.tt(t[:, :, 12:16], t[:, :, 12:16], nA[0:L, :].un(1).bc([L, G, 4]), ALU.mult)
            out[nm] = t
        return out

    def masks(self, mode):
        s = 'P' if mode == 'p' else 'S'
        C = self.C
        return dict(tri=C['tri' + s], negT=C['negT' + s], neg=C['neg' + s], strictT=C['strictT' + s], last=C['last' + s])

    def rowbc(self, ps, col, L, tmp):
        self.cp(tmp[0:L, :, 0:L], col.un(2).bc([L, 4, L]))
        for h in range(4):
            self.mm(ps[0:L, h * L:(h + 1) * L], tmp[0:L, h, 0:L], self.ident[0:L, 0:L])

    def phaseML(self, l):
        cfg = self.cfg
        NT, SEQ, NCH = cfg.NT, cfg.SEQ, cfg.NCH
        al = self.alloc
        qT = al("ml_qT", [128, 4, NT])
        kT = al("ml_kT", [128, 4, NT])
        self.dma(qT, self.FM[0:512, :].re("(h d) t -> d h t", h=4, d=128))
        self.dma(kT, self.FM[512:1024, :].re("(h d) t -> d h t", h=4, d=128))
        G = self.gates(l, "ml_")
        nw = al("ml_nw", [128, 1024])
        self.dma(nw, V(self.W['ml_norm_w'].ap[l, :].partition_broadcast(128), 'ml_norm_w'))
        yaT = [al(f"ml_yaT{i}", [128, 8, 128], BF16) for i in range(2)]
        CT = al("ml_CT", [128, 4, 257])
        self.memset(CT, 0.0)
        m0 = al("ml_m0", [128, 4])
        self.memset(m0, 0.0)
        ktm = al("ml_ktm", [128, 4, 128])
        vaug = al("ml_vaug", [128, 4, 257])
        self.memset(vaug[:, :, 256:257], 1.0)
        otm = al("ml_otm", [128, 1024])
        bc_ = al("ml_bc", [128, 4])
        Bv = al("ml_B", [128, 4])
        big = al("ml_big", [128, 4, 128])
        Rm = al("ml_Rm", [128, 4, 128])
        DT = al("ml_DT", [128, 4, 128])
        ST = al("ml_ST", [128, 4, 128])
        cm = al("ml_cm", [128, 4])
        X12 = al("ml_X12", [128, 12])
        inter = al("ml_inter", [128, 4])
        enm = al("ml_enm", [128, 4])
        lb = al("ml_lb", [128, 12])
        tmpc = al("ml_tmpc", [128, 257])
        nd = al("ml_nd", [128, 4, 257])
        dd = al("ml_dd", [128, 4])
        hh = al("ml_hh", [128, 4, 256])
        sq = al("ml_sq", [128, 4, 256])
        ss = al("ml_ss", [128, 4])
        wsig = al("ml_wsig", [128, 1024])
        ya = al("ml_ya", [128, 1024])
        wend = al("ml_wend", [128, 4])
        kw = al("ml_kw", [128, 4, 128])
        dec = al("ml_dec", [128, 4])
        CTs = al("ml_CTs", [128, 16, 257])
        CTn = al("ml_CTn", [128, 16, 257])
        qTz = al("ml_qTz", [128, 16, 128])
        kwz = al("ml_kwz", [128, 16, 128])
        Wm = al("ml_Wm", [128, 4, 16])
        X64 = al("ml_X64", [128, 4, 16])
        decb = al("ml_decb", [128, 4, 16])
        sm_sb = al("ml_sm", [16, 4])
        sn_sb = al("ml_snsb", [128, 16])
        mo = al("ml_mo", [16, 4])
        ps = self.ps

        def tile(mode, t0, L, ip, lf, part):
            M = self.masks(mode)
            rows = slice(t0, t0 + L)
            if part == 'head':
                self.dma(ktm[0:L], self.TM[rows, 0:512].re("p (h d) -> p h d", h=4, d=128))
                self.dma(vaug[0:L, :, 0:256], self.TM[rows, 512:1536].re("p (h e) -> p h e", h=4, e=256))
                self.dma(otm[0:L], self.TM[rows, 1536:2560])
                self.mm(ps[0][0:L, 0:4], M['tri'][0:L, 0:L], lf)
                self.cp(bc_[0:L], ps[0][0:L, 0:4])
                self.tt(Bv[0:L], ip, bc_[0:L], ALU.subtract)
                self.rowbc(ps[1], Bv[0:L], L, big)
                self.tt(Rm[0:L, :, 0:L], ps[1][0:L, 0:4 * L].re("p (h s) -> p h s", h=4, s=L),
                        M['neg'][0:L, 0:L].un(1).bc([L, 4, L]), ALU.add)
                self.red(cm[0:L], Rm[0:L, :, 0:L], ALU.max)
                self.tt(cm[0:L], cm[0:L], m0[0:L], ALU.max)
                self.ts(X12[0:L, 0:4], cm[0:L], -1.0, ALU.mult)
                self.tt(X12[0:L, 4:8], m0[0:L], cm[0:L], ALU.subtract)
                self.tt(X12[0:L, 8:12], bc_[0:L], cm[0:L], ALU.add)
                self.act(inter[0:L], X12[0:L, 4:8], AF.Exp)
                self.act(enm[0:L], X12[0:L, 8:12], AF.Exp, scale=-1.0)
                self.mm(ps[0][:, 16:28], M['last'][0:L, :], X12[0:L, :])
                self.cp(lb, ps[0][:, 16:28])
                self.rowbc(ps[1], X12[0:L, 0:4], L, big)
                self.tt(Rm[0:L, :, 0:L], ps[1][0:L, 0:4 * L].re("p (h s) -> p h s", h=4, s=L),
                        M['negT'][0:L, 0:L].un(1).bc([L, 4, L]), ALU.add)
                for h in range(4):
                    self.act(DT[0:L, h, 0:L], Rm[0:L, h, 0:L], AF.Exp, bias=Bv[0:L, h:h + 1])
                for h in range(4):
                    self.mm(ps[3][0:L, h * L:(h + 1) * L], kT[:, h, rows], qT[:, h, rows])
                self.tt(ST[0:L, :, 0:L], ps[3][0:L, 0:4 * L].re("p (h s) -> p h s", h=4, s=L), DT[0:L, :, 0:L], ALU.mult)
                for h in range(4):
                    pn = ps[4 + (h % 2) * 2]
                    pc = ps[5 + (h % 2) * 2]
                    self.mm(pn[0:L, 0:257], ST[0:L, h, 0:L], vaug[0:L, h, :])
                    if mode == 'p':
                        self.mm(pc[0:L, 0:257], qT[:, h, rows], CT[:, h, :])
                    else:
                        self.dma(CTs[:, :, 0:256], self.sCT[l, :, h, :, :].re("i d e -> d i e"))
                        self.dma(sn_sb, self.snT[l, h, :, :])
                        self.cp(CTs[:, :, 256], sn_sb)
                        self.tt(qTz, qT[:, h, rows].un(1).bc([128, 16, 128]), self.bm3, ALU.mult)
                        for i in range(16):
                            self.mm(pc[0:L, 0:257], qTz[:, i, :], CTs[:, i, :], start=(i == 0), stop=(i == 15))
                    self.act(tmpc[0:L], pc[0:L, 0:257], AF.Copy, scale=inter[0:L, h:h + 1])
                    self.tt(nd[0:L, h, :], pn[0:L, 0:257], tmpc[0:L], ALU.add)
                    if mode == 's':
                        if h == 0:
                            self.tt(Bv[0:L], Bv[0:L], lb[0:L, 0:4], ALU.add)
                            self.act(wend[0:L], Bv[0:L], AF.Exp)
                            self.tt(Wm, wend.un(2).bc([128, 4, 16]), self.blockind.un(1).bc([128, 4, 16]), ALU.mult)
                            self.tt(X64, inter.un(2).bc([128, 4, 16]), self.lastind.un(1).bc([128, 4, 16]), ALU.mult)
                            self.mm(ps[0][:, 64:128], self.C['ones'], X64.re("p h i -> p (h i)"))
                            self.cp(decb.re("p h i -> p (h i)"), ps[0][:, 64:128])
                        self.tt(kwz, ktm[:, h, :].un(1).bc([128, 16, 128]), Wm[:, h, :].un(2).bc([128, 16, 128]), ALU.mult)
                        for i in range(16):
                            pu = ps[self.rot('mlpu', 2)]
                            self.mm(pu[:, 0:257], kwz[:, i, :], vaug[:, h, :])
                            self.stt(CTn[:, i, :], CTs[:, i, :], decb[:, h, i:i + 1], pu[:, 0:257], ALU.mult, ALU.add)
                        self.dma(self.o_sCT[l, :, h, :, :].re("i d e -> d i e").k(h), CTn)
                if mode == 'p':
                    self.tt(Bv[0:L], Bv[0:L], lb[0:L, 0:4], ALU.add)
                    self.act(wend[0:L], Bv[0:L], AF.Exp)
                    self.tt(kw[0:L], ktm[0:L], wend[0:L].un(2).bc([L, 4, 128]), ALU.mult)
                    self.act(dec, lb[:, 4:8], AF.Exp)
                    for h in range(4):
                        pu = ps[self.rot('mlpu', 2)]
                        self.mm(pu[:, 0:257], kw[0:L, h, :], vaug[0:L, h, :])
                        self.stt(CT[:, h, :], CT[:, h, :], dec[:, h:h + 1], pu[:, 0:257], ALU.mult, ALU.add)
                    self.cp(m0, lb[:, 8:12])
                else:
                    self.mm(ps[0][0:16, 32:36], self.lastind, X12[:, 8:12])
                    self.cp(mo, ps[0][0:16, 32:36])
                    self.dma(self.o_sm[l], mo)

            elif part == 'tailpro':
                self.ts(dd[0:L], nd[0:L, :, 256], -1.0, ALU.mult)
                self.tt(dd[0:L], dd[0:L], nd[0:L, :, 256], ALU.max)
                self.tt(dd[0:L], dd[0:L], enm[0:L], ALU.max)
                self.recip(dd[0:L], dd[0:L])
                self.tt(hh[0:L], nd[0:L, :, 0:256], dd[0:L].un(2).bc([L, 4, 256]), ALU.mult)
                self.act(wsig[0:L], otm[0:L], AF.Sigmoid)
                self.tt(wsig[0:L], wsig[0:L], nw[0:L], ALU.mult)
            else:
                self.tt(sq[0:L], hh[0:L], hh[0:L], ALU.mult)
                self.red(ss[0:L], sq[0:L], ALU.add)
                self.ts(ss[0:L], ss[0:L], 1.0 / 256.0, ALU.mult, 1e-6, ALU.add)
                self.act(ss[0:L], ss[0:L], AF.Sqrt)
                self.recip(ss[0:L], ss[0:L])
                yav = ya[0:L].re("p (h e) -> p h e", h=4, e=256)
                self.tt(yav, hh[0:L], ss[0:L].un(2).bc([L, 4, 256]), ALU.mult)
                self.tt(ya[0:L], ya[0:L], wsig[0:L], ALU.mult)
                for j in range(8):
                    pt = (ps[2] if L == 64 else ps[0]) if (j * L) < 512 else ps[1]
                    c0 = (j * L) % 512
                    self.tr(pt[:, c0:c0 + L], ya[0:L, j * 128:(j + 1) * 128])
                yst = yaT[self.rot('ml_yst', 2)]
                if L == 64:
                    self.cp(yst[:, :, 0:64], ps[2][:, 0:512].re("p (j t) -> p j t", j=8, t=64), eng='act')
                else:
                    self.cp(yst[:, 0:4, :], ps[0][:, 0:512].re("p (j t) -> p j t", j=4, t=128), eng='act')
                    self.cp(yst[:, 4:8, :], ps[1][:, 0:512].re("p (j t) -> p j t", j=4, t=128), eng='act')
                self.dma(self.YA[:, rows].re("(j p) t -> p j t", p=128).k(t0), yst[:, :, 0:L])
        args = lambda c: ('p', c * 64, 64, G['p'][:, c, 0:4], G['p'][:, c, 4:8])
        tile(*args(0), 'head')
        for c in range(NCH):
            tile(*args(c), 'tailpro')
            tl = self.record(tile, *args(c), 'tail')
            hd_ = self.record(tile, *args(c + 1), 'head') if c + 1 < NCH else []
            self.play_merged(hd_, tl)
        self.dma(self.o_pCT[l].re("h d e -> d h e"), CT)
        self.dma(self.o_pm[l:l + 1, :], m0[0:1, :])
        self.dma(sm_sb, self.sm[l])
        self.mm(ps[0][:, 40:44], self.C['blockindT'][0:16, :], sm_sb)
        self.cp(m0, ps[0][:, 40:44])
        for part in ('head', 'tailpro', 'tail'):
            tile('s', SEQ, 128, G['s'][:, 0, 0:4], G['s'][:, 0, 4:8], part)

    def phaseGD(self, l):
        cfg = self.cfg
        NT, SEQ, NCH, NTT = cfg.NT, cfg.SEQ, cfg.NCH, cfg.NTT
        al = self.alloc
        W = self.W
        ps = self.ps
        qT = al("gd_qT", [128, 4, NT])
        kT = al("gd_kT", [128, 4, NT])
        cw = al("gd_cw", [128, 16, 4])
        self.dma(cw, W['gd_conv_wT'][l].re("(c p) j -> p c j", p=128))
        rn_p = al("gd_rnp", [64, NCH, 8])
        rn_s = al("gd_rns", [128, 1, 8])
        mark0 = self.off
        xpad_p = al("gd_xp", [128, SEQ + 3])
        xpad_s = al("gd_xs", [128, 16, 11])
        cv = al("gd_cv", [128, NT])
        sqb = al("gd_sqb", [128, NT])
        stg = [al(f"gd_stg{i}", [128, 128]) for i in range(3)]
        for ch in range(16):
            if ch < 4:
                dst = qT[:, ch, :]
            elif ch < 8:
                dst = kT[:, ch - 4, :]
            else:
                dst = cv
            self.conv_fm(self.FM[3072 + ch * 128:3072 + (ch + 1) * 128, :], self.gcT[l, ch * 128:(ch + 1) * 128, :, :],
                         cw[:, ch, :], cv, None, xpad_p, xpad_s)
            self.act(dst, cv, AF.Silu)
            if ch < 8:
                self.act(sqb, dst, AF.Square)
                for c in range(NCH):
                    self.mm(ps[0][0:64, c * 8 + ch:c * 8 + ch + 1], sqb[:, c * 64:(c + 1) * 64], self.C['ones'][:, 0:1])
                self.mm(ps[1][:, ch:ch + 1], sqb[:, SEQ:SEQ + 128], self.C['ones'][:, 0:1])
            if ch >= 4:
                for tt in range(NTT):
                    pt = ps[2 + self.rot('gdtp', 4)]
                    self.tr(pt[:, 0:128], dst[:, tt * 128:(tt + 1) * 128])
                    st = stg[self.rot('gdstg', 3)]
                    self.evac(st, pt[:, 0:128])
                    self.dma(self.KV[tt * 128:(tt + 1) * 128, (ch - 4) * 128:(ch - 3) * 128].k((tt, ch)), st)
        self.ts(rn_p.re("p c j -> p (c j)"), ps[0][0:64, 0:NCH * 8], 1e-6, ALU.add)
        self.act(rn_p, rn_p, AF.Sqrt)
        self.recip(rn_p, rn_p)
        self.ts(rn_s.re("p c j -> p (c j)"), ps[1][:, 0:8], 1e-6, ALU.add)
        self.act(rn_s, rn_s, AF.Sqrt)
        self.recip(rn_s, rn_s)
        self.P.barrier()
        self.off = mark0
        G = self.gates(l, "gd_")
        gnw = al("gd_gnw", [128, 256])
        self.dma(gnw, V(W['gd_norm_w'].ap[l, :].partition_broadcast(128), 'gd_norm_w'))
        ycT = [al(f"gd_ycT{i}", [128, 8, 128], BF16) for i in range(2)]
        S = al("gd_S", [128, 4, 256])
        self.memset(S, 0.0)
        ktm = al("gd_ktm", [128, 4, 128])
        vtm = al("gd_vtm", [128, 4, 256])
        ztm = al("gd_ztm", [128, 1024])
        gc = al("gd_gc", [128, 4])
        ngc = al("gd_ngc", [128, 4])
        eg = al("gd_eg", [128, 4])
        gl = al("gd_gl", [128, 4])
        egl = al("gd_egl", [128, 4])
        kds = al("gd_kds", [128, 4])
        big = al("gd_big", [128, 4, 128])
        Rm = al("gd_Rm", [128, 4, 128])
        DT = al("gd_DT", [128, 4, 128])
        QKT = al("gd_QKT", [128, 4, 128])
        KKD = al("gd_KKD", [128, 4, 128])
        Am = al("gd_Am", [128, 4, 128])
        TT = al("gd_TT", [128, 4, 128])
        sc4 = al("gd_sc4", [128, 4])
        bv = al("gd_bv", [128, 4, 256])
        bk = al("gd_bk", [128, 4, 128])
        upre = al("gd_upre", [128, 256])
        wT = al("gd_wT", [128, 4, 128])
        u = al("gd_u", [128, 4, 256])
        tmpo = al("gd_tmpo", [128, 256])
        o_ = al("gd_o", [128, 4, 256])
        qs = al("gd_qs", [128, 4])
        egq = al("gd_egq", [128, 4])
        kdec = al("gd_kdec", [128, 4, 128])
        ss = al("gd_ss", [128, 4])
        yc = al("gd_yc", [128, 1024])
        wz = al("gd_wz", [128, 1024])

        set0 = dict(gc=gc, ngc=ngc, eg=eg, gl=gl, egl=egl, kds=kds, big=big, Rm=Rm, DT=DT, QKT=QKT, TT=TT, sc4=sc4, bv=bv, bk=bk, qs=qs, egq=egq, kdec=kdec, ktm=ktm, vtm=vtm, ztm=ztm)

        def common(mode, t0, L, beta, g, rnq, rnk, need_A, bs=None):
            bs = set0 if bs is None else bs
            gc, ngc, big, Rm, DT, sc4 = (bs[n] for n in ('gc', 'ngc', 'big', 'Rm', 'DT', 'sc4'))
            M = self.masks(mode)
            rows = slice(t0, t0 + L)
            self.mm(ps[0][0:L, 0:4], M['tri'][0:L, 0:L], g)
            self.cp(gc[0:L], ps[0][0:L, 0:4])
            self.ts(ngc[0:L], gc[0:L], -1.0, ALU.mult)
            self.rowbc(ps[1], gc[0:L], L, big)
            self.tt(Rm[0:L, :, 0:L], ps[1][0:L, 0:4 * L].re("p (h s) -> p h s", h=4, s=L),
                    M['negT'][0:L, 0:L].un(1).bc([L, 4, L]), ALU.add)
            for h in range(4):
                self.act(DT[0:L, h, 0:L], Rm[0:L, h, 0:L], AF.Exp, bias=ngc[0:L, h:h + 1])
            if need_A:
                for h in range(4):
                    self.mm(ps[2][0:L, h * L:(h + 1) * L], kT[:, h, rows], kT[:, h, rows])
                self.tt(KKD[0:L, :, 0:L], ps[2][0:L, 0:4 * L].re("p (h s) -> p h s", h=4, s=L), DT[0:L, :, 0:L], ALU.mult)
                self.tt(KKD[0:L, :, 0:L], KKD[0:L, :, 0:L], M['strictT'][0:L, 0:L].un(1).bc([L, 4, L]), ALU.mult)
                self.tt(KKD[0:L, :, 0:L], KKD[0:L, :, 0:L], rnk.un(2).bc([L, 4, L]), ALU.mult)
                for h in range(4):
                    self.tr(ps[3][0:L, h * L:(h + 1) * L], KKD[0:L, h, 0:L])
                self.tt(sc4[0:L], beta, rnk, ALU.mult)
                self.tt(Am[0:L, :, 0:L], ps[3][0:L, 0:4 * L].re("p (h s) -> p h s", h=4, s=L),
                        sc4[0:L].un(2).bc([L, 4, L]), ALU.mult)

        for c in range(NCH):
            common('p', c * 64, 64, G['p'][:, c, 8:12], G['p'][:, c, 12:16], rn_p[:, c, 0:4], rn_p[:, c, 4:8], True)
            self.dma(self.Ascr[c * 4:(c + 1) * 4, :, :].re("h t s -> t h s").k(c), Am[0:64, :, 0:64])
        NP = NCH * 4
        mark1 = self.off
        Ab = al("gd_Ab", [NP, 64, 64])
        Tt = al("gd_Tt", [NP, 64, 64])
        prod = al("gd_prod", [NP, 32, 64])
        rr = al("gd_rr", [NP, 64])
        self.emit_op('sp', lambda h: h.dma_start(out=Ab.ap, in_=self.Ascr.ap), reads=[("Ascr", c) for c in range(NCH)],
                  writes=[Ab.key], dma=True)
        self.memset(Tt, 0.0)
        self.memset(Tt[:, 0:1, 0], 1.0)
        for t in range(1, 64):
            for jh in range(2):
                js = slice(jh * 32, jh * 32 + 32)
                self.tt(prod[:, :, 0:t], Tt[:, js, 0:t], Ab[:, t, 0:t].un(1).bc([NP, 32, t]), ALU.mult)
                self.red(rr[:, js], prod[:, :, 0:t], ALU.add)
            self.ts(Tt[:, :, t], rr, -1.0, ALU.mult)
            self.ts(Tt[:, t:t + 1, t], Tt[:, t:t + 1, t], 1.0, ALU.add)
        self.dma(V(self.Tscr.ap, 'Tscr_all'), Tt)
        self.P.barrier()
        self.off = mark1
        def pass2(mode, t0, L, beta, g, rnq, rnk, c, part, bs):
            M = self.masks(mode)
            rows = slice(t0, t0 + L)
            (gc, ngc, eg, gl, egl, kds, big, Rm, DT, QKT, TT, sc4, bv, bk, qs, egq, kdec, ktm, vtm, ztm) = (bs[n] for n in SETN)
            if part == 'load':
                self.dma(ktm[0:L], self.KV[rows, 0:512].re("p (h d) -> p h d", h=4, d=128))
                self.dma(vtm[0:L], self.KV[rows, 512:1536].re("p (h e) -> p h e", h=4, e=256))
                self.dma(ztm[0:L], self.TM[rows, 2560:3584])
                if mode == 'p':
                    self.dma(TT[0:64, :, 0:64], V(self.Tscr.ap[c * 4:(c + 1) * 4, :, :].rearrange("h s t -> s h t"), 'Tscr_all'))
            elif part == 'pre':
                common(mode, t0, L, beta, g, rnq, rnk, mode == 's', bs)
                if mode == 's':
                    self.ts(Nm, Am, -1.0, ALU.mult)
                    for h in range(4):
                        self.tr(ps[2][:, h * 128:(h + 1) * 128], Nm[:, h, :])
                    self.cp(Mm, ps[2][:, :].re("p (h s) -> p h s", h=4, s=128))
                    for h in range(4):
                        self.mm(ps[3][:, h * 128:(h + 1) * 128], Mm[:, h, :], Nm[:, h, :])
                        self.mm(ps[4][:, h * 128:(h + 1) * 128], Nm[:, h, :], Mm[:, h, :])
                    self.cp(N2, ps[3][:, :].re("p (h s) -> p h s", h=4, s=128))
                    self.cp(M2, ps[4][:, :].re("p (h s) -> p h s", h=4, s=128), eng='act')
                    for h in range(4):
                        self.mm(ps[5][:, h * 128:(h + 1) * 128], M2[:, h, :], N2[:, h, :])
                    self.tt(N4, ps[5][:, :].re("p (h s) -> p h s", h=4, s=128), idb, ALU.add)
                    self.tt(N2, N2, idb, ALU.add)
                    self.tt(Mm, Mm, idb, ALU.add)
                    for h in range(4):
                        self.mm(ps[2][:, h * 128:(h + 1) * 128], N2[:, h, :], Mm[:, h, :])
                    self.cp(Q1, ps[2][:, :].re("p (h s) -> p h s", h=4, s=128))
                    for h in range(4):
                        self.mm(ps[3][:, h * 128:(h + 1) * 128], N4[:, h, :], Q1[:, h, :])
                    self.cp(TT, ps[3][:, :].re("p (h s) -> p h s", h=4, s=128))
                for h in range(4):
                    self.mm(ps[4][0:L, h * L:(h + 1) * L], kT[:, h, rows], qT[:, h, rows])
                self.tt(QKT[0:L, :, 0:L], ps[4][0:L, 0:4 * L].re("p (h s) -> p h s", h=4, s=L), DT[0:L, :, 0:L], ALU.mult)
                self.tt(QKT[0:L, :, 0:L], QKT[0:L, :, 0:L], rnk.un(2).bc([L, 4, L]), ALU.mult)
                self.act(eg[0:L], gc[0:L], AF.Exp)
                self.mm(ps[0][:, 16:20], M['last'][0:L, :], gc[0:L])
                self.cp(gl, ps[0][:, 16:20])
                self.act(egl, gl, AF.Exp)
                self.tt(kds[0:L], gl[0:L], gc[0:L], ALU.subtract)
                self.act(kds[0:L], kds[0:L], AF.Exp)
                self.tt(kds[0:L], kds[0:L], rnk, ALU.mult)
                self.tt(sc4[0:L], beta, eg[0:L], ALU.mult)
                self.tt(sc4[0:L], sc4[0:L], rnk, ALU.mult)
                self.ts(qs[0:L], rnq, cfg.DK ** -0.5, ALU.mult)
                self.tt(egq[0:L], eg[0:L], qs[0:L], ALU.mult)
                self.tt(bv[0:L], vtm[0:L], beta.un(2).bc([L, 4, 256]), ALU.mult)
                self.tt(bk[0:L], ktm[0:L], sc4[0:L].un(2).bc([L, 4, 128]), ALU.mult)
                self.tt(kdec[0:L], ktm[0:L], kds[0:L].un(2).bc([L, 4, 128]), ALU.mult)
                if mode == 's':
                    self.tt(X64, gc.un(2).bc([128, 4, 16]), self.lastind.un(1).bc([128, 4, 16]), ALU.mult)
                    self.mm(ps[0][:, 64:128], self.C['ones'], X64.re("p h i -> p (h i)"))
                    self.act(eglb.re("p h i -> p (h i)"), ps[0][:, 64:128], AF.Exp)
            elif part == 'dep':
                for h in range(4):
                    self.mm(ps[5][0:L, 0:256], TT[0:L, h, 0:L], bv[0:L, h, :])
                    self.cp(upre[0:L], ps[5][0:L, 0:256], eng='act')
                    self.mm(ps[6][:, 0:L], bk[0:L, h, :], TT[0:L, h, 0:L])
                    self.cp(wT[:, h, 0:L], ps[6][:, 0:L])
                    if mode == 'p':
                        self.mm(ps[7][0:L, 0:256], wT[:, h, 0:L], S[:, h, :])
                    else:
                        self.dma(Ss, self.gS[l, :, h, :, :].re("i d e -> d i e"))
                        self.tt(wTz, wT[:, h, :].un(1).bc([128, 16, 128]), self.bm3, ALU.mult)
                        for i in range(16):
                            self.mm(ps[7][0:L, 0:256], wTz[:, i, :], Ss[:, i, :], start=(i == 0), stop=(i == 15))
                    self.tt(u[0:L, h, :], upre[0:L], ps[7][0:L, 0:256], ALU.subtract)
                    if mode == 'p':
                        self.mm(ps[5][0:L, 256:512], qT[:, h, rows], S[:, h, :])
                    else:
                        self.tt(qTz, qT[:, h, rows].un(1).bc([128, 16, 128]), self.bm3, ALU.mult)
                        for i in range(16):
                            self.mm(ps[5][0:L, 256:512], qTz[:, i, :], Ss[:, i, :], start=(i == 0), stop=(i == 15))
                    self.act(tmpo[0:L], ps[5][0:L, 256:512], AF.Copy, scale=egq[0:L, h:h + 1])
                    self.mm(ps[6][0:L, 256:512], QKT[0:L, h, 0:L], u[0:L, h, :])
                    self.stt(o_[0:L, h, :], ps[6][0:L, 256:512], qs[0:L, h:h + 1], tmpo[0:L], ALU.mult, ALU.add)
                    if mode == 'p':
                        self.mm(ps[7][:, 256:512], kdec[0:L, h, :], u[0:L, h, :])
                        self.stt(S[:, h, :], S[:, h, :], egl[:, h:h + 1], ps[7][:, 256:512], ALU.mult, ALU.add)
                    else:
                        self.tt(kdz, kdec[:, h, :].un(1).bc([128, 16, 128]), self.blockind.un(2).bc([128, 16, 128]), ALU.mult)
                        for i in range(16):
                            pu = ps[2 + self.rot('gdpu', 2)]
                            self.mm(pu[:, 0:256], kdz[:, i, :], u[:, h, :])
                            self.stt(Ss[:, i, :], Ss[:, i, :], eglb[:, h, i:i + 1], pu[:, 0:256], ALU.mult, ALU.add)
                        self.dma(self.o_sgS[l, :, h, :, :].re("i d e -> d i e").k(h), Ss)
            elif part == 'tailpro':
                ycv = yc[0:L].re("p (h e) -> p h e", h=4, e=256)
                self.tt(ycv, o_[0:L], o_[0:L], ALU.mult)
                self.red(ss[0:L], ycv, ALU.add)
                self.ts(ss[0:L], ss[0:L], 1.0 / 256.0, ALU.mult, 1e-6, ALU.add)
                self.act(ss[0:L], ss[0:L], AF.Sqrt)
                self.recip(ss[0:L], ss[0:L])
                self.act(wz[0:L], ztm[0:L], AF.Silu)
                self.tt(wz[0:L].re("p (h e) -> p h e", h=4, e=256), wz[0:L].re("p (h e) -> p h e", h=4, e=256),
                        gnw[0:L].un(1).bc([L, 4, 256]), ALU.mult)
                self.tt(ycv, o_[0:L], ss[0:L].un(2).bc([L, 4, 256]), ALU.mult)
            else:
                self.tt(yc[0:L], yc[0:L], wz[0:L], ALU.mult)
                for j in range(8):
                    pt = (ps[2] if L == 64 else ps[0]) if (j * L) < 512 else ps[1]
                    c0 = (j * L) % 512
                    self.tr(pt[:, c0:c0 + L], yc[0:L, j * 128:(j + 1) * 128])
                yst = ycT[self.rot('gd_yst', 2)]
                if L == 64:
                    self.cp(yst[:, :, 0:64], ps[2][:, 0:512].re("p (j t) -> p j t", j=8, t=64), eng='act')
                else:
                    self.cp(yst[:, 0:4, :], ps[0][:, 0:512].re("p (j t) -> p j t", j=4, t=128), eng='act')
                    self.cp(yst[:, 4:8, :], ps[1][:, 0:512].re("p (j t) -> p j t", j=4, t=128), eng='act')
                self.dma(self.YC[:, rows].re("(j p) t -> p j t", p=128).k(t0), yst[:, :, 0:L])

        SETN = ('gc', 'ngc', 'eg', 'gl', 'egl', 'kds', 'big', 'Rm', 'DT', 'QKT', 'TT', 'sc4', 'bv', 'bk', 'qs', 'egq', 'kdec', 'ktm', 'vtm', 'ztm')
        set1 = {'gc': al('gd2_gc', [128, 4]), 'ngc': al('gd2_ngc', [128, 4]), 'eg': al('gd2_eg', [128, 4]), 'gl': al('gd2_gl', [128, 4]), 'egl': al('gd2_egl', [128, 4]), 'kds': al('gd2_kds', [128, 4]), 'big': al('gd2_big', [128, 4, 128]), 'Rm': al('gd2_Rm', [128, 4, 128]), 'DT': al('gd2_DT', [128, 4, 128]), 'QKT': al('gd2_QKT', [128, 4, 128]), 'TT': al('gd2_TT', [128, 4, 128]), 'sc4': al('gd2_sc4', [128, 4]), 'bv': al('gd2_bv', [128, 4, 256]), 'bk': al('gd2_bk', [128, 4, 128]), 'qs': al('gd2_qs', [128, 4]), 'egq': al('gd2_egq', [128, 4]), 'kdec': al('gd2_kdec', [128, 4, 128]), 'ktm': al('gd2_ktm', [128, 4, 128]), 'vtm': al('gd2_vtm', [128, 4, 256]), 'ztm': al('gd2_ztm', [128, 1024])}
        sets = (set0, set1)
        pa = lambda c: ('p', c * 64, 64, G['p'][:, c, 8:12], G['p'][:, c, 12:16], rn_p[:, c, 0:4], rn_p[:, c, 4:8], c)
        pass2(*pa(0), 'load', sets[0])
        pass2(*pa(0), 'pre', sets[0])
        for c in range(NCH):
            if c + 1 < NCH:
                pass2(*pa(c + 1), 'load', sets[(c + 1) % 2])
            pass2(*pa(c), 'dep', sets[c % 2])
            A = []
            for part in ('tailpro', 'tail'):
                A += self.record(pass2, *pa(c), part, sets[c % 2])
            Bq = self.record(pass2, *pa(c + 1), 'pre', sets[(c + 1) % 2]) if c + 1 < NCH else []
            self.play_merged(A, Bq)
        self.dma(self.o_pgS[l].re("h d e -> d h e"), S)
        self.P.barrier()
        self.off = mark1
        Ss = al("gd_Ss", [128, 16, 256])
        qTz = al("gd_qTz", [128, 16, 128])
        wTz = qTz
        kdz = al("gd_kdz", [128, 16, 128])
        Nm = al("gd_Nm", [128, 4, 128])
        Mm = al("gd_Mm", [128, 4, 128])
        N2 = al("gd_N2", [128, 4, 128])
        M2 = al("gd_M2", [128, 4, 128])
        N4 = al("gd_N4", [128, 4, 128])
        Q1 = al("gd_Q1", [128, 4, 128])
        X64 = al("gd_X64", [128, 4, 16])
        eglb = al("gd_eglb", [128, 4, 16])
        idb = self.ident.un(1).bc([128, 4, 128])

        for part in ('load', 'pre', 'dep', 'tailpro', 'tail'):
            pass2('s', SEQ, 128, G['s'][:, 0, 8:12], G['s'][:, 0, 12:16], rn_s[:, 0, 0:4], rn_s[:, 0, 4:8], None, part, set0)

    def phaseC1(self, l):
        cfg = self.cfg
        NT, D = cfg.NT, cfg.D
        al = self.alloc
        ys = []
        for nm, src in (('a', self.YA), ('b', self.YB), ('c', self.YC)):
            t = al("c1_y" + nm, [128, 8, NT], BF16)
            self.dma(t, src.re("(j p) t -> p j t", p=128))
            ys.append(t)
        wts = [[al(f"c1_w{x}{i}", [128, 8, 128], BF16) for i in range(2)] for x in range(3)]
        sg = [[al(f"c1_sg{x}{i}", [128, NT], BF16) for i in range(2)] for x in range(3)]
        acc = [al(f"c1_acc{i}", [128, 512]) for i in range(2)]
        tmpb = [al(f"c1_tmp{i}", [128, 512]) for i in range(2)]
        mt = [al(f"c1_mt{i}", [128, NT], BF16) for i in range(2)]
        wn = ('w_up_mlstm', 'w_up_rglru', 'w_up_gdn')
        for j in range(D // 128):
            b = j % 2
            for x in range(3):
                self.dma(wts[x][b], self.W[wn[x]][l, :, j * 128:(j + 1) * 128].re("(k p) n -> p k n", p=128), eng='pool')
                self.dma(sg[x][b], self.MG[x * D + j * 128:x * D + (j + 1) * 128, :])
            for (t0, n) in cfg.groups:
                pss = []
                for x in range(3):
                    ps = self.ps[self.rot('c1ps', 6)]
                    for k in range(8):
                        self.mm(ps[:, 0:n], wts[x][b][:, k, :], ys[x][:, k, t0:t0 + n], start=(k == 0), stop=(k == 7))
                    pss.append(ps)
                a_ = acc[self.rot('c1acc', 2)]
                tm_ = tmpb[self.rot('c1tmp', 2)]
                self.tt(a_[:, 0:n], pss[0][:, 0:n], sg[0][b][:, t0:t0 + n], ALU.mult)
                self.tt(tm_[:, 0:n], pss[1][:, 0:n], sg[1][b][:, t0:t0 + n], ALU.mult)
                self.tt(a_[:, 0:n], a_[:, 0:n], tm_[:, 0:n], ALU.add)
                self.tt(tm_[:, 0:n], pss[2][:, 0:n], sg[2][b][:, t0:t0 + n], ALU.mult)
                self.tt(mt[b][:, t0:t0 + n], a_[:, 0:n], tm_[:, 0:n], ALU.add)
            self.dma(self.MT[j * 128:(j + 1) * 128, :].k(j), mt[b])

    def layernorm(self, z, g, b, out, tmp, s):
        D = self.cfg.D
        self.red(s[:, 0:1], z, ALU.add)
        self.ts(s[:, 0:1], s[:, 0:1], -1.0 / D, ALU.mult)
        self.act(z, z, AF.Identity, bias=s[:, 0:1])
        self.act(tmp, z, AF.Square)
        self.red(s[:, 1:2], tmp, ALU.add)
        self.ts(s[:, 1:2], s[:, 1:2], 1.0 / D, ALU.mult, 1e-5, ALU.add)
        self.act(s[:, 1:2], s[:, 1:2], AF.Sqrt)
        self.recip(s[:, 1:2], s[:, 1:2])
        self.act(z, z, AF.Copy, scale=s[:, 1:2])
        self.tt(z, z, g, ALU.mult, eng='pool')
        self.tt(out, z, b, ALU.add, eng='pool')

    def load_w_bf16(self, dst, src2d, kc):
        N = src2d.ap.shape[1]
        for c0 in range(0, N, 512):
            n = min(512, N - c0)
            self.dma(dst[:, :, c0:c0 + n].k(c0), src2d[:, c0:c0 + n].re("(k p) n -> p k n", p=128), eng='pool')

    def phaseC2(self, l):
        cfg = self.cfg
        NT, D, KC, NTT = cfg.NT, cfg.D, cfg.KC, cfg.NTT
        al = self.alloc
        wout = al("c2_wout", [128, KC, D], BF16)
        self.load_w_bf16(wout, self.W['w_out'][l], KC)
        g = al("c2_g", [128, D])
        b = al("c2_b", [128, D])
        self.dma(g, V(self.W['ln'].ap[l, 0, :].partition_broadcast(128), 'ln'))
        self.dma(b, V(self.W['ln'].ap[l, 1, :].partition_broadcast(128), 'ln'))
        mtl = [al(f"c2_mt{i}", [128, KC, 128], BF16) for i in range(2)]
        xt = [al(f"c2_x{i}", [128, D]) for i in range(2)]
        z = [al(f"c2_z{i}", [128, D]) for i in range(2)]
        tmp = al("c2_tmp", [128, D])
        x1 = [al(f"c2_x1{i}", [128, D]) for i in range(2)]
        s = [al(f"c2_s{i}", [128, 4]) for i in range(2)]
        rkeys = [c0 for c0 in range(0, D, 512)]
        def front(tt):
            bb = tt % 2
            self.dma(mtl[bb], self.MT[:, tt * 128:(tt + 1) * 128].re("(k p) t -> p k t", p=128))
            self.dma(xt[bb], self.x_src(l, tt))
            for cb in range(D // 512):
                ps = self.ps[self.rot('c2ps', 8)]
                for k in range(KC):
                    self.mm(ps, mtl[bb][:, k, :], wout[:, k, cb * 512:(cb + 1) * 512].k(cb * 512), start=(k == 0), stop=(k == KC - 1))
                self.stt(z[bb][:, cb * 512:(cb + 1) * 512], xt[bb][:, cb * 512:(cb + 1) * 512], cfg.alpha, ps, ALU.mult, ALU.add)

        def back(tt):
            bb = tt % 2
            self.layernorm(z[bb], g, b, x1[bb], tmp, s[bb])
            self.dma(self.X1[tt * 128:(tt + 1) * 128, :].k(tt), x1[bb])

        front(0)
        for tt in range(NTT):
            fr = self.record(front, tt + 1) if tt + 1 < NTT else []
            bk = self.record(back, tt)
            self.play_merged(fr, bk)

    def phaseFFN(self, l):
        cfg = self.cfg
        NT, D, KC, NTT = cfg.NT, cfg.D, cfg.KC, cfg.NTT
        al = self.alloc
        moe = (l % 2 == 1)
        j = l // 2
        W = self.W
        if moe:
            NE, DF = cfg.NE, cfg.DFFE
            w1 = lambda e: W['moe_w1'][j, e]
            w3 = lambda e: W['moe_w3'][j, e]
            w2 = lambda e: W['moe_w2'][j, e]
        else:
            NE, DF = 1, cfg.DFF
            w1 = lambda e: W['ffn_w1'][j]
            w3 = lambda e: W['ffn_w3'][j]
            w2 = lambda e: W['ffn_w2'][j]
        x1T = al("ff_x1T", [128, KC, NT], BF16)
        Gbc = None
        if moe:
            Gbc = al("ff_Gbc", [128, NE, NT], BF16)
            rt = al("ff_rt", [128, KC, NE])
            if 'rtdma' not in MOE_DBG:
                self.dma(rt, W['moe_router'][j].re("(k p) e -> p k e", p=128))
            xf = al("ff_xf", [128, KC, 128])
            lg = al("ff_lg", [128, NE])
            lg2 = al("ff_lg2", [128, NE])
            eq1 = al("ff_eq1", [128, NE])
            eq2 = al("ff_eq2", [128, NE])
            gts = al("ff_gts", [128, NE])
            sc = al("ff_sc", [128, 4])
            gtmp = al("ff_gtmp", [128, NE, 128])
        xt = [al(f"ff_xs{i}", [128, D]) for i in range(2)]
        for tt in range(NTT):
            st = xt[tt % 2]
            self.dma(st, self.X1[tt * 128:(tt + 1) * 128, :].k(tt))
            for q in range((KC + 3) // 4):
                nk = min(4, KC - q * 4)
                ps = self.ps[self.rot('xTps', 2)]
                for jj in range(nk):
                    k = q * 4 + jj
                    self.tr(ps[:, jj * 128:(jj + 1) * 128], st[:, k * 128:(k + 1) * 128])
                self.evac(x1T[:, q * 4:q * 4 + nk, tt * 128:(tt + 1) * 128].k(tt), ps[:, 0:nk * 128].re("p (a b) -> p a b", a=nk, b=128))
                if moe and 'xf' not in MOE_DBG:
                    self.cp(xf[:, q * 4:q * 4 + nk, :], ps[:, 0:nk * 128].re("p (a b) -> p a b", a=nk, b=128), eng='act')
            if moe and 'router' in MOE_DBG:
                self.memset(gts, 0.125)
            if moe and 'gbc' in MOE_DBG:
                self.memset(Gbc[:, :, tt * 128:(tt + 1) * 128].k(tt), 1.0)
            if moe and 'router' not in MOE_DBG:
                pl = self.ps[2]
                for k in range(KC):
                    self.mm(pl[:, 0:NE], xf[:, k, :], rt[:, k, :], start=(k == 0), stop=(k == KC - 1))
                self.cp(lg, pl[:, 0:NE])
                self.red(sc[:, 0:1], lg, ALU.max)
                self.ts(eq1, lg, sc[:, 0:1], ALU.is_equal)
                self.stt(lg2, eq1, NEG, lg, ALU.mult, ALU.add)
                self.red(sc[:, 1:2], lg2, ALU.max)
                self.ts(eq2, lg2, sc[:, 1:2], ALU.is_equal)
                self.tt(sc[:, 2:3], sc[:, 1:2], sc[:, 0:1], ALU.subtract)
                self.act(sc[:, 2:3], sc[:, 2:3], AF.Exp)
                self.ts(sc[:, 3:4], sc[:, 2:3], 1.0, ALU.add)
                self.recip(sc[:, 3:4], sc[:, 3:4])
                self.tt(sc[:, 2:3], sc[:, 2:3], sc[:, 3:4], ALU.mult)
                self.ts(eq1, eq1, sc[:, 3:4], ALU.mult)
                self.stt(gts, eq2, sc[:, 2:3], eq1, ALU.mult, ALU.add)
            if moe and 'gbc' not in MOE_DBG:
                self.cp(gtmp, gts.un(2).bc([128, NE, 128]))
                for e0 in range(0, NE, 4):
                    pg = self.ps[3 + self.rot('gbps', 2)]
                    ne = min(4, NE - e0)
                    for e in range(ne):
                        self.mm(pg[:, e * 128:(e + 1) * 128], gtmp[:, e0 + e, :], self.ident)
                    self.cp(Gbc[:, e0:e0 + ne, tt * 128:(tt + 1) * 128].k(tt), pg[:, 0:ne * 128].re("p (a b) -> p a b", a=ne, b=128), eng='act')
        WT = 256 if moe else 512
        w1b = [al(f"ff_w1b{i}", [128, KC, WT], BF16) for i in range(2)]
        w3b = [al(f"ff_w3b{i}", [128, KC, WT], BF16) for i in range(2)]
        sil = [al(f"ff_sil{i}", [128, 512]) for i in range(2)]
        hb = [al(f"ff_hb{i}", [128, 512]) for i in range(2)]
        hst = [al(f"ff_hst{i}", [128, 512], BF16) for i in range(3)]
        xkeys = lambda t0, n: [("ff_x1T", tt) for tt in range(t0 // 128, (t0 + n + 127) // 128)]
        gkeys = lambda t0, n: [("ff_Gbc", tt) for tt in range(t0 // 128, (t0 + n + 127) // 128)]
        for e in range(1 if 'ne1' in MOE_DBG else NE):
            for c0 in range(0, DF, WT):
                nb = min(WT, DF - c0)
                bi = self.rot('ffw', 2)
                self.dma(w1b[bi][:, :, 0:nb], w1(e)[:, c0:c0 + nb].re("(k p) n -> p k n", p=128), eng='pool')
                self.dma(w3b[bi][:, :, 0:nb], w3(e)[:, c0:c0 + nb].re("(k p) n -> p k n", p=128), eng='pool')
                for sub in range(nb // 128):
                    row0 = e * DF + c0 + sub * 128
                    for (t0, n) in cfg.groups:
                        p1 = self.ps[self.rot('ffps', 8)]
                        p3 = self.ps[self.rot('ffps', 8)]
                        for (pp_, wb_) in ((p1, w1b[bi]), (p3, w3b[bi])):
                            for k in range(KC):
                                self.emit_op('pe', lambda h, pp_=pp_, wb_=wb_, k=k, t0=t0, n=n, sub=sub: h.matmul(
                                    pp_.ap[:, 0:n], lhsT=wb_.ap[:, k, sub * 128:(sub + 1) * 128], rhs=x1T.ap[:, k, t0:t0 + n],
                                    start=(k == 0), stop=(k == KC - 1)), reads=[wb_.key] + xkeys(t0, n), writes=[pp_.key], accum=True)
                        si = sil[self.rot('ffsil', 2)]
                        self.act(si[:, 0:n], p1[:, 0:n], AF.Silu)
                        ho = hst[self.rot('ffhst', 3)]
                        if moe:
                            hh_ = hb[self.rot('ffhb', 2)]
                            self.tt(hh_[:, 0:n], si[:, 0:n], p3[:, 0:n], ALU.mult)
                            geng = 'dve' if 'poolmul' in MOE_DBG else 'pool'
                            hnd = (lambda h: h)
                            self.emit_op(geng, lambda h, ho=ho, hh_=hh_, e=e, t0=t0, n=n: h.tensor_tensor(
                                out=ho.ap[:, 0:n], in0=hh_.ap[:, 0:n], in1=Gbc.ap[:, e, t0:t0 + n], op=ALU.mult),
                                reads=[hh_.key] + gkeys(t0, n), writes=[ho.key])
                        else:
                            self.tt(ho[:, 0:n], si[:, 0:n], p3[:, 0:n], ALU.mult)
                        self.dma(self.HT[row0:row0 + 128, t0:t0 + n].k((row0, t0)), ho[:, 0:n])
        self.P.barrier()
        self.off = self.mark
        if self.stop_after == (l, 'phaseFFN_s1'):
            self.stopped = True
            return
        KT = NE * DF // 128
        SEG = 8
        facc = al("ff_facc", [128, NTT, 1024])
        w2b = [al(f"ff_w2b{i}", [128, SEG, 1024], BF16) for i in range(2)]
        hTb = [al(f"ff_hTb{i}", [128, SEG, NT], BF16) for i in range(2)]
        CH = min(1024, D)
        CW = min(512, CH)
        NCI = CH // CW
        for half in range(D // CH):
            first = True
            for e in range(NE):
                kpe = DF // 128
                for k0 in range(0, kpe, SEG):
                    nk = min(SEG, kpe - k0)
                    bi = self.rot('ffw2', 2)
                    r0 = k0 * 128
                    for ci in range(NCI):
                        cc = ci * CW
                        self.dma(w2b[bi][:, 0:nk, cc:cc + CW].k(cc),
                                 w2(e)[r0:r0 + nk * 128, half * CH + cc:half * CH + cc + CW].re("(k p) n -> p k n", p=128), eng='pool')
                    self.dma(hTb[bi][:, 0:nk, :], self.HT[e * DF + r0:e * DF + r0 + nk * 128, :].re("(k p) t -> p k t", p=128))
                    for tt in range(NTT):
                        for ci in range(NCI):
                            pq = self.ps[self.rot('ff2ps', 8)]
                            for k in range(nk):
                                self.mm(pq[:, 0:CW], hTb[bi][:, k, tt * 128:(tt + 1) * 128], w2b[bi][:, k, ci * CW:(ci + 1) * CW].k(ci * CW),
                                        start=(k == 0), stop=(k == nk - 1))
                            dstv = facc[:, tt, ci * CW:(ci + 1) * CW].k((tt, ci))
                            if first:
                                self.evac(dstv, pq[:, 0:CW])
                            else:
                                self.tt(dstv, dstv, pq[:, 0:CW], ALU.add)
                    first = False
            self.emit_op('sp', lambda h, half=half: h.dma_start(
                out=self.FF.ap[:, half * CH:(half + 1) * CH].rearrange("(t p) c -> p t c", p=128), in_=facc.ap[:, :, 0:CH]),
                reads=[("ff_facc", (tt, ci)) for tt in range(NTT) for ci in range(NCI)], writes=[("FF", half)], dma=True)

    def phaseF(self, l):
        cfg = self.cfg
        NT, D, KC, NTT, PD = cfg.NT, cfg.D, cfg.KC, cfg.NTT, cfg.PD
        al = self.alloc
        pgw = al("f_pgw", [128, KC, D], BF16)
        self.load_w_bf16(pgw, self.W['ple_gate_w'][l], KC)
        plw = al("f_plw", [128, PD // 128, D], BF16)
        self.load_w_bf16(plw, self.W['ple_w'][l], PD // 128)
        g = al("f_g", [128, D])
        b = al("f_b", [128, D])
        self.dma(g, V(self.W['ln'].ap[l, 2, :].partition_broadcast(128), 'ln'))
        self.dma(b, V(self.W['ln'].ap[l, 3, :].partition_broadcast(128), 'ln'))
        x1 = [al(f"f_x1{i}", [128, D]) for i in range(2)]
        ff = [al(f"f_ff{i}", [128, D]) for i in range(2)]
        pt = [al(f"f_p{i}", [128, PD]) for i in range(2)]
        x1T = [al(f"f_x1T{i}", [128, KC, 128], BF16) for i in range(2)]
        pT = [al(f"f_pT{i}", [128, PD // 128, 128], BF16) for i in range(2)]
        z = [al(f"f_z{i}", [128, D]) for i in range(2)]
        sgt = [al(f"f_sg{i}", [128, 512]) for i in range(2)]
        tmp = al("f_tmp", [128, D])
        out = [al(f"f_out{i}", [128, D]) for i in range(2)]
        s = [al(f"f_s{i}", [128, 4]) for i in range(2)]
        def front(tt):
            bb = tt % 2
            self.dma(x1[bb], self.X1[tt * 128:(tt + 1) * 128, :].k(tt))
            self.emit_op('sp', lambda h, bb=bb, tt=tt: h.dma_start(out=ff[bb].ap, in_=self.FF.ap[tt * 128:(tt + 1) * 128, :]),
                      reads=[("FF", hf) for hf in range(max(1, D // 1024))], writes=[ff[bb].key], dma=True)
            if tt < NTT - 1:
                self.dma(pt[bb], self.pp[l, tt * 128:(tt + 1) * 128, :])
            else:
                self.dma(pt[bb], self.psm[l])
            for q in range((KC + 3) // 4):
                nk = min(4, KC - q * 4)
                ps = self.ps[self.rot('xTps', 2)]
                for jj in range(nk):
                    k = q * 4 + jj
                    self.tr(ps[:, jj * 128:(jj + 1) * 128], x1[bb][:, k * 128:(k + 1) * 128])
                self.evac(x1T[bb][:, q * 4:q * 4 + nk, :], ps[:, 0:nk * 128].re("p (a b) -> p a b", a=nk, b=128))
            ps = self.ps[self.rot('xTps', 2)]
            for jj in range(PD // 128):
                self.tr(ps[:, jj * 128:(jj + 1) * 128], pt[bb][:, jj * 128:(jj + 1) * 128])
            self.evac(pT[bb], ps[:, 0:PD].re("p (a b) -> p a b", a=PD // 128, b=128))
            for cb in range(D // 512):
                pg = self.ps[2 + self.rot('fps', 6)]
                pl = self.ps[2 + self.rot('fps', 6)]
                for k in range(KC):
                    self.mm(pg, x1T[bb][:, k, :], pgw[:, k, cb * 512:(cb + 1) * 512].k(cb * 512), start=(k == 0), stop=(k == KC - 1))
                for k in range(PD // 128):
                    self.mm(pl, pT[bb][:, k, :], plw[:, k, cb * 512:(cb + 1) * 512].k(cb * 512), start=(k == 0), stop=(k == PD // 128 - 1))
                sg_ = sgt[self.rot('fsg', 2)]
                self.act(sg_, pg, AF.Sigmoid)
                self.tt(sg_, sg_, pl, ALU.mult)
                zc = z[bb][:, cb * 512:(cb + 1) * 512]
                self.stt(zc, x1[bb][:, cb * 512:(cb + 1) * 512], cfg.alpha, ff[bb][:, cb * 512:(cb + 1) * 512], ALU.mult, ALU.add)
                self.tt(zc, zc, sg_, ALU.add, eng='pool')

        def back(tt):
            bb = tt % 2
            self.layernorm(z[bb], g, b, out[bb], tmp, s[bb])
            self.dma(self.x_dst(l, tt), out[bb])

        front(0)
        for tt in range(NTT):
            fr = self.record(front, tt + 1) if tt + 1 < NTT else []
            bk = self.record(back, tt)
            self.play_merged(fr, bk)


def run_cfg(cfg, inp, n_cores, debug=False, stop_after=None):
    DEPTH, D = cfg.DEPTH, cfg.D
    names, carr, c2 = make_consts()
    bld = Builder(cfg, debug=debug, stop_after=stop_after)
    nc = bld.build()
    f = lambda a: np.ascontiguousarray(a, dtype=np.float32)
    shared = {
        'consts': carr, 'consts2': c2,
        'w_in': f(inp['w_in']),
        'gbias': f(np.concatenate([inp['ml_b_i'], inp['ml_b_f'], np.zeros_like(inp['ml_b_i']), inp['gd_dt_bias']], axis=1)),
        'gd_A_log': f(inp['gd_A_log']), 'ml_norm_w': f(inp['ml_norm_w']),
        'rg_conv_wT': f(np.transpose(inp['rg_conv_w'], (0, 2, 1))),
        'rg_vecs': f(np.stack([inp['rg_conv_b'], inp['rg_b_a'], inp['rg_b_x'], inp['rg_lambda']], axis=-1)),
        'rg_w_a': f(inp['rg_w_a']), 'rg_w_x': f(inp['rg_w_x']),
        'gd_conv_wT': f(np.transpose(inp['gd_conv_w'], (0, 2, 1))),
        'gd_norm_w': f(inp['gd_norm_w']),
        'w_up_mlstm': f(inp['w_up_mlstm']), 'w_up_rglru': f(inp['w_up_rglru']), 'w_up_gdn': f(inp['w_up_gdn']),
        'w_out': f(inp['w_out']),
        'ln': f(np.stack([inp['ln1_g'], inp['ln1_b'], inp['ln2_g'], inp['ln2_b']], axis=1)),
        'ffn_w1': f(inp['ffn_w1']), 'ffn_w3': f(inp['ffn_w3']), 'ffn_w2': f(inp['ffn_w2']),
        'moe_router': f(inp['moe_router']), 'moe_w1': f(inp['moe_w1']), 'moe_w3': f(inp['moe_w3']), 'moe_w2': f(inp['moe_w2']),
        'ple_w': f(inp['ple_w']), 'ple_gate_w': f(inp['ple_gate_w']),
    }
    in_maps = []
    for c in range(n_cores):
        sq = c // 2
        sl = slice(16 * c, 16 * c + 16)
        m = dict(shared)
        m['xp'] = f(inp['x_prompt'][sq])
        m['xs'] = f(inp['x_sample'][sl].reshape(128, D))
        m['pp'] = f(inp['p_prompt'][:, sq])
        m['psm'] = f(inp['p_sample'][:, sl].reshape(DEPTH, 128, cfg.PD))
        m['sCT'] = f(np.transpose(inp['state_mlstm_C'][:, sl], (0, 1, 2, 4, 3)))
        m['snT'] = f(np.transpose(inp['state_mlstm_n'][:, sl], (0, 2, 3, 1)))
        m['sm'] = f(inp['state_mlstm_m'][:, sl])
        m['rhT'] = f(np.transpose(inp['state_rglru_h'][:, sl], (0, 2, 1)))
        m['rcT'] = f(np.transpose(inp['state_rglru_conv'][:, sl], (0, 3, 1, 2)))
        m['gS'] = f(inp['state_gdn_S'][:, sl])
        m['gcT'] = f(np.transpose(inp['state_gdn_conv'][:, sl], (0, 3, 1, 2)))
        in_maps.append(m)
    res = run_bass_kernel_spmd(nc, in_maps, core_ids=list(range(n_cores)))
    R = res.results
    if debug:
        return R
    B = n_cores // 2
    NB = n_cores * 16
    SEQ = cfg.SEQ
    y_p = np.zeros((B, SEQ, D), np.float32)
    y_s = np.zeros((NB, 8, D), np.float32)
    pC = np.zeros((DEPTH, B, 4, 256, 128), np.float32)
    pn = np.zeros((DEPTH, B, 4, 128), np.float32)
    pm = np.zeros((DEPTH, B, 4), np.float32)
    prh = np.zeros((DEPTH, B, 1024), np.float32)
    prc = np.zeros((DEPTH, B, 3, 1024), np.float32)
    pgS = np.zeros((DEPTH, B, 4, 128, 256), np.float32)
    pgc = np.zeros((DEPTH, B, 3, 2048), np.float32)
    sC = np.zeros((DEPTH, NB, 4, 256, 128), np.float32)
    sn = np.zeros((DEPTH, NB, 4, 128), np.float32)
    sm = np.zeros((DEPTH, NB, 4), np.float32)
    srh = np.zeros((DEPTH, NB, 1024), np.float32)
    src = np.zeros((DEPTH, NB, 3, 1024), np.float32)
    sgS = np.zeros((DEPTH, NB, 4, 128, 256), np.float32)
    sgc = np.zeros((DEPTH, NB, 3, 2048), np.float32)
    for c in range(n_cores):
        r = R[c]
        sl = slice(16 * c, 16 * c + 16)
        y_s[sl] = r['ys'].reshape(16, 8, D)
        sC[:, sl] = np.transpose(r['o_sCT'][..., 0:256], (0, 1, 2, 4, 3))
        sn[:, sl] = r['o_sCT'][..., 256]
        sm[:, sl] = r['o_sm']
        srh[:, sl] = np.transpose(r['o_rh'][:, :, 1:17], (0, 2, 1))
        oc = r['o_conv'][:, 1].reshape(DEPTH, 16, 8, 3072)[:, :, 5:8, :]
        src[:, sl] = oc[..., 0:1024]
        sgc[:, sl] = oc[..., 1024:3072]
        sgS[:, sl] = r['o_sgS']
        if c % 2 == 0:
            sq = c // 2
            y_p[sq] = r['yp']
            pC[:, sq] = np.transpose(r['o_pCT'][..., 0:256], (0, 1, 3, 2))
            pn[:, sq] = r['o_pCT'][..., 256]
            pm[:, sq] = r['o_pm']
            prh[:, sq] = r['o_rh'][:, :, 0]
            prc[:, sq] = r['o_conv'][:, 0, 125:128, 0:1024]
            pgc[:, sq] = r['o_conv'][:, 0, 125:128, 1024:3072]
            pgS[:, sq] = r['o_pgS']
    return (y_p, y_s, pC, pn, pm, prh, prc, pgS, pgc, sC, sn, sm, srh, src, sgS, sgc)


def kernel(**inputs):
    cfg = Cfg()
    inp = {k: np.asarray(v) for k, v in inputs.items()}
    return run_cfg(cfg, inp, 8)
```
